# Optimizing a Trainium2 kernel written in Bass

```python
import math
import jax
import jax.numpy as jnp
from jax import lax
import numpy as np

D_MODEL = 1024
BATCH = 4
SEQ = 4096
DEPTH = 2

D_MIX = D_MODEL
RWKV_HEADS = 4
RWKV_HEAD_DIM = 64
D_RWKV = RWKV_HEADS * RWKV_HEAD_DIM
DECAY_LORA = 64
AAA_LORA = 64
MV_LORA = 32
GATE_LORA = 128
D_RWKV_SHIFT = 3 * D_RWKV + DECAY_LORA + AAA_LORA + GATE_LORA
W_DECAY_SCALE = 0.6065306597126334
GN_EPS = 64e-5
DIFF_HEADS = 4
DIFF_QK_DIM = 64
DIFF_V_DIM = 2 * DIFF_QK_DIM
D_DIFF_QK = DIFF_HEADS * 2 * DIFF_QK_DIM
D_DIFF = DIFF_HEADS * DIFF_V_DIM
ROT_DIM = DIFF_QK_DIM // 4
ROPE_THETA = 500000.0
Q_BLOCK = 128
SUBLN_EPS = 1e-5
POOL_WINDOWS = (2, 4, 8, 16)
POOL_GROUPS = len(POOL_WINDOWS)
POOL_GROUP_DIM = 64
D_POOL = POOL_GROUPS * POOL_GROUP_DIM
P_IN_FIRST = D_RWKV_SHIFT + 2 * D_DIFF_QK + D_DIFF + D_POOL
P_IN_REST = P_IN_FIRST + MV_LORA
D_FF = 3584
N_EXPERTS = 8
TOP_K = 2
NORM_EPS = 1e-6
N_DENSE = (DEPTH + 1) // 2
N_MOE = DEPTH // 2

kernel_name = 'hybrid_rwkv7_diffattn_pool_moe_encoder'


def split_cols(t, sizes):
    offs = np.cumsum([0] + list(sizes))
    return [t[..., int(offs[i]):int(offs[i + 1])] for i in range(len(sizes))]


def rmsnorm(x, g, eps=NORM_EPS):
    xf = x.astype(jnp.float32)
    y = xf * lax.rsqrt(jnp.mean(xf * xf, axis=-1, keepdims=True) + eps)
    return (y * g.astype(jnp.float32)).astype(x.dtype)


def centred_shift(u, mu):
    prev = jnp.pad(u[:, :-1], ((0, 0), (1, 0), (0, 0)))
    nxt = jnp.pad(u[:, 1:], ((0, 0), (0, 1), (0, 0)))
    return u + mu[0] * (prev - u) + mu[1] * (nxt - u)


def wkv7_scan(r, w, k, v, kk, a, reverse):
    bsz, _, h, n = r.shape

    def step(state, inp):
        r_t, w_t, k_t, v_t, kk_t, a_t = inp
        sa = jnp.einsum('bhvk,bhk->bhv', state, -kk_t)
        state = (state * w_t[:, :, None, :]
                 + sa[..., None] * (kk_t * a_t)[:, :, None, :]
                 + v_t[..., None] * k_t[:, :, None, :])
        return state, jnp.einsum('bhvk,bhk->bhv', state, r_t)

    xs = tuple(jnp.swapaxes(t, 0, 1) for t in (r, w, k, v, kk, a))
    s0 = jnp.zeros((bsz, h, n, n), jnp.float32)
    _, y = lax.scan(step, s0, xs, reverse=reverse)
    return jnp.swapaxes(y, 0, 1)


def rwkv7_mixer(u, v_down, v_first, tshift, decay_bias, decay_up, iclr_bias, iclr_up,
                gate_up, k_k, k_a, r_k, lnx_w, lnx_b, vres_bias, vres_up):
    f32 = jnp.float32
    bsz, seq, _ = u.shape
    us = centred_shift(u, tshift).astype(f32)
    r, k, v, xw, xa, xg = split_cols(us, (D_RWKV, D_RWKV, D_RWKV, DECAY_LORA, AAA_LORA, GATE_LORA))
    if v_first is None:
        v_first = v
    else:
        v = v + (v_first - v) * jax.nn.sigmoid(vres_bias + v_down.astype(f32) @ vres_up)
    g = jax.nn.sigmoid(xg) @ gate_up
    hs = (bsz, seq, RWKV_HEADS, RWKV_HEAD_DIM)
    kk = (k * k_k).reshape(hs)
    kk = kk * lax.rsqrt(jnp.maximum(jnp.sum(kk * kk, axis=-1, keepdims=True), 1e-24))
    rh = r.reshape(hs)
    vh = v.reshape(hs)
    ys = []
    bonuses = []
    for d in range(2):
        w = jnp.exp(-W_DECAY_SCALE * jax.nn.sigmoid(decay_bias[d] + jnp.tanh(xw) @ decay_up[d]))
        a = jax.nn.sigmoid(iclr_bias[d] + xa @ iclr_up[d])
        kd = (k * (1.0 + (a - 1.0) * k_a)).reshape(hs)
        ys.append(wkv7_scan(rh, w.reshape(hs), kd, vh, kk, a.reshape(hs), reverse=(d == 1)))
        bonuses.append(jnp.sum(rh * kd * r_k, axis=-1, keepdims=True) * vh)
    y = ys[0] + ys[1]
    mu = jnp.mean(y, axis=-1, keepdims=True)
    var = jnp.mean(jnp.square(y - mu), axis=-1, keepdims=True)
    y = ((y - mu) * lax.rsqrt(var + GN_EPS)).reshape(bsz, seq, D_RWKV) * lnx_w + lnx_b
    out = (y + (bonuses[0] + bonuses[1]).reshape(bsz, seq, D_RWKV)) * g
    return out.astype(u.dtype), v_first


def partial_rope(t, positions):
    half = ROT_DIM // 2
    inv_freq = jnp.power(ROPE_THETA, -(jnp.arange(half, dtype=jnp.float32) * 2.0 / ROT_DIM))
    ang = positions.astype(jnp.float32)[..., None] * inv_freq
    cos = jnp.cos(ang)[:, :, None, None, :]
    sin = jnp.sin(ang)[:, :, None, None, :]
    tf = t[..., :ROT_DIM].astype(jnp.float32)
    t1, t2 = tf[..., :half], tf[..., half:]
    rot = jnp.concatenate([t1 * cos - t2 * sin, t2 * cos + t1 * sin], axis=-1)
    return jnp.concatenate([rot.astype(t.dtype), t[..., ROT_DIM:]], axis=-1)


def diff_attention(q, k, v, positions, lambda_q, lambda_k, subln_w, lam_init):
    f32 = jnp.float32
    bsz, seq, _ = q.shape
    H, Dk, Dv = DIFF_HEADS, DIFF_QK_DIM, DIFF_V_DIM
    q = partial_rope(q.reshape(bsz, seq, H, 2, Dk), positions)
    k = partial_rope(k.reshape(bsz, seq, H, 2, Dk), positions)
    nb = seq // Q_BLOCK
    qb = q.reshape(bsz, nb, Q_BLOCK, H, 2, Dk).transpose(1, 0, 3, 4, 2, 5)
    kh = k.transpose(0, 2, 3, 1, 4)
    vh = v.reshape(bsz, seq, H, Dv).transpose(0, 2, 1, 3)
    lq = lambda_q.astype(f32)
    lk = lambda_k.astype(f32)
    lam = jnp.exp(jnp.sum(lq[0] * lk[0])) - jnp.exp(jnp.sum(lq[1] * lk[1])) + lam_init
    scale = DIFF_QK_DIM ** -0.5

    def block(q_blk):
        s = jnp.einsum('bhmqd,bhmkd->bhmqk', q_blk, kh).astype(f32) * scale
        p = jax.nn.softmax(s, axis=-1)
        attn = p[:, :, 0] - lam * p[:, :, 1]
        return jnp.einsum('bhqk,bhkd->bhqd', attn.astype(vh.dtype), vh)

    o = lax.map(block, qb)
    o = o.transpose(1, 0, 3, 2, 4).reshape(bsz, seq, H, Dv)
    o = rmsnorm(o, subln_w, SUBLN_EPS) * (1.0 - lam_init)
    return o.reshape(bsz, seq, D_DIFF)


def pool_mixer(u, pool_mix, pool_scale):
    f32 = jnp.float32
    bsz, seq, c = u.shape
    uf = u.astype(f32)
    csum = jnp.concatenate([jnp.zeros((bsz, 1, c), f32), jnp.cumsum(uf, axis=1)], axis=1)
    t = jnp.arange(seq)
    outs = []
    for g, w in enumerate(POOL_WINDOWS):
        sl = slice(g * POOL_GROUP_DIM, (g + 1) * POOL_GROUP_DIM)
        cs = csum[..., sl]
        lo = jnp.clip(t - w // 2, 0, seq)
        hi = jnp.clip(t + (w - w // 2), 0, seq)
        cnt = (hi - lo).astype(f32)[None, :, None]
        outs.append((cs[:, hi] - cs[:, lo]) / cnt - uf[..., sl])
    pooled = jnp.stack(outs, axis=2)
    mixed = jnp.einsum('bsgc,gcd->bsgd', pooled, pool_mix).reshape(bsz, seq, c)
    return (mixed * pool_scale).astype(u.dtype)


def swiglu(h, w_gate, w_up, w_down):
    return (jax.nn.silu(h @ w_gate) * (h @ w_up)) @ w_down


def moe_swiglu(h, router, w_gate, w_up, w_down):
    f32 = jnp.float32
    bsz, seq, d = h.shape
    t = h.reshape(-1, d)
    logits = (t @ router).astype(f32)
    top_val, top_idx = lax.top_k(logits, TOP_K)
    gates = jax.nn.softmax(top_val, axis=-1)
    combine = jnp.sum(jax.nn.one_hot(top_idx, N_EXPERTS, dtype=f32) * gates[..., None], axis=1)
    out = jnp.zeros((t.shape[0], d), f32)
    for e in range(N_EXPERTS):
        out = out + combine[:, e:e + 1] * swiglu(t, w_gate[e], w_up[e], w_down[e]).astype(f32)
    return out.reshape(bsz, seq, d).astype(h.dtype)


def setup_inputs(seed: int = 0) -> dict:
    key = jax.random.key(seed)
    ks = iter(jax.random.split(key, 48))
    f32 = jnp.float32
    L = DEPTH

    def nrm(shape, scale):
        return scale * jax.random.normal(next(ks), shape, f32)

    def gain(shape):
        return 1.0 + 0.1 * jax.random.normal(next(ks), shape, f32)

    def unif(shape, lo, hi):
        return jax.random.uniform(next(ks), shape, f32, lo, hi)

    x = jax.random.normal(next(ks), (BATCH, SEQ, D_MODEL), f32)
    offset = jax.random.randint(next(ks), (BATCH, 1), 0, SEQ, dtype=jnp.int32)
    positions = jnp.arange(SEQ, dtype=jnp.int32)[None, :] + offset
    return {
        'x': x,
        'positions': positions,
        'norm_mix': gain((L, D_MODEL)),
        'w_in_first': nrm((D_MODEL, P_IN_FIRST), D_MODEL ** -0.5),
        'w_in_rest': nrm((L - 1, D_MODEL, P_IN_REST), D_MODEL ** -0.5),
        'tshift': unif((L, 2, D_RWKV_SHIFT), 0.0, 0.5),
        'decay_bias': unif((L, 2, D_RWKV), -3.0, 3.0),
        'decay_up': nrm((L, 2, DECAY_LORA, D_RWKV), DECAY_LORA ** -0.5),
        'iclr_bias': nrm((L, 2, D_RWKV), 0.5),
        'iclr_up': nrm((L, 2, AAA_LORA, D_RWKV), AAA_LORA ** -0.5),
        'gate_up': nrm((L, GATE_LORA, D_RWKV), GATE_LORA ** -0.5),
        'k_k': gain((L, D_RWKV)),
        'k_a': gain((L, D_RWKV)),
        'r_k': nrm((L, RWKV_HEADS, RWKV_HEAD_DIM), 0.3),
        'lnx_w': gain((L, D_RWKV)),
        'lnx_b': nrm((L, D_RWKV), 0.02),
        'vres_bias': nrm((L - 1, D_RWKV), 0.5),
        'vres_up': nrm((L - 1, MV_LORA, D_RWKV), MV_LORA ** -0.5),
        'lambda_q': nrm((L, 2, DIFF_QK_DIM), 0.1),
        'lambda_k': nrm((L, 2, DIFF_QK_DIM), 0.1),
        'subln_w': gain((L, DIFF_V_DIM)),
        'pool_mix': nrm((L, POOL_GROUPS, POOL_GROUP_DIM, POOL_GROUP_DIM), POOL_GROUP_DIM ** -0.5),
        'pool_scale': gain((L, D_POOL)),
        'w_out': nrm((L, D_MIX, D_MODEL), D_MIX ** -0.5),
        'norm_ffn': gain((L, D_MODEL)),
        'ffn_gate': nrm((N_DENSE, D_MODEL, D_FF), D_MODEL ** -0.5),
        'ffn_up': nrm((N_DENSE, D_MODEL, D_FF), D_MODEL ** -0.5),
        'ffn_down': nrm((N_DENSE, D_FF, D_MODEL), D_FF ** -0.5),
        'router': nrm((N_MOE, D_MODEL, N_EXPERTS), D_MODEL ** -0.5),
        'exp_gate': nrm((N_MOE, N_EXPERTS, D_MODEL, D_FF), D_MODEL ** -0.5),
        'exp_up': nrm((N_MOE, N_EXPERTS, D_MODEL, D_FF), D_MODEL ** -0.5),
        'exp_down': nrm((N_MOE, N_EXPERTS, D_FF, D_MODEL), D_FF ** -0.5),
        'norm_out': gain((D_MODEL,)),
    }


def reference(x, positions, norm_mix, w_in_first, w_in_rest, tshift, decay_bias, decay_up,
              iclr_bias, iclr_up, gate_up, k_k, k_a, r_k, lnx_w, lnx_b, vres_bias, vres_up,
              lambda_q, lambda_k, subln_w, pool_mix, pool_scale, w_out, norm_ffn,
              ffn_gate, ffn_up, ffn_down, router, exp_gate, exp_up, exp_down, norm_out):
    v_first = None
    for l in range(DEPTH):
        h = rmsnorm(x, norm_mix[l])
        if l == 0:
            proj = h @ w_in_first
            u_rwkv, q, k, v, u_pool = split_cols(proj, (D_RWKV_SHIFT, D_DIFF_QK, D_DIFF_QK, D_DIFF, D_POOL))
            v_down, vb, vu = None, None, None
        else:
            proj = h @ w_in_rest[l - 1]
            u_rwkv, v_down, q, k, v, u_pool = split_cols(
                proj, (D_RWKV_SHIFT, MV_LORA, D_DIFF_QK, D_DIFF_QK, D_DIFF, D_POOL))
            vb, vu = vres_bias[l - 1], vres_up[l - 1]
        y_a, v_first = rwkv7_mixer(u_rwkv, v_down, v_first, tshift[l], decay_bias[l], decay_up[l],
                                   iclr_bias[l], iclr_up[l], gate_up[l], k_k[l], k_a[l], r_k[l],
                                   lnx_w[l], lnx_b[l], vb, vu)
        lam_init = 0.8 - 0.6 * math.exp(-0.3 * l)
        y_b = diff_attention(q, k, v, positions, lambda_q[l], lambda_k[l], subln_w[l], lam_init)
        y_c = pool_mixer(u_pool, pool_mix[l], pool_scale[l])
        x = x + jnp.concatenate([y_a, y_b, y_c], axis=-1) @ w_out[l]
        h = rmsnorm(x, norm_ffn[l])
        if l % 2 == 0:
            i = l // 2
            x = x + swiglu(h, ffn_gate[i], ffn_up[i], ffn_down[i])
        else:
            i = l // 2
            x = x + moe_swiglu(h, router[i], exp_gate[i], exp_up[i], exp_down[i])
    return rmsnorm(x, norm_out)
```

```python
import numpy as np
import concourse.bass as bass
import concourse.mybir as mybir
from concourse.bass_utils import run_bass_kernel_spmd

F32 = mybir.dt.float32
BF16 = mybir.dt.bfloat16
I32 = mybir.dt.int32
AF = mybir.ActivationFunctionType
ALU = mybir.AluOpType
AX = mybir.AxisListType

SAME_ENG_SYNC = True
ENGS = ["pe", "act", "dve", "pool", "sp"]


class Buf:
    __slots__ = ("name", "w", "rs", "uid", "dram")
    _n = [0]

    def __init__(self, name="", dram=False):
        self.name = name
        self.w = None
        self.rs = []
        Buf._n[0] += 1
        self.uid = Buf._n[0]
        self.dram = dram


class Op:
    __slots__ = ("eng", "fn", "deps", "semkey", "val", "signal", "dma", "force", "cc")


class Prog:
    _uid = [0]

    def __init__(self):
        self.ops = {e: [] for e in ENGS}

    def add(self, eng, fn, reads=(), writes=(), dma=False, semkey=None):
        op = Op()
        op.eng = eng
        op.fn = fn
        op.deps = []
        op.signal = False
        op.val = None
        op.dma = dma
        op.force = []
        op.cc = False
        op.semkey = semkey or (eng + ("_dma" if dma else ""))
        for b in reads:
            if b.w is not None:
                op.deps.append(b.w)
            b.rs.append(op)
        for b in writes:
            if b.w is not None and b.w is not op:
                op.deps.append(b.w)
            op.deps.extend(r for r in b.rs if r is not op)
            b.w = op
            b.rs = []
        self.ops[eng].append(op)
        return op

    @staticmethod
    def _needs_sync(op, d):
        if d.eng != op.eng:
            return True
        if d in op.force:
            return True
        if op.dma or d.dma:
            return True
        if op.eng == "pe":
            return False
        return SAME_ENG_SYNC

    def emit(self, nc, final_waits=()):
        from contextlib import ExitStack
        for e in ENGS:
            for op in self.ops[e]:
                for d in op.deps:
                    if self._needs_sync(op, d):
                        d.signal = True
        for op in final_waits:
            op.signal = True
        last = {}
        for e in ENGS:
            for op in self.ops[e]:
                last[op.semkey] = op
        for op in last.values():
            op.signal = True
        cnt = {}
        for e in ENGS:
            for op in self.ops[e]:
                if op.signal:
                    cnt[op.semkey] = cnt.get(op.semkey, 0) + (16 if (op.dma and not op.cc) else 1)
                    op.val = cnt[op.semkey]
        with ExitStack() as st:
            Prog._uid[0] += 1
            sems = {k: nc.alloc_semaphore(name=f"s{Prog._uid[0]}_" + k) for k in sorted(cnt)}
            block = st.enter_context(nc.Block())

            def run_engine(e, engobj):
                seen = {}
                for op in self.ops[e]:
                    need = {}
                    for d in op.deps:
                        if self._needs_sync(op, d):
                            if need.get(d.semkey, 0) < d.val:
                                need[d.semkey] = d.val
                    for k, v in need.items():
                        if seen.get(k, 0) < v:
                            engobj.wait_ge(sems[k], v)
                            seen[k] = v
                    ins = op.fn(engobj)
                    if op.signal:
                        if op.cc:
                            ins.then_inc(sems[op.semkey])
                        else:
                            ins.then_inc(sems[op.semkey], 16 if op.dma else 1)
                for k in sorted(cnt):
                    if seen.get(k, 0) < cnt[k]:
                        engobj.wait_ge(sems[k], cnt[k])

            block.tensor(lambda eng: run_engine("pe", eng))
            block.scalar(lambda eng: run_engine("act", eng))
            block.vector(lambda eng: run_engine("dve", eng))
            block.gpsimd(lambda eng: run_engine("pool", eng))
            block.sync(lambda eng: run_engine("sp", eng))


class Ph:
    def __init__(self, nc):
        from contextlib import ExitStack
        self.nc = nc
        self.cm = nc.cleanup_on_exit()
        self.cm.__enter__()
        self.st = ExitStack()
        self.P = Prog()
        self.fin = []
        self.n = 0

    _uid = [0]

    def sb(self, shape, dt, name=None):
        Ph._uid[0] += 1
        return self.st.enter_context(self.nc.sbuf_tensor(f"{name or 't'}_{Ph._uid[0]}", list(shape), dt))

    def ps(self, shape, dt, name=None):
        Ph._uid[0] += 1
        return self.st.enter_context(self.nc.psum_tensor(f"{name or 'p'}_{Ph._uid[0]}", list(shape), dt))

    def close(self):
        self.P.emit(self.nc, final_waits=self.fin)
        self.st.close()
        self.cm.__exit__(None, None, None)

    def mm(self, out, lhsT, rhs, start, stop, r, w):
        op = self.P.add("pe", lambda e: e.matmul(out, lhsT=lhsT, rhs=rhs, start=start, stop=stop), r, w)
        rng = (lhsT.base_partition(), lhsT.base_partition() + lhsT.shape[0])
        prev = getattr(self, "_pe_prev", None)
        if prev is not None and (prev[1][1] <= rng[0] or rng[1] <= prev[1][0]):
            op.deps.append(prev[0])
            op.force.append(prev[0])
        self._pe_prev = (op, rng)
        return op

    def tr(self, out, in_, ident, r, w):
        op = self.P.add("pe", lambda e: e.transpose(out, in_, ident), r, w)
        self._pe_prev = (op, (in_.base_partition(), in_.base_partition() + in_.shape[0]))
        return op

    def act(self, out, in_, func, r, w, bias=None, scale=1.0, accum=None):
        def f(e):
            kw = {}
            if bias is not None:
                kw["bias"] = bias
            if accum is not None:
                kw["accum_out"] = accum
            return e.activation(out=out, in_=in_, func=func, scale=scale, **kw)
        return self.P.add("act", f, r, w)

    def tt(self, eng, out, in0, in1, op, r, w):
        return self.P.add(eng, lambda e: e.tensor_tensor(out=out, in0=in0, in1=in1, op=op), r, w)

    def ts(self, eng, out, in0, s1, s2, op0, op1, r, w):
        if s2 is None:
            return self.P.add(eng, lambda e: e.tensor_scalar(out=out, in0=in0, scalar1=s1, scalar2=None, op0=op0), r, w)
        return self.P.add(eng, lambda e: e.tensor_scalar(out=out, in0=in0, scalar1=s1, scalar2=s2, op0=op0, op1=op1), r, w)

    def stt(self, eng, out, in0, scalar, in1, op0, op1, r, w):
        return self.P.add(eng, lambda e: e.scalar_tensor_tensor(out=out, in0=in0, scalar=scalar, in1=in1, op0=op0, op1=op1), r, w)

    def cp(self, eng, out, in_, r, w):
        if eng == "act":
            return self.P.add("act", lambda e: e.copy(out=out, in_=in_), r, w)
        return self.P.add(eng, lambda e: e.tensor_copy(out=out, in_=in_), r, w)

    def memset(self, eng, ap, val, w):
        return self.P.add(eng, lambda e: e.memset(ap, val), (), w)

    def recip(self, out, in_, r, w):
        return self.P.add("dve", lambda e: e.reciprocal(out=out, in_=in_), r, w)

    def scan(self, out, d0, d1, r, w):
        return self.P.add("dve", lambda e: e.tensor_tensor_scan(out=out, data0=d0, data1=d1, initial=0.0, op0=ALU.mult, op1=ALU.add), r, w)

    def dma(self, eng, out, in_, r, w, final=False, key=None):
        slot = None
        for b in list(w) + list(r):
            if not b.dram:
                slot = b
                break
        key = f"{eng}_q{slot.uid}" if slot is not None else f"{eng}_{key or 'x'}"
        op = self.P.add(eng, lambda e: e.dma_start(out=out, in_=in_), r, w, dma=True, semkey=key)
        if final:
            self.fin.append(op)
        return op

    def allgather(self, out_t, in_t, groups, rows_per_chunk):
        nrows = in_t.shape[0]
        RC = rows_per_chunk
        for k in range(nrows // RC):
            Ph._uid[0] += 1
            i_ap = in_t.ap()[k * RC:(k + 1) * RC, :].opt()
            o_ap = out_t.ap()[2 * k * RC:2 * (k + 1) * RC, :].opt()
            op = self.P.add("pool", lambda e, i_ap=i_ap, o_ap=o_ap: e.collective_compute(
                "AllGather", ALU.bypass, replica_groups=groups, ins=[i_ap], outs=[o_ap]),
                (), (), dma=True, semkey=f"pool_cc{Ph._uid[0]}")
            op.cc = True
            self.fin.append(op)

    def rsqrt(self, out, in_, r, w, scale=1.0, bias=None):
        self.act(out, in_, AF.Ln, r, w, bias=bias, scale=scale)
        return self.act(out, out, AF.Exp, w, w, scale=-0.5)


S = 4096
D = 1024
KC = 8
NTB = 8
NTT = 32
DFF = 3584
NE = 8
WDS = 0.6065306597126334
GN_EPS = 64e-5
HP = S + 2


def _norm_to_hT(nc, x, gdummy, hT_dram, rowmap=None, ntt=NTT):
    ph = Ph(nc)
    xt = [ph.sb([128, D], F32) for _ in range(3)]
    bx = [Buf() for _ in range(3)]
    junk = ph.sb([128, D], BF16)
    bj = Buf()
    ss = ph.sb([128, ntt], F32)
    bss = Buf()
    eps = ph.sb([128, 1], F32)
    beps = Buf()
    ident = ph.sb([128, 128], BF16)
    identf = ph.sb([128, 128], F32)
    bid = Buf()
    xn = [ph.sb([128, D], BF16) for _ in range(2)]
    bxn = [Buf() for _ in range(2)]
    pT = [ph.ps([128, KC, 128], BF16) for _ in range(2)]
    bpT = [Buf() for _ in range(2)]
    hs = [ph.sb([128, KC, 512], BF16) for _ in range(2)]
    bhs = [Buf() for _ in range(2)]
    zc = ph.sb([128, KC, 1], BF16)
    bz = Buf()
    ph.memset("pool", eps[:], 1e-6, [beps])
    ph.memset("pool", zc[:], 0.0, [bz])
    ph.memset("pool", identf[:], 1.0, [bid])
    ph.P.add("pool", lambda e: e.affine_select(out=identf[:], in_=identf[:], pattern=[[-1, 128]], compare_op=ALU.is_equal, fill=0.0, base=0, channel_multiplier=1), [bid], [bid])
    ph.cp("dve", ident[:], identf[:], [bid], [bid])
    class _XV:
        def __getitem__(self, t):
            r0 = t * 128 if rowmap is None else rowmap(t)
            return x[r0:r0 + 128, :]
    xv = _XV()
    for t in range(ntt):
        i = t % 3
        ph.dma("sp", xt[i][:], xv[t], [], [bx[i]], key="ld")
        ph.act(junk[:], xt[i][:], AF.Square, [bx[i]], [bj, bss], accum=ss[:, t:t + 1])
    ph.rsqrt(ss[:], ss[:], [bss, beps], [bss], scale=1.0 / D, bias=eps[:])
    for t in range(ntt):
        i = t % 3
        j = t % 2
        ph.dma("sp", xt[i][:], xv[t], [], [bx[i]], key="ld")
        ph.ts("dve", xn[j][:], xt[i][:], ss[:, t:t + 1], None, ALU.mult, None, [bx[i], bss], [bxn[j]])
        for kc in range(KC):
            ph.tr(pT[j][:, kc, :], xn[j][:, kc * 128:(kc + 1) * 128], ident[:], [bxn[j], bid], [bpT[j]])
        g = (t // 4) % 2
        ph.cp("act", hs[g][:, :, (t % 4) * 128:(t % 4 + 1) * 128], pT[j][:], [bpT[j]], [bhs[g]])
        if t % 4 == 3:
            tb = t // 4
            ph.dma("pool", hT_dram[:, :, tb * 512:(tb + 1) * 512], hs[g][:], [bhs[g]], [], final=True, key="st")
    ph.close()


def _load_hT(ph, hT_dram):
    hT = ph.sb([128, KC, HP], BF16, name="hT")
    bh = [Buf() for _ in range(KC)]
    for kc in range(KC):
        ph.memset("pool", hT[:, kc, 0:1], 0.0, [bh[kc]])
        ph.memset("pool", hT[:, kc, HP - 1:HP], 0.0, [bh[kc]])
        if callable(hT_dram):
            for off, ap_ in hT_dram(kc):
                ph.dma("sp" if kc % 2 == 0 else "act", hT[:, kc, 1 + off:1 + off + ap_.shape[1]], ap_, [], [bh[kc]])
        else:
            ph.dma("sp" if kc % 2 == 0 else "act", hT[:, kc, 1:1 + S], hT_dram[:, kc, :], [], [bh[kc]])
    return hT, bh


def _prep_w(ph, w_dram, ncols, gm, bgm, name, colvec=None, bcol=None, dst=None):
    wb = dst if dst is not None else ph.sb([128, KC, ncols], BF16, name=name)
    bw = [Buf() for _ in range(KC)]
    stg = [ph.sb([128, ncols], F32, name=f"{name}_s{i}") for i in range(2)]
    bs = [Buf() for _ in range(2)]
    wv = w_dram.rearrange("(kc p) n -> kc p n", p=128)
    for kc in range(KC):
        i = kc % 2
        ph.dma("sp", stg[i][:], wv[kc], [], [bs[i]], key="ldw")
        if colvec is None:
            ph.ts("pool" if kc % 2 else "dve", wb[:, kc, :], stg[i][:], gm[:, kc:kc + 1], None, ALU.mult, None, [bs[i], bgm], [bw[kc]])
        else:
            ph.stt("dve", wb[:, kc, :], stg[i][:], gm[:, kc:kc + 1], colvec, ALU.mult, ALU.mult, [bs[i], bgm, bcol], [bw[kc]])
    return wb, bw


def _attention(nc, hT_dram, d, lam_init, yT):
    ph = Ph(nc)
    hT, bh = _load_hT(ph, hT_dram)
    gm = ph.sb([128, KC], F32)
    bgm = Buf()
    ph.dma("sp", gm[:], d["gmix"], [], [bgm])
    pp = ph.sb([128, 8], F32)
    bpp = Buf()
    ph.dma("sp", pp[:], d["att_pp"], [], [bpp])
    lqk = ph.sb([128, 2, 128], F32)
    blq = Buf()
    ph.dma("sp", lqk[:, 0, :], d["lam_q"].partition_broadcast(128), [], [blq])
    ph.dma("sp", lqk[:, 1, :], d["lam_k"].partition_broadcast(128), [], [blq])
    lprod = ph.sb([128, 128], F32)
    lsum = ph.sb([128, 2], F32)
    neglam = ph.sb([128, 1], F32)
    sw = ph.sb([128, 1], F32)
    bl = Buf()
    ph.tt("dve", lprod[:], lqk[:, 0, :], lqk[:, 1, :], ALU.mult, [blq], [bl])
    ph.P.add("dve", lambda e: e.reduce_sum(out=lsum[:], in_=lprod[:].rearrange("p (a b) -> p a b", a=2), axis=AX.X), [bl], [bl])
    ph.act(lsum[:], lsum[:], AF.Exp, [bl], [bl])
    ph.tt("dve", neglam[:], lsum[:, 1:2], lsum[:, 0:1], ALU.subtract, [bl], [bl])
    ph.ts("dve", neglam[:], neglam[:], -lam_init, None, ALU.add, None, [bl], [bl])
    ph.ts("dve", sw[:], pp[:, 2:3], 1.0 - lam_init, None, ALU.mult, None, [bpp], [bl])
    cosT = ph.sb([128, S], BF16)
    sinT = ph.sb([128, S], BF16)
    halfpi = ph.sb([128, 1], F32)
    bcs = Buf()
    bhp = Buf()
    ph.memset("pool", halfpi[:], float(np.pi / 2), [bhp])
    CW = 1024
    posi = [ph.sb([128, CW], I32)] * 2
    ang = [ph.sb([128, CW], F32)] * 2
    angf = [ph.sb([128, CW], F32)] * 2
    btt = [Buf()] * 2
    for c4 in range(S // CW):
        j = c4 % 2
        cs_ = slice(c4 * CW, (c4 + 1) * CW)
        bt = btt[j]
        ph.dma("sp", posi[j][:], d["pos"][:, cs_].partition_broadcast(128), [], [bt])
        ph.cp("dve", ang[j][:], posi[j][:], [bt], [bt])
        ph.ts("dve", ang[j][:], ang[j][:], pp[:, 0:1], float(1.0 / (2 * np.pi)), ALU.mult, ALU.mult, [bt, bpp], [bt])
        ph.cp("dve", posi[j][:], ang[j][:], [bt], [bt])
        ph.cp("pool", angf[j][:], posi[j][:], [bt], [bt])
        ph.tt("dve", ang[j][:], ang[j][:], angf[j][:], ALU.subtract, [bt], [bt])
        ph.act(angf[j][:], ang[j][:], AF.Sin, [bt], [bt], scale=float(2 * np.pi))
        ph.ts("dve", sinT[:, cs_], angf[j][:], pp[:, 1:2], None, ALU.mult, None, [bt, bpp], [bcs])
        ph.act(ang[j][:], ang[j][:], AF.Abs, [bt], [bt], scale=float(2 * np.pi))
        ph.act(cosT[:, cs_], ang[j][:], AF.Sin, [bt, bhp], [bcs], scale=-1.0, bias=halfpi[:])
    wat, bwat = _prep_w(ph, d["w_att"], 768, gm, bgm, "wat")
    wsw, bwsw = _prep_w(ph, d["w_att_sw"], 512, gm, bgm, "wsw")
    qk = [ph.sb([128, S], BF16, name=f"qk{c}") for c in range(4)]
    bqk = [Buf() for _ in range(4)]
    pa = [ph.ps([128, 512], F32) for _ in range(2)]
    pb = [ph.ps([128, 512], F32) for _ in range(2)]
    bpa = [Buf() for _ in range(2)]
    bpb = [Buf() for _ in range(2)]
    t1 = [ph.sb([128, 512], F32) for _ in range(2)]
    t2 = [ph.sb([128, 512], F32) for _ in range(2)]
    bt1 = [Buf() for _ in range(2)]
    bt2 = [Buf() for _ in range(2)]
    it = 0
    for cc in range(4):
        for tb in range(NTB):
            j = it % 2
            it += 1
            tsl = slice(tb * 512, (tb + 1) * 512)
            for kc in range(KC):
                ph.mm(pa[j][:], wat[:, kc, cc * 128:(cc + 1) * 128], hT[:, kc, 1 + tb * 512:1 + (tb + 1) * 512], kc == 0, kc == KC - 1, [bwat[kc], bh[kc]], [bpa[j]])
            for kc in range(KC):
                ph.mm(pb[j][:], wsw[:, kc, cc * 128:(cc + 1) * 128], hT[:, kc, 1 + tb * 512:1 + (tb + 1) * 512], kc == 0, kc == KC - 1, [bwsw[kc], bh[kc]], [bpb[j]])
            ph.tt("dve", t1[j][:], pa[j][:], cosT[:, tsl], ALU.mult, [bpa[j], bcs], [bt1[j]])
            ph.tt("dve", t2[j][:], pb[j][:], sinT[:, tsl], ALU.mult, [bpb[j], bcs], [bt2[j]])
            ph.tt("pool", qk[cc][:, tsl], t1[j][:], t2[j][:], ALU.add, [bt1[j], bt2[j]], [bqk[cc]])
    vtm = ph.sb([128, NTT, 256], BF16)
    bv = Buf()
    for t in range(NTT):
        j = t % 2
        for kc in range(KC):
            ph.mm(pa[j][:, 0:256], hT[:, kc, 1 + t * 128:1 + (t + 1) * 128], wat[:, kc, 512:768], kc == 0, kc == KC - 1, [bwat[kc], bh[kc]], [bpa[j]])
        ph.cp("act", vtm[:, t, :], pa[j][:, 0:256], [bpa[j]], [bv])
    ones = ph.sb([128, 128], BF16)
    bones = Buf()
    ph.memset("pool", ones[:], 1.0, [bones])
    eps = ph.sb([128, 1], F32)
    ph.memset("pool", eps[:], 1e-5, [bones])
    NPS, NET, LAG = 2, 3, 1
    psS = [ph.ps([128, 2, 512], F32) for _ in range(NPS)]
    bpsS = [Buf() for _ in range(NPS)]
    eT = [ph.sb([128, 2, 512], BF16) for _ in range(NET)]
    beT = [Buf() for _ in range(NET)]
    accO = pa
    baO = bpa
    accZ = pb
    baZ = bpb
    onesf = ph.sb([128, 128], F32)
    ph.memset("pool", onesf[:], 1.0, [bones])
    _zt = [ang[0][:], angf[0][:]]
    zt2 = ph.sb([128, 1024], F32)
    _zt.append(zt2[:])
    _zt.append(posi[0][:].bitcast(F32))
    _zold = [btt[0], btt[0], None, btt[0]]
    zaccs = [[_zt[bk * 2 + m_] for m_ in range(2)] for bk in range(2)]
    bzaccs = [[Buf() for _ in range(2)] for _ in range(2)]
    zold = [[[_zold[bk * 2 + m_]] if _zold[bk * 2 + m_] is not None else [] for m_ in range(2)] for bk in range(2)]
    rz = [t1[0], t1[1]]
    om = [t2[0], t2[1]]
    brz = [bt1[0], bt1[1]]
    bom = [bt2[0], bt2[1]]
    o = ph.sb([128, 512], F32)
    sq = ph.sb([128, 512], BF16)
    yb = [ph.sb([128, 512], BF16) for _ in range(2)]
    byb = [Buf() for _ in range(2)]
    bo = Buf()
    bsq = Buf()
    scale = 0.125
    NKP = NTT // 2
    its = [(h, qb, m, kp) for h in range(2) for qb in range(NTB) for m in range(2) for kp in range(NKP)]
    n = len(its)

    def finalize(h, qb):
        qs = slice(qb * 512, (qb + 1) * 512)
        zacc, bzacc = zaccs[(h * NTB + qb) % 2], bzaccs[(h * NTB + qb) % 2]
        for m in range(2):
            ph.mm(accZ[m][:], onesf[:], zacc[m][:, 0:512], True, False, [bones, bzacc[m]], [baZ[m]])
            ph.mm(accZ[m][:], onesf[:], zacc[m][:, 512:1024], False, True, [bones, bzacc[m]], [baZ[m]])
            ph.recip(rz[m][:], accZ[m][:], [baZ[m]], [brz[m]])
            ph.tt("dve", om[m][:], accO[m][:], rz[m][:], ALU.mult, [baO[m], brz[m]], [bom[m]])
        ph.stt("dve", o[:], om[1][:], neglam[:, 0:1], om[0][:], ALU.mult, ALU.add, [bom[0], bom[1], bl], [bo])
        ph.tt("pool", sq[:], o[:], o[:], ALU.mult, [bo], [bsq])
        ph.mm(accZ[0][:], ones[:], sq[:], True, True, [bones, bsq], [baZ[0]])
        ph.rsqrt(rz[0][:], accZ[0][:], [baZ[0], bones], [brz[0]], scale=1.0 / 128, bias=eps[:])
        ph.tt("dve", o[:], o[:], rz[0][:], ALU.mult, [bo, brz[0]], [bo])
        y = yb[qb % 2]
        ph.ts("dve", y[:], o[:], sw[:, 0:1], None, ALU.mult, None, [bo, bl], [byb[qb % 2]])
        ph.dma("pool", yT[128 + h * 128:256 + h * 128, qs], y[:], [byb[qb % 2]], [], final=True, key="st")

    for i in range(n + LAG):
        if i < n:
            h, qb, m, kp = its[i]
            ms = slice(m * 64, (m + 1) * 64)
            qs = slice(qb * 512, (qb + 1) * 512)
            a, b = i % NPS, i % NET
            for u in range(2):
                kt = kp * 2 + u
                ph.mm(psS[a][:, u, :], qk[2 + h][ms, kt * 128:(kt + 1) * 128], qk[h][ms, qs], True, True, [bqk[2 + h], bqk[h]], [bpsS[a]])
            ph.act(eT[b][:], psS[a][:], AF.Exp, [bpsS[a]], [beT[b]], scale=scale)
            zacc, bzacc = zaccs[(h * NTB + qb) % 2], bzaccs[(h * NTB + qb) % 2]
            ef = eT[b][:].rearrange("p a n -> p (a n)")
            if kp == 0:
                zo = zold[(h * NTB + qb) % 2][m]
                ph.cp("dve", zacc[m], ef, [beT[b]], [bzacc[m]] + zo)
                del zo[:]
            else:
                ph.tt("dve", zacc[m], zacc[m], ef, ALU.add, [beT[b], bzacc[m]], [bzacc[m]])
        if i >= LAG:
            j = i - LAG
            h, qb, m, kp = its[j]
            b = j % NET
            for u in range(2):
                kt = kp * 2 + u
                ph.mm(accO[m][:], vtm[:, kt, h * 128:(h + 1) * 128], eT[b][:, u, :], kt == 0, kt == NTT - 1, [bv, beT[b]], [baO[m]])
            if m == 1 and kp == NKP - 1:
                finalize(h, qb)
    ph.close()


def _pool_mixer(nc, hT_dram, d, yT):
    ph = Ph(nc)
    hT, bh = _load_hT(ph, hT_dram)
    gm = ph.sb([128, KC], F32)
    bgm = Buf()
    ph.dma("sp", gm[:], d["gmix"], [], [bgm])
    pc = ph.sb([128, 8 + 64], F32)
    bpc = Buf()
    ph.dma("sp", pc[:], d["pool_pp"], [], [bpc])
    wp, bwp = _prep_w(ph, d["w_pool"], 128, gm, bgm, "wp")
    pmx_f = ph.sb([128, 128], F32)
    pmx = ph.sb([128, 128], BF16)
    bpm = Buf()
    ph.dma("sp", pmx_f[:], d["pool_mix_bd"], [], [bpm])
    ph.cp("dve", pmx[:], pmx_f[:], [bpm], [bpm])
    PADW = 8
    W = S + 2 * PADW
    u = ph.sb([128, W], F32)
    s2 = ph.sb([128, W], F32)
    s4 = ph.sb([128, W], F32)
    s8 = ph.sb([128, W], F32)
    s16 = ph.sb([128, W], F32)
    bu, b2, b4, b8, b16 = [Buf() for _ in range(5)]
    ph.memset("pool", u[:, 0:PADW], 0.0, [bu])
    ph.memset("pool", u[:, W - PADW:W], 0.0, [bu])
    pa = [ph.ps([128, 512], F32) for _ in range(2)]
    bpa = [Buf() for _ in range(2)]
    for tb in range(NTB):
        j = tb % 2
        for kc in range(KC):
            ph.mm(pa[j][:], wp[:, kc, :], hT[:, kc, 1 + tb * 512:1 + (tb + 1) * 512], kc == 0, kc == KC - 1, [bwp[kc], bh[kc]], [bpa[j]])
        ph.cp("act", u[:, PADW + tb * 512:PADW + (tb + 1) * 512], pa[j][:], [bpa[j]], [bu])
    c = slice(PADW, PADW + S)

    ph.tt("dve", s2[:, 1:W], u[:, 0:W - 1], u[:, 1:W], ALU.add, [bu], [b2])
    ph.tt("pool", s4[:, 2:W - 1], s2[:, 1:W - 2], s2[:, 3:W], ALU.add, [b2], [b4])
    ph.tt("dve", s8[:, 4:W - 3], s4[:, 2:W - 5], s4[:, 6:W - 1], ALU.add, [b4], [b8])
    ph.tt("pool", s16[:, 8:W - 7], s8[:, 4:W - 11], s8[:, 12:W - 3], ALU.add, [b8], [b16])
    acc = ph.sb([128, S], F32)
    bacc = Buf()
    ph.ts("dve", acc[:], s2[:, c], pc[:, 0:1], None, ALU.mult, None, [b2, bpc], [bacc])
    ph.stt("dve", acc[:], s4[:, c], pc[:, 1:2], acc[:], ALU.mult, ALU.add, [b4, bpc, bacc], [bacc])
    ph.stt("dve", acc[:], s8[:, c], pc[:, 2:3], acc[:], ALU.mult, ALU.add, [b8, bpc, bacc], [bacc])
    ph.stt("dve", acc[:], s16[:, c], pc[:, 3:4], acc[:], ALU.mult, ALU.add, [b16, bpc, bacc], [bacc])
    bt_ = ph.sb([128, 16], F32)
    tmpb = ph.sb([128, 16], F32)
    bbt = Buf()
    for wi, (sw_, bs_) in enumerate(((s2, b2), (s4, b4), (s8, b8), (s16, b16))):
        for half in range(2):
            src = sw_[:, PADW:PADW + 8] if half == 0 else sw_[:, PADW + S - 8:PADW + S]
            dst = bt_[:, half * 8:(half + 1) * 8]
            tb_ = pc[:, 8 + wi * 16 + half * 8:8 + wi * 16 + half * 8 + 8]
            if wi == 0:
                ph.tt("dve", dst, src, tb_, ALU.mult, [bs_, bpc], [bbt])
            else:
                ph.tt("dve", tmpb[:, half * 8:(half + 1) * 8], src, tb_, ALU.mult, [bs_, bpc], [bbt])
                ph.tt("dve", dst, dst, tmpb[:, half * 8:(half + 1) * 8], ALU.add, [bbt], [bbt])
    ph.cp("dve", acc[:, 0:8], bt_[:, 0:8], [bbt, bacc], [bacc])
    ph.cp("dve", acc[:, S - 8:S], bt_[:, 8:16], [bbt, bacc], [bacc])
    pooled = ph.sb([128, S], BF16)
    bpl = Buf()
    ph.tt("dve", pooled[:], acc[:], u[:, c], ALU.subtract, [bacc, bu], [bpl])
    yb = [ph.sb([128, 512], BF16) for _ in range(2)]
    byb = [Buf() for _ in range(2)]
    for tb in range(NTB):
        j = tb % 2
        ph.mm(pa[j][:], pmx[:], pooled[:, tb * 512:(tb + 1) * 512], True, True, [bpm, bpl], [bpa[j]])
        ph.ts("dve", yb[j][:], pa[j][:], pc[:, 4:5], None, ALU.mult, None, [bpa[j], bpc], [byb[j]])
        ph.dma("pool", yT[384:512, tb * 512:(tb + 1) * 512], yb[j][:], [byb[j]], [], final=True, key="st")
    ph.close()


def _rwkv_proj(nc, hT_dram, d, layer, rw):
    ph = Ph(nc)
    hT, bh = _load_hT(ph, hT_dram)
    gm = ph.sb([128, KC], F32)
    bgm = Buf()
    ph.dma("sp", gm[:], d["gmix"], [], [bgm])
    mu0 = ph.sb([128, 640], F32)
    mu1 = ph.sb([128, 640], F32)
    c0 = ph.sb([128, 640], F32)
    bmu = Buf()
    ph.dma("sp", mu0[:], d["mu"][0:1, :].partition_broadcast(128), [], [bmu])
    ph.dma("sp", mu1[:], d["mu"][1:2, :].partition_broadcast(128), [], [bmu])
    ph.tt("dve", c0[:], mu0[:], mu1[:], ALU.add, [bmu], [bmu])
    ph.ts("dve", c0[:], c0[:], -1.0, 1.0, ALU.mult, ALU.add, [bmu], [bmu])
    ws = []
    for i, cv in enumerate((c0, mu0, mu1)):
        ws.append(_prep_w(ph, d["w_rwkv"], 640, gm, bgm, f"wr{i}", colvec=cv[:], bcol=bmu))
    offs = (1, 0, 2)
    gup_f = ph.sb([128, 128], F32)
    gup = ph.sb([128, 128], BF16)
    bgu = Buf()
    ph.dma("sp", gup_f[:], d["gate_up"], [], [bgu])
    ph.cp("dve", gup[:], gup_f[:], [bgu], [bgu])
    if layer > 0:
        wvd, bwvd = _prep_w(ph, d["w_vdown"], 32, gm, bgm, "wvd")
        vup_f = ph.sb([32, 128], F32)
        vup = ph.sb([32, 128], BF16)
        vb_ = ph.sb([128, 1], F32)
        bvu = Buf()
        ph.dma("sp", vup_f[:], d["vres_up"], [], [bvu])
        ph.dma("sp", vb_[:], d["vres_bias"], [], [bvu])
        ph.cp("dve", vup[:], vup_f[:], [bvu], [bvu])
        pvd = ph.ps([32, 512], F32)
        pvg = ph.ps([128, 512], F32)
        bpvd, bpvg = Buf(), Buf()
    pr = [ph.ps([128, 512], F32) for _ in range(5)]
    bpr = [Buf() for _ in range(5)]
    pg = ph.ps([128, 512], F32)
    bpg = Buf()
    ob = [ph.sb([128, 5, 512], BF16) for _ in range(2)]
    bob = [Buf() for _ in range(2)]
    vf = [ph.sb([128, 512], F32) for _ in range(2)]
    bvf = [Buf() for _ in range(2)]
    sgx = ph.sb([128, 512], BF16)
    bsgx = Buf()
    if layer > 0:
        vdb = ph.sb([32, 512], BF16)
        sgv = ph.sb([128, 512], F32)
        dif = ph.sb([128, 512], F32)
        bvdb, bsgv, bdif = Buf(), Buf(), Buf()
    for tb in range(NTB):
        j = tb % 2
        tsl = slice(tb * 512, (tb + 1) * 512)
        for cc in range(5):
            n = 0
            for si in range(3):
                wb, bw = ws[si]
                for kc in range(KC):
                    ph.mm(pr[cc][:], wb[:, kc, cc * 128:(cc + 1) * 128], hT[:, kc, offs[si] + tb * 512:offs[si] + (tb + 1) * 512],
                          n == 0, n == 3 * KC - 1, [bw[kc], bh[kc]], [bpr[cc]])
                    n += 1
        ph.cp("act", ob[j][:, 0, :], pr[0][:], [bpr[0]], [bob[j]])
        ph.cp("dve", ob[j][:, 1, :], pr[1][:], [bpr[1]], [bob[j]])
        ph.cp("act", ob[j][:, 3, :], pr[3][:], [bpr[3]], [bob[j]])
        if layer == 0:
            ph.cp("dve", vf[j][:], pr[2][:], [bpr[2]], [bvf[j]])
            ph.cp("pool", ob[j][:, 2, :], vf[j][:], [bvf[j]], [bob[j]])
            ph.dma("pool", rw["vfirst"][:, tsl], vf[j][:], [bvf[j]], [], final=True, key="st")
        else:
            ph.dma("sp", vf[j][:], rw["vfirst"][:, tsl], [], [bvf[j]], key="ldv")
            for kc in range(KC):
                ph.mm(pvd[:], wvd[:, kc, :], hT[:, kc, 1 + tb * 512:1 + (tb + 1) * 512], kc == 0, kc == KC - 1, [bwvd[kc], bh[kc]], [bpvd])
            ph.cp("act", vdb[:], pvd[:], [bpvd], [bvdb])
            ph.mm(pvg[:], vup[:], vdb[:], True, True, [bvu, bvdb], [bpvg])
            ph.act(sgv[:], pvg[:], AF.Sigmoid, [bpvg, bvu], [bsgv], bias=vb_[:])
            ph.tt("dve", dif[:], vf[j][:], pr[2][:], ALU.subtract, [bvf[j], bpr[2]], [bdif])
            ph.tt("pool", dif[:], dif[:], sgv[:], ALU.mult, [bdif, bsgv], [bdif])
            ph.tt("dve", ob[j][:, 2, :], dif[:], pr[2][:], ALU.add, [bdif, bpr[2]], [bob[j]])
        ph.act(sgx[:], pr[4][:], AF.Sigmoid, [bpr[4]], [bsgx])
        ph.mm(pg[:], gup[:], sgx[:], True, True, [bgu, bsgx], [bpg])
        ph.cp("act", ob[j][:, 4, :], pg[:], [bpg], [bob[j]])
        ph.dma("pool", rw["tok"][:, :, tsl], ob[j][:], [bob[j]], [], final=True, key="st")
    ph.close()


QN = 7


def _rwkv_derive(nc, d, rw):
    ph = Ph(nc)
    pp = ph.sb([128, 16], F32)
    bpp = Buf()
    ph.dma("sp", pp[:], d["rw_pp"], [], [bpp])
    bones = ph.sb([128, 128], F32)
    bbo = Buf()
    ph.dma("sp", bones[:], d["blockones"], [], [bbo])
    lo_f = ph.sb([128, 4, 128], F32)
    lo = ph.sb([128, 4, 128], BF16)
    blo = Buf()
    for i in range(4):
        ph.dma("sp", lo_f[:, i, :], d["lora"][i], [], [blo])
    ph.cp("dve", lo[:], lo_f[:], [blo], [blo])
    cmask = ph.sb([128, 512], F32)
    bcm = Buf()
    ph.memset("pool", cmask[:], 1.0, [bcm])
    ph.memset("pool", cmask[:].rearrange("p (c i) -> p c i", i=128)[:, :, 0:1], 0.0, [bcm])
    tiny = ph.sb([128, 1], F32)
    ph.memset("pool", tiny[:], 0.0, [bcm])
    tin = [ph.sb([128, 4, 512], BF16) for _ in range(2)]
    btin = [Buf() for _ in range(2)]
    th = ph.sb([128, 512], BF16)
    bth = Buf()
    pL = [ph.ps([128, 512], F32) for _ in range(4)]
    bpL = [Buf() for _ in range(4)]
    pN = ph.ps([128, 512], F32)
    pB = ph.ps([128, 512], F32)
    bpN, bpB = Buf(), Buf()
    sig = [ph.sb([128, 512], F32) for _ in range(2)]
    aa = [ph.sb([128, 512], F32) for _ in range(2)]
    bsig = [Buf() for _ in range(2)]
    baa = [Buf() for _ in range(2)]
    kk = ph.sb([128, 512], F32)
    sq = ph.sb([128, 512], F32)
    rs = ph.sb([128, 512], F32)
    kkn = ph.sb([128, 512], F32)
    rk = ph.sb([128, 512], F32)
    kd = [ph.sb([128, 512], F32) for _ in range(2)]
    tmp = ph.sb([128, 512], F32)
    ksum = ph.sb([128, 512], F32)
    bon = [ph.sb([128, 512], BF16) for _ in range(2)]
    bkk, bsq, brs, bkkn, brk, btmp, bks = [Buf() for _ in range(7)]
    bkd = [Buf() for _ in range(2)]
    bbon = [Buf() for _ in range(2)]
    cs = ph.sb([128, 512], F32)
    csx = ph.sb([128, 512], F32)
    dln = ph.sb([128, 512], F32)
    eL = ph.sb([128, 512], F32)
    eLm = ph.sb([128, 512], F32)
    eLex = ph.sb([128, 512], F32)
    eLC = ph.sb([128, 512], F32)
    ka = ph.sb([128, 512], F32)
    bcs, bcsx, bdln, beL, beLm, beLex, beLC, bka = [Buf() for _ in range(8)]
    outq = [ph.sb([128, QN, 512], BF16) for _ in range(2)]
    bout = [Buf() for _ in range(2)]
    wc = [ph.sb([128, 4], F32) for _ in range(2)]
    bwc = [Buf() for _ in range(2)]
    oi = 0
    for tb in range(NTB):
        j = tb % 2
        tsl = slice(tb * 512, (tb + 1) * 512)
        ti = tin[j]
        ph.dma("sp", ti[:], rw["tok"][:, 0:4, tsl], [], [btin[j]], key="ld")
        rb, kb, vb, xb = ti[:, 0, :], ti[:, 1, :], ti[:, 2, :], ti[:, 3, :]
        ph.act(th[:], xb, AF.Tanh, [btin[j]], [bth])
        for dd in range(2):
            ph.mm(pL[dd][:], lo[:, dd, :], th[:], True, True, [blo, bth], [bpL[dd]])
            ph.mm(pL[2 + dd][:], lo[:, 2 + dd, :], xb, True, True, [blo, btin[j]], [bpL[2 + dd]])
        for dd in range(2):
            ph.act(sig[dd][:], pL[dd][:], AF.Sigmoid, [bpL[dd], bpp], [bsig[dd]], bias=pp[:, 5 + dd:6 + dd])
            ph.act(aa[dd][:], pL[2 + dd][:], AF.Sigmoid, [bpL[2 + dd], bpp], [baa[dd]], bias=pp[:, 7 + dd:8 + dd])
        ph.ts("dve", kk[:], kb, pp[:, 0:1], None, ALU.mult, None, [btin[j], bpp], [bkk])
        ph.tt("pool", sq[:], kk[:], kk[:], ALU.mult, [bkk], [bsq])
        ph.mm(pN[:], bones[:], sq[:], True, True, [bbo, bsq], [bpN])
        ph.ts("dve", rs[:], pN[:], 1e-24, None, ALU.max, None, [bpN], [brs])
        ph.rsqrt(rs[:], rs[:], [brs, bcm], [brs], bias=tiny[:])
        ph.tt("dve", kkn[:], kk[:], rs[:], ALU.mult, [bkk, brs], [bkkn])
        ph.ts("pool", rk[:], rb, pp[:, 2:3], None, ALU.mult, None, [btin[j], bpp], [brk])
        for dd in range(2):
            ph.ts("dve", tmp[:], aa[dd][:], -1.0, pp[:, 1:2], ALU.add, ALU.mult, [baa[dd], bpp], [btmp])
            ph.stt("dve", kd[dd][:], tmp[:], 1.0, kb, ALU.add, ALU.mult, [btmp, btin[j]], [bkd[dd]])
        ph.tt("pool", ksum[:], kd[0][:], kd[1][:], ALU.add, [bkd[0], bkd[1]], [bks])
        ph.tt("pool", ksum[:], ksum[:], rk[:], ALU.mult, [bks, brk], [bks])
        ph.mm(pB[:], bones[:], ksum[:], True, True, [bbo, bks], [bpB])
        ph.tt("dve", bon[j][:], pB[:], vb, ALU.mult, [bpB, btin[j]], [bbon[j]])
        ph.dma("pool", rw["bonus"][:, tsl], bon[j][:], [bbon[j]], [], final=True, key="st")
        for dd in range(2):
            def rv(ap, dd=dd):
                return ap if dd == 0 else ap[:, ::-1]
            obk = tb if dd == 0 else NTB - 1 - tb
            osl = slice(obk * 512, (obk + 1) * 512)
            oq = outq[oi % 2]
            boq = bout[oi % 2]
            wct = wc[oi % 2]
            bwct = bwc[oi % 2]
            oi += 1
            ph.scan(cs[:], cmask[:], rv(sig[dd][:]), [bcm, bsig[dd]], [bcs])
            ph.tt("pool", csx[:], cs[:], rv(sig[dd][:]), ALU.subtract, [bcs, bsig[dd]], [bcsx])
            csv = cs[:].rearrange("p (c i) -> p c i", i=128)
            ph.tt("pool", dln[:].rearrange("p (c i) -> p c i", i=128), csv, csv[:, :, 127:128].to_broadcast([128, 4, 128]), ALU.subtract, [bcs], [bdln])
            ph.act(eL[:], cs[:], AF.Exp, [bcs], [beL], scale=-WDS)
            ph.act(eLm[:], cs[:], AF.Exp, [bcs], [beLm], scale=WDS)
            ph.act(eLex[:], csx[:], AF.Exp, [bcsx], [beLex], scale=-WDS)
            ph.act(eLC[:], dln[:], AF.Exp, [bdln], [beLC], scale=WDS)
            ph.act(wct[:], csv[:, :, 127], AF.Exp, [bcs], [bwct], scale=-WDS)
            ph.tt("pool", ka[:], kkn[:], aa[dd][:], ALU.mult, [bkkn, baa[dd]], [bka])
            ph.stt("dve", oq[:, 0, :], rv(kkn[:]), -1.0, eLex[:], ALU.mult, ALU.mult, [bkkn, beLex], [boq])
            ph.tt("dve", oq[:, 1, :], rv(ka[:]), eLm[:], ALU.mult, [bka, beLm], [boq])
            ph.tt("pool", oq[:, 2, :], rv(kd[dd][:]), eLm[:], ALU.mult, [bkd[dd], beLm], [boq])
            ph.tt("dve", oq[:, 3, :], rv(rb), eL[:], ALU.mult, [btin[j], beL], [boq])
            ph.tt("pool", oq[:, 4, :], rv(ka[:]), eLC[:], ALU.mult, [bka, beLC], [boq])
            ph.tt("dve", oq[:, 5, :], rv(kd[dd][:]), eLC[:], ALU.mult, [bkd[dd], beLC], [boq])
            ph.cp("pool", oq[:, 6, :], rv(vb), [btin[j]], [boq])
            ph.dma("pool", rw["sc"][dd][:, :, osl], oq[:], [boq], [], final=True, key="st")
            ph.dma("pool", rw["wc"][dd][:, obk * 4:(obk + 1) * 4], wct[:], [bwct], [], final=True, key="st")
    ph.close()


def _rwkv_scan(nc, d, rw):
    ph = Ph(nc)
    NCH = S // 128
    X = []
    bX = []
    for dd in range(2):
        x_ = ph.sb([128, QN, S], BF16, name=f"X{dd}")
        b_ = [Buf() for _ in range(QN)]
        for q in range(QN):
            ph.dma("sp" if q % 2 == 0 else "act", x_[:, q, :], rw["sc"][dd][:, q, :], [], [b_[q]])
        X.append(x_)
        bX.append(b_)
    WC = ph.sb([128, 2, NCH], F32)
    bWC = Buf()
    for dd in range(2):
        ph.dma("sp", WC[:, dd, :], rw["wc"][dd], [], [bWC])
    mk = ph.sb([128, 3, 4, 128], F32)
    bmk = Buf()
    for i in range(3):
        for r_ in range(4):
            ph.dma("sp", mk[:, i, r_, :], d["masks"][i], [], [bmk])
    idf = ph.sb([128, 128], F32)
    ident = ph.sb([128, 128], BF16)
    id64 = ph.sb([128, 64], F32)
    bid = Buf()
    ph.dma("sp", idf[:], d["ident"], [], [bid])
    ph.dma("sp", id64[:], d["ident64x2"], [], [bid])
    ph.cp("dve", ident[:], idf[:], [bid], [bid])
    import os
    B = [ph.ps([128, 4, 128], F32, name=f"B{i}") for i in range(int(os.environ.get("SCAN_NB", 7)))]
    B = (B * 7)[:7]
    bB = [Buf() for _ in range(7)]
    BT = ph.ps([128, 4, 2, 128], BF16, name="BT")
    bBT = Buf()
    Nn = [ph.sb([128, 4, 128], BF16) for _ in range(2)]
    NT = [ph.sb([128, 4, 128], BF16) for _ in range(2)]
    bNn = [Buf() for _ in range(2)]
    bNT = [Buf() for _ in range(2)]
    Mak = ph.sb([128, 4, 128], BF16)
    Mbr = ph.sb([128, 4, 128], BF16)
    Mkr = ph.sb([128, 4, 128], BF16)
    bMak, bMbr, bMkr = Buf(), Buf(), Buf()
    TTs = ph.sb([128, 4, 2, 128], BF16)
    bTT = Buf()
    Z = [ph.sb([128, 4, 128], BF16) for _ in range(2)]
    bZ = [Buf() for _ in range(2)]
    G = ph.sb([128, 2, 128], BF16)
    Phi = ph.sb([128, 2, 64], BF16)
    bG, bPhi = Buf(), Buf()
    ST = [ph.sb([128, 2, 64], BF16) for _ in range(2)]
    bST = [Buf() for _ in range(2)]
    Yo = [ph.sb([128, 2, 128], F32) for _ in range(2)]
    bYo = [Buf() for _ in range(2)]
    qsel = (0, 4, 5, 6)
    import os
    DBG_N = int(os.environ.get('SCAN_NCH', NCH))
    DBG_S = int(os.environ.get('SCAN_STAGE', 9))
    for c in range(DBG_N):
        cs_ = slice(c * 128, (c + 1) * 128)
        for h in range(2):
            for dd in range(2):
                i = dd * 2 + h
                hs = slice(h * 64, (h + 1) * 64)
                At, Bt, Kt, Rt = (X[dd][hs, q, cs_] for q in range(4))
                if os.environ.get("SCAN_H") is not None and h != int(os.environ["SCAN_H"]):
                    continue
                ph.mm(B[0][:, i, :], Bt, At, True, True, bX[dd][0:4], [bB[0]])
                ph.mm(B[1][:, i, :], At, Bt, True, True, bX[dd][0:4], [bB[1]])
                ph.mm(B[2][:, i, :], Kt, At, True, True, bX[dd][0:4], [bB[2]])
                ph.mm(B[3][:, i, :], Bt, Rt, True, True, bX[dd][0:4], [bB[3]])
                ph.mm(B[4][:, i, :], Kt, Rt, True, True, bX[dd][0:4], [bB[4]])
        if os.environ.get("SCAN_NOMASK"):
            continue
        ph.tt("dve", Nn[0][:], B[0][:], mk[:, 0], ALU.mult, [bB[0], bmk], [bNn[0]])
        ph.tt("dve", NT[0][:], B[1][:], mk[:, 1], ALU.mult, [bB[1], bmk], [bNT[0]])
        ph.tt("dve", Mak[:], B[2][:], mk[:, 0], ALU.mult, [bB[2], bmk], [bMak])
        ph.tt("dve", Mbr[:], B[3][:], mk[:, 2], ALU.mult, [bB[3], bmk], [bMbr])
        ph.tt("dve", Mkr[:], B[4][:], mk[:, 2], ALU.mult, [bB[4], bmk], [bMkr])
        if DBG_S < 2:
            continue
        for qi, q in enumerate(qsel):
            for dd in range(2):
                ph.tr(BT[:, qi, dd, :], X[dd][:, q, cs_], ident[:], [bX[dd][q], bid], [bBT])
        ph.cp("act", TTs[:], BT[:], [bBT], [bTT])
        for dd in range(2):
            for h in range(2):
                i = dd * 2 + h
                ph.mm(B[5][:, i, 0:64], Mak[:, i, :], TTs[:, 3, dd, h * 64:(h + 1) * 64], True, True, [bMak, bTT], [bB[5]])
        ph.cp("pool", Z[0][:, :, 0:64], TTs[:, 0].rearrange("p d (h k) -> p (d h) k", h=2), [bTT], [bZ[0]])
        ph.cp("act", Z[0][:, :, 64:128], B[5][:, :, 0:64], [bB[5]], [bZ[0]])
        if DBG_S < 3:
            continue
        zi = 0
        ni = 0
        for lvl in range(7):
            for i in range(4):
                ph.mm(B[0][:, i, :], Nn[ni][:, i, :], Z[zi][:, i, :], True, True, [bNn[ni], bZ[zi]], [bB[0]])
            if lvl < 6:
                for i in range(4):
                    ph.mm(B[1][:, i, :], NT[ni][:, i, :], Nn[ni][:, i, :], True, True, [bNn[ni], bNT[ni]], [bB[1]])
                    ph.mm(B[2][:, i, :], Nn[ni][:, i, :], NT[ni][:, i, :], True, True, [bNn[ni], bNT[ni]], [bB[2]])
            ph.tt("dve", Z[1 - zi][:], B[0][:], Z[zi][:], ALU.add, [bB[0], bZ[zi]], [bZ[1 - zi]])
            zi = 1 - zi
            if lvl < 6:
                ph.cp("act", Nn[1 - ni][:], B[1][:], [bB[1]], [bNn[1 - ni]])
                ph.cp("act", NT[1 - ni][:], B[2][:], [bB[2]], [bNT[1 - ni]])
                ni = 1 - ni
        Zf = Z[zi]
        bZf = bZ[zi]
        if DBG_S < 4:
            continue
        so = c % 2
        for dd in range(2):
            for h in range(2):
                i = dd * 2 + h
                hc = slice(h * 64, (h + 1) * 64)
                ph.mm(B[3][0:64, i, :], Zf[:, i, 0:64], Mbr[:, i, :], True, True, [bZf, bMbr], [bB[3]])
                ph.mm(B[4][0:64, i, 0:64], Zf[:, i, 0:64], TTs[:, 1, dd, hc], True, True, [bZf, bTT], [bB[4]])
        for dd in range(2):
            for h in range(2):
                i = dd * 2 + h
                hs = slice(h * 64, (h + 1) * 64)
                ph.tt("dve", G[hs, dd, :], B[3][0:64, i, :], X[dd][hs, 3, cs_], ALU.add, [bB[3], bX[dd][3]], [bG])
                ph.stt("dve", Phi[hs, dd, :], id64[hs, :], WC[hs, dd, c:c + 1], B[4][0:64, i, 0:64], ALU.mult, ALU.add, [bid, bWC, bB[4]], [bPhi])
        for dd in range(2):
            for h in range(2):
                i = dd * 2 + h
                hs = slice(h * 64, (h + 1) * 64)
                hc = hs
                last = (c == 0)
                ph.mm(B[5][0:64, i, 0:64], TTs[:, 1, dd, hc], Zf[:, i, 64:128], True, False, [bTT, bZf], [bB[5]])
                ph.mm(B[5][0:64, i, 0:64], TTs[:, 2, dd, hc], TTs[:, 3, dd, hc], False, last, [bTT], [bB[5]])
                if not last:
                    ph.mm(B[5][0:64, i, 0:64], Phi[hs, dd, :], ST[so][hs, dd, :], False, True, [bPhi, bST[so]], [bB[5]])
                ph.mm(B[6][0:64, i, :], Zf[:, i, 64:128], Mbr[:, i, :], True, False, [bZf, bMbr], [bB[6]])
                ph.mm(B[6][0:64, i, :], TTs[:, 3, dd, hc], Mkr[:, i, :], False, last, [bTT, bMkr], [bB[6]])
                if not last:
                    ph.mm(B[6][0:64, i, :], ST[so][hs, dd, :], G[hs, dd, :], False, True, [bST[so], bG], [bB[6]])
        yo = Yo[c % 2]
        for dd in range(2):
            for h in range(2):
                i = dd * 2 + h
                hs = slice(h * 64, (h + 1) * 64)
                ph.cp("act", ST[1 - so][hs, dd, :], B[5][0:64, i, 0:64], [bB[5]], [bST[1 - so]])
                ph.cp("dve" if h else "act", yo[hs, dd, :], B[6][0:64, i, :], [bB[6]], [bYo[c % 2]])
        for dd in range(2):
            ph.dma("pool", rw["y"][dd][:, cs_], yo[:, dd, :], [bYo[c % 2]], [], final=True, key="st")
    ph.close()


def _rwkv_final(nc, d, rw, yT):
    ph = Ph(nc)
    pp = ph.sb([128, 16], F32)
    bpp = Buf()
    ph.dma("sp", pp[:], d["rw_pp"], [], [bpp])
    bones = ph.sb([128, 128], F32)
    bbo = Buf()
    ph.dma("sp", bones[:], d["blockones"], [], [bbo])
    geps = ph.sb([128, 1], F32)
    ph.memset("pool", geps[:], GN_EPS, [bbo])
    y0 = [ph.sb([128, 512], F32) for _ in range(2)]
    y1 = [ph.sb([128, 512], F32) for _ in range(2)]
    bg = [ph.sb([128, 2, 512], BF16) for _ in range(2)]
    by = [Buf() for _ in range(2)]
    bbg = [Buf() for _ in range(2)]
    ysum = ph.sb([128, 512], F32)
    yc = ph.sb([128, 512], F32)
    sq = ph.sb([128, 512], F32)
    rstd = ph.sb([128, 512], F32)
    bys, byc, bsq, brs = Buf(), Buf(), Buf(), Buf()
    pM = ph.ps([128, 512], F32)
    pV = ph.ps([128, 512], F32)
    bpM, bpV = Buf(), Buf()
    ob = [ph.sb([128, 512], BF16) for _ in range(2)]
    bob = [Buf() for _ in range(2)]
    for tb in range(NTB):
        j = tb % 2
        tsl = slice(tb * 512, (tb + 1) * 512)
        rsl = slice((NTB - 1 - tb) * 512, (NTB - tb) * 512)
        ph.dma("sp", y0[j][:], rw["y"][0][:, tsl], [], [by[j]], key="ld")
        ph.dma("sp", y1[j][:], rw["y"][1][:, rsl], [], [by[j]], key="ld")
        ph.dma("sp", bg[j][:, 0, :], rw["bonus"][:, tsl], [], [bbg[j]], key="ld2")
        ph.dma("sp", bg[j][:, 1, :], rw["tok"][:, 4, tsl], [], [bbg[j]], key="ld2")
        ph.tt("dve", ysum[:], y0[j][:], y1[j][:, ::-1], ALU.add, [by[j]], [bys])
        ph.mm(pM[:], bones[:], ysum[:], True, True, [bbo, bys], [bpM])
        ph.stt("dve", yc[:], pM[:], -1.0 / 64, ysum[:], ALU.mult, ALU.add, [bpM, bys], [byc])
        ph.tt("pool", sq[:], yc[:], yc[:], ALU.mult, [byc], [bsq])
        ph.mm(pV[:], bones[:], sq[:], True, True, [bbo, bsq], [bpV])
        ph.rsqrt(rstd[:], pV[:], [bpV, bbo], [brs], scale=1.0 / 64, bias=geps[:])
        ph.tt("dve", yc[:], yc[:], rstd[:], ALU.mult, [byc, brs], [byc])
        ph.ts("dve", yc[:], yc[:], pp[:, 3:4], pp[:, 4:5], ALU.mult, ALU.add, [byc, bpp], [byc])
        ph.tt("pool", yc[:], yc[:], bg[j][:, 0, :], ALU.add, [byc, bbg[j]], [byc])
        ph.tt("dve", ob[j][:], yc[:], bg[j][:, 1, :], ALU.mult, [byc, bbg[j]], [bob[j]])
        ph.dma("pool", yT[0:128, tsl], ob[j][:], [bob[j]], [], final=True, key="st")
    ph.close()


TPC = 2048
NT2 = TPC // 128
NB2 = TPC // 512
NFC = DFF // 128


def _outproj_norm(nc, x, yT, d, sc, moe, sel=None):
    ph = Ph(nc)
    wo_f = [ph.sb([128, D], F32) for _ in range(2)]
    bwf = [Buf() for _ in range(2)]
    wo = ph.sb([128, KC, D], BF16)
    bwo = Buf()
    wv = d["w_out"].rearrange("(kc p) n -> kc p n", p=128)
    for kc in range(KC):
        ph.dma("sp", wo_f[kc % 2][:], wv[kc], [], [bwf[kc % 2]], key="ldw")
        ph.cp("pool" if kc % 2 else "dve", wo[:, kc, :], wo_f[kc % 2][:], [bwf[kc % 2]], [bwo])
    ident = ph.sb([128, 128], BF16)
    identf = ph.sb([128, 128], F32)
    bid = Buf()
    ph.dma("sp", identf[:], d["ident"], [], [bid])
    ph.cp("dve", ident[:], identf[:], [bid], [bid])
    eps = ph.sb([128, 1], F32)
    ph.memset("pool", eps[:], 1e-6, [bid])
    if moe:
        rg = ph.sb([128, NE, D], F32)
        gb = ph.sb([128, D], F32)
        brg = Buf()
        ph.dma("sp", gb[:], d["gffn_row"].partition_broadcast(128), [], [brg])
        for e_ in range(NE):
            ph.dma("sp", rg[:, e_, :], d["routerT"][e_:e_ + 1, :].partition_broadcast(128), [], [brg])
        ph.tt("dve", rg[:], rg[:], gb[:].unsqueeze(1).to_broadcast([128, NE, D]), ALU.mult, [brg], [brg])
        lg = ph.sb([128, NE], F32)
        l2 = ph.sb([128, NE], F32)
        m1 = ph.sb([128, 1], F32)
        m2 = ph.sb([128, 1], F32)
        k1 = ph.sb([128, NE], F32)
        k2 = ph.sb([128, NE], F32)
        g1 = ph.sb([128, 1], F32)
        g2 = ph.sb([128, 1], F32)
        comb = [ph.sb([128, NE], F32) for _ in range(2)]
        bcomb = [Buf() for _ in range(2)]
        blg = Buf()
        junkf = ph.sb([128, D], F32)
        bjf = Buf()
    yt = [ph.sb([128, KC, 128], BF16) for _ in range(2)]
    byt = [Buf() for _ in range(2)]
    xt = [ph.sb([128, D], F32) for _ in range(2)]
    bxt = [Buf() for _ in range(2)]
    x1 = [ph.sb([128, D], F32) for _ in range(2)]
    bx1 = [Buf() for _ in range(2)]
    pO = [ph.ps([128, 2, 512], F32) for _ in range(2)]
    bpO = [Buf() for _ in range(2)]
    junk = ph.sb([128, D], BF16)
    bj = Buf()
    ss = [ph.sb([128, 1], F32) for _ in range(2)]
    bss = [Buf() for _ in range(2)]
    xn = [ph.sb([128, D], BF16) for _ in range(2)]
    bxn = [Buf() for _ in range(2)]
    xnf = ph.sb([128, D], F32)
    bxnf = Buf()
    pT = [ph.ps([128, KC, 128], BF16) for _ in range(2)]
    bpT = [Buf() for _ in range(2)]
    hs = [ph.sb([128, KC, 128], BF16) for _ in range(2)]
    bhs = [Buf() for _ in range(2)]
    yv = yT.rearrange("(kc p) t -> p kc t", p=128)
    xv = x.rearrange("(t p) d -> t p d", p=128)
    if sel is not None:
        msk = ph.sb([128, 2], F32)
        bmsk = Buf()
        ph.dma("sp", msk[:], sel, [], [bmsk])
        yt2 = [ph.sb([128, KC, 128], BF16) for _ in range(2)]
        byt2 = [Buf() for _ in range(2)]
        ysel = [ph.sb([128, KC, 128], BF16) for _ in range(2)]
        bysel = [Buf() for _ in range(2)]
    x1v = sc["x1"].rearrange("(t p) d -> t p d", p=128)
    for t in range(NT2):
        j = t % 2
        ph.dma("sp", yt[j][:], yv[:, :, t * 128:(t + 1) * 128], [], [byt[j]], key="ld")
        ph.dma("sp", xt[j][:], xv[t], [], [bxt[j]], key="ld2")
        ysrc, bysrc = yt[j], byt[j]
        if sel is not None:
            ph.dma("sp", yt2[j][:], yv[:, :, TPC + t * 128:TPC + (t + 1) * 128], [], [byt2[j]], key="ld")
            ph.ts("pool", ysel[j][:], yt[j][:], msk[:, 0:1], None, ALU.mult, None, [byt[j], bmsk], [bysel[j]])
            ph.stt("dve", ysel[j][:], yt2[j][:], msk[:, 1:2], ysel[j][:], ALU.mult, ALU.add, [byt2[j], bmsk, bysel[j]], [bysel[j]])
            ysrc, bysrc = ysel[j], bysel[j]
        for hf in range(2):
            for kc in range(KC):
                ph.mm(pO[j][:, hf, :], ysrc[:, kc, :], wo[:, kc, hf * 512:(hf + 1) * 512], kc == 0, kc == KC - 1, [bysrc, bwo], [bpO[j]])
        ph.tt("dve", x1[j][:], pO[j][:].rearrange("p a b -> p (a b)"), xt[j][:], ALU.add, [bpO[j], bxt[j]], [bx1[j]])
        ph.dma("pool", x1v[t], x1[j][:], [bx1[j]], [], final=True, key="st")
        ph.act(junk[:], x1[j][:], AF.Square, [bx1[j]], [bj, bss[j]], accum=ss[j][:])
        ph.rsqrt(ss[j][:], ss[j][:], [bss[j], bid], [bss[j]], scale=1.0 / D, bias=eps[:])
        ph.ts("dve", xn[j][:], x1[j][:], ss[j][:, 0:1], None, ALU.mult, None, [bx1[j], bss[j]], [bxn[j]])
        for kc in range(KC):
            ph.tr(pT[j][:, kc, :], xn[j][:, kc * 128:(kc + 1) * 128], ident[:], [bxn[j], bid], [bpT[j]])
        ph.cp("act", hs[j][:], pT[j][:], [bpT[j]], [bhs[j]])
        ph.dma("pool", sc["hT"][:, :, t * 128:(t + 1) * 128], hs[j][:], [bhs[j]], [], final=True, key="st")
        if moe:
            ph.ts("pool", xnf[:], x1[j][:], ss[j][:, 0:1], None, ALU.mult, None, [bx1[j], bss[j]], [bxnf])
            for e_ in range(NE):
                ph.P.add("dve", lambda e, e_=e_: e.scalar_tensor_tensor(out=junkf[:], in0=xnf[:], scalar=1.0, in1=rg[:, e_, :], op0=ALU.mult, op1=ALU.mult, accum_out=lg[:, e_:e_ + 1]), [bxnf, brg], [bjf, blg])
            ph.P.add("dve", lambda e: e.reduce_max(out=m1[:], in_=lg[:], axis=AX.X), [blg], [blg])
            ph.ts("dve", k1[:], lg[:], m1[:, 0:1], None, ALU.is_equal, None, [blg], [blg])
            ph.stt("dve", l2[:], k1[:], -1e30, lg[:], ALU.mult, ALU.add, [blg], [blg])
            ph.P.add("dve", lambda e: e.reduce_max(out=m2[:], in_=l2[:], axis=AX.X), [blg], [blg])
            ph.ts("dve", k2[:], l2[:], m2[:, 0:1], None, ALU.is_equal, None, [blg], [blg])
            ph.tt("dve", g2[:], m2[:], m1[:], ALU.subtract, [blg], [blg])
            ph.act(g2[:], g2[:], AF.Sigmoid, [blg], [blg])
            ph.ts("dve", g1[:], g2[:], -1.0, 1.0, ALU.mult, ALU.add, [blg], [blg])
            ph.ts("dve", k1[:], k1[:], g1[:, 0:1], None, ALU.mult, None, [blg], [blg])
            ph.stt("dve", comb[j][:], k2[:], g2[:, 0:1], k1[:], ALU.mult, ALU.add, [blg], [bcomb[j]])
            ph.dma("pool", sc["comb"][:, t, :], comb[j][:], [bcomb[j]], [], final=True, key="st")
    ph.close()


def _ffn_experts(nc, d, sc, n_exp, moe, final_gain, out):
    ph = Ph(nc)
    FG = 4
    NG = NFC // FG
    hT = ph.sb([128, KC, TPC], BF16)
    bh = [Buf() for _ in range(KC)]
    for kc in range(KC):
        ph.dma("sp", hT[:, kc, :], sc["hT"][:, kc, :], [], [bh[kc]])
    gm = ph.sb([128, KC], F32)
    bgm = Buf()
    ph.dma("sp", gm[:], d["gffn"], [], [bgm])
    acc = ph.sb([128, NT2, D], F32)
    bacc = [Buf() for _ in range(NT2)]
    x1v = sc["x1"].rearrange("(t p) d -> p t d", p=128)
    for t in range(NT2):
        ph.dma("sp", acc[:, t, :], x1v[:, t, :], [], [bacc[t]])
    if moe:
        comb = ph.sb([128, NT2, NE], F32)
        bcomb = Buf()
        ph.dma("sp", comb[:], sc["comb"], [], [bcomb])
    wgs = [ph.sb([128, KC, 2, 128], F32) for _ in range(2)]
    bwgs = [Buf() for _ in range(2)]
    wgb = [ph.sb([128, KC, 2, 128], BF16) for _ in range(2)]
    bwgb = [Buf() for _ in range(2)]
    wds = ph.sb([128, FG, D], F32)
    bwds = Buf()
    wdg = [ph.sb([128, FG, D], BF16) for _ in range(2)]
    bwdg = [Buf() for _ in range(2)]
    aTg = [ph.sb([128, FG, TPC], BF16) for _ in range(2)]
    baTg = [[Buf() for _ in range(FG)] for _ in range(2)]
    pG = [ph.ps([128, 512], F32) for _ in range(2)]
    pU = [ph.ps([128, 512], F32) for _ in range(2)]
    bpG = [Buf() for _ in range(2)]
    bpU = [Buf() for _ in range(2)]
    pD = [ph.ps([128, 2, 512], F32) for _ in range(2)]
    bpD = [Buf() for _ in range(2)]
    sg = [ph.sb([128, 512], F32) for _ in range(2)]
    bsg = [Buf() for _ in range(2)]
    it = 0
    gi = 0
    di = 0
    wi = 0
    for e_ in range(n_exp):
        wd_v = d["w_down"][e_].rearrange("(g f p) n -> g p f n", p=128, f=FG)
        for g in range(NG):
            gb_ = gi % 2
            gi += 1
            ph.dma("sp", wds[:], wd_v[g], [], [bwds])
            ph.cp("pool", wdg[gb_][:], wds[:], [bwds], [bwdg[gb_]])
            for f in range(FG):
                fc = g * FG + f
                j = wi % 2
                wi += 1
                ph.dma("sp", wgs[j][:], d["w_gu"][e_, fc], [], [bwgs[j]])
                ph.tt("pool", wgb[j][:].rearrange("p k a n -> p k (a n)"), wgs[j][:].rearrange("p k a n -> p k (a n)"),
                      gm[:].unsqueeze(2).to_broadcast([128, KC, 256]), ALU.mult, [bwgs[j], bgm], [bwgb[j]])
                for tb in range(NB2):
                    a = it % 2
                    it += 1
                    tsl = slice(tb * 512, (tb + 1) * 512)
                    for kc in range(KC):
                        ph.mm(pG[a][:], wgb[j][:, kc, 0, :], hT[:, kc, tsl], kc == 0, kc == KC - 1, [bwgb[j], bh[kc]], [bpG[a]])
                    for kc in range(KC):
                        ph.mm(pU[a][:], wgb[j][:, kc, 1, :], hT[:, kc, tsl], kc == 0, kc == KC - 1, [bwgb[j], bh[kc]], [bpU[a]])
                    ph.act(sg[a][:], pG[a][:], AF.Silu, [bpG[a]], [bsg[a]])
                    ph.tt("dve", aTg[gb_][:, f, tsl], pU[a][:], sg[a][:], ALU.mult, [bpU[a], bsg[a]], [baTg[gb_][f]])
            for t in range(NT2):
                k = di % 2
                di += 1
                for hf in range(2):
                    for f in range(FG):
                        ph.mm(pD[k][:, hf, :], aTg[gb_][:, f, t * 128:(t + 1) * 128], wdg[gb_][:, f, hf * 512:(hf + 1) * 512],
                              f == 0, f == FG - 1, [baTg[gb_][f], bwdg[gb_]], [bpD[k]])
                pflat = pD[k][:].rearrange("p a b -> p (a b)")
                if moe:
                    ph.stt("dve", acc[:, t, :], pflat, comb[:, t, e_:e_ + 1], acc[:, t, :], ALU.mult, ALU.add, [bpD[k], bcomb, bacc[t]], [bacc[t]])
                else:
                    ph.tt("dve", acc[:, t, :], pflat, acc[:, t, :], ALU.add, [bpD[k], bacc[t]], [bacc[t]])
    ov = out.rearrange("(t p) d -> t p d", p=128)
    if final_gain is None:
        for t in range(NT2):
            ph.dma("pool", ov[t], acc[:, t, :], [bacc[t]], [], final=True)
    else:
        gb = wds[:, 0, :]
        bgb = bwds
        ph.dma("sp", gb, final_gain.partition_broadcast(128), [], [bgb])
        eps = ph.sb([128, 1], F32)
        beps_ = Buf()
        ph.memset("pool", eps[:], 1e-6, [beps_])
        ss = ph.sb([128, NT2], F32)
        bss = Buf()
        junk = aTg[0][:, 0, 0:D]
        bj = baTg[0][0]
        for t in range(NT2):
            ph.act(junk, acc[:, t, :], AF.Square, [bacc[t]], [bj, bss], accum=ss[:, t:t + 1])
        ph.rsqrt(ss[:], ss[:], [bss, beps_], [bss], scale=1.0 / D, bias=eps[:])
        for t in range(NT2):
            ph.stt("dve", acc[:, t, :], acc[:, t, :], ss[:, t:t + 1], gb, ALU.mult, ALU.mult, [bacc[t], bss, bgb], [bacc[t]])
            ph.dma("pool", ov[t], acc[:, t, :], [bacc[t]], [], final=True)
    ph.close()


def _lam_init(layer):
    import math
    return 0.8 - 0.6 * math.exp(-0.3 * layer)


MIXER_INPUTS = {
    "x": ([S, D], F32), "gmix": ([128, KC], F32), "att_pp": ([128, 8], F32), "lam_q": ([1, 128], F32),
    "lam_k": ([1, 128], F32), "pos": ([1, S], I32), "w_att": ([D, 768], F32), "w_att_sw": ([D, 512], F32),
    "pool_pp": ([128, 72], F32), "w_pool": ([D, 128], F32), "pool_mix_bd": ([128, 128], F32),
    "mu": ([2, 640], F32), "w_rwkv": ([D, 640], F32), "gate_up": ([128, 128], F32),
    "rw_pp": ([128, 16], F32), "blockones": ([128, 128], F32), "lora": ([4, 128, 128], F32),
    "masks": ([3, 128, 128], F32), "ident": ([128, 128], F32), "ident64x2": ([128, 64], F32),
}
MIXER_INPUTS_L1 = {"w_vdown": ([D, 32], F32), "vres_up": ([32, 128], F32), "vres_bias": ([128, 1], F32),
                   "vfirst_in": ([128, S], F32)}


def _mixer_body(nc, layer, d, x, yT, vfirst, stages=None, rowmap=None, hT_src=None):
    def scr(name, shape, dt):
        return nc.dram_tensor(f"scr{layer}_{name}", shape, dt, kind="Internal").ap()
    hT_dram = scr("hT", [128, KC, S], BF16)
    rw = {"tok": scr("tok", [128, 5, S], BF16), "bonus": scr("bonus", [128, S], BF16),
          "sc": [scr(f"sc{i}", [128, QN, S], BF16) for i in range(2)],
          "wc": [scr(f"wc{i}", [128, S // 128], F32) for i in range(2)],
          "y": [scr(f"y{i}", [128, S], F32) for i in range(2)], "vfirst": vfirst}
    if hT_src is not None:
        hT_dram = hT_src
    else:
        _norm_to_hT(nc, x, None, hT_dram, rowmap)
    if stages is None or "att" in stages:
        _attention(nc, hT_dram, d, _lam_init(layer), yT)
    if stages is None or "pool" in stages:
        _pool_mixer(nc, hT_dram, d, yT)
    if stages is None or "rwkv" in stages or "rwkv1" in stages:
        _rwkv_proj(nc, hT_dram, d, layer, rw)
    if stages is None or "rwkv" in stages or "rwkv2" in stages:
        _rwkv_derive(nc, d, rw)
    if stages is None or "rwkv" in stages or "rwkv3" in stages:
        _rwkv_scan(nc, d, rw)
    if stages is None or "rwkv" in stages or "rwkv4" in stages:
        _rwkv_final(nc, d, rw, yT)
    return rw


def build_mixer(layer, stages=None):
    nc = bass.Bass("TRN2", target_bir_lowering=False)
    d = {}
    spec = dict(MIXER_INPUTS)
    if layer > 0:
        spec.update(MIXER_INPUTS_L1)
    for k, (shp, dt) in spec.items():
        d[k] = nc.dram_tensor(k, shp, dt, kind="ExternalInput").ap()
    yT = nc.dram_tensor("yT", [512, S], BF16, kind="ExternalOutput").ap()
    if layer == 0:
        vfirst = nc.dram_tensor("vfirst", [128, S], F32, kind="ExternalOutput").ap()
    else:
        vfirst = d["vfirst_in"]
    _mixer_body(nc, layer, d, d["x"], yT, vfirst, stages)
    return nc


def build_ffn(layer):
    moe = (layer % 2 == 1)
    last = (layer == 1)
    n_exp = NE if moe else 1
    nc = bass.Bass("TRN2", target_bir_lowering=False)
    d = {}
    spec = {"x": ([TPC, D], F32), "yT": ([D, TPC], BF16), "w_out": ([D, D], F32), "ident": ([128, 128], F32),
            "gffn": ([128, KC], F32), "w_gu": ([n_exp, NFC, 128, KC, 2, 128], F32),
            "w_down": ([n_exp, DFF, D], F32)}
    if moe:
        spec.update({"gffn_row": ([1, D], F32), "routerT": ([NE, D], F32)})
    if last:
        spec["gout_row"] = ([1, D], F32)
    for k, (shp, dt) in spec.items():
        d[k] = nc.dram_tensor(k, shp, dt, kind="ExternalInput").ap()
    out = nc.dram_tensor("out", [TPC, D], F32, kind="ExternalOutput").ap()
    sc = {"x1": nc.dram_tensor("s_x1", [TPC, D], F32, kind="Internal").ap(),
          "hT": nc.dram_tensor("s_hT", [128, KC, TPC], BF16, kind="Internal").ap(),
          "aT": nc.dram_tensor("s_aT", [128, NFC, TPC], BF16, kind="Internal").ap()}
    if moe:
        sc["comb"] = nc.dram_tensor("s_comb", [128, NT2, NE], F32, kind="Internal").ap()
    _outproj_norm(nc, d["x"], d["yT"], d, sc, moe)
    _ffn_experts(nc, d, sc, n_exp, moe, d["gout_row"] if last else None, out)
    return nc


def _consts():
    c = {}
    c["blockones"] = np.kron(np.eye(2, dtype=np.float32), np.ones((64, 64), np.float32))
    c["masks"] = np.stack([np.triu(np.ones((128, 128), np.float32), 1), np.tril(np.ones((128, 128), np.float32), -1),
                           np.triu(np.ones((128, 128), np.float32), 0)])
    c["ident"] = np.eye(128, dtype=np.float32)
    c["ident64x2"] = np.concatenate([np.eye(64, dtype=np.float32)] * 2, axis=0)
    return c


def _colmajor(v):
    return np.ascontiguousarray(v.reshape(KC, 128).T)


def _mixer_inputs(layer, b, hh, inp, x_b, consts, vfirst=None):
    f32 = np.float32
    l = layer
    w_in = inp["w_in_first"] if l == 0 else inp["w_in_rest"][l - 1]
    o_q = 1024 if l == 0 else 1056
    o_k, o_v, o_p = o_q + 512, o_q + 1024, o_q + 1536
    hs = [2 * hh, 2 * hh + 1]
    m = dict(consts)
    m["x"] = x_b
    m["gmix"] = _colmajor(inp["norm_mix"][l])
    m["pos"] = np.ascontiguousarray(inp["positions"][b:b + 1].astype(np.int32))
    qc = np.concatenate([np.arange(o_q + h * 128, o_q + (h + 1) * 128) for h in hs])
    kc = np.concatenate([np.arange(o_k + h * 128, o_k + (h + 1) * 128) for h in hs])
    vc = np.concatenate([np.arange(o_v + h * 128, o_v + (h + 1) * 128) for h in hs])
    m["w_att"] = np.ascontiguousarray(w_in[:, np.concatenate([qc, kc, vc])])
    perm = np.arange(64)
    perm[0:8] = np.arange(8, 16)
    perm[8:16] = np.arange(0, 8)
    perm512 = np.concatenate([blk * 64 + perm for blk in range(8)])
    m["w_att_sw"] = np.ascontiguousarray(w_in[:, np.concatenate([qc, kc])][:, perm512])
    half = 8
    inv_freq = np.power(f32(500000.0), -(np.arange(half, dtype=f32) * f32(2.0) / f32(16)))
    pp = np.zeros((128, 8), f32)
    for p in range(128):
        dloc = p % 64
        if dloc < 16:
            pp[p, 0] = inv_freq[dloc % 8]
            pp[p, 1] = -1.0 if dloc < 8 else 1.0
    pp[:, 2] = inp["subln_w"][l]
    m["att_pp"] = pp
    m["lam_q"] = np.ascontiguousarray(inp["lambda_q"][l].reshape(1, 128))
    m["lam_k"] = np.ascontiguousarray(inp["lambda_k"][l].reshape(1, 128))
    gs = hs
    m["w_pool"] = np.ascontiguousarray(w_in[:, o_p + gs[0] * 64:o_p + (gs[1] + 1) * 64])
    pmb = np.zeros((128, 128), f32)
    for i, g in enumerate(gs):
        pmb[i * 64:(i + 1) * 64, i * 64:(i + 1) * 64] = inp["pool_mix"][l, g]
    m["pool_mix_bd"] = pmb
    ppp = np.zeros((128, 72), f32)
    wins = (2, 4, 8, 16)
    for i, g in enumerate(gs):
        rows = slice(i * 64, (i + 1) * 64)
        w = wins[g]
        ppp[rows, g] = 1.0 / w
        for half_ in range(2):
            for cidx in range(8):
                t = cidx if half_ == 0 else S - 8 + cidx
                lo = min(max(t - w // 2, 0), S)
                hi = min(max(t + (w - w // 2), 0), S)
                ppp[rows, 8 + g * 16 + half_ * 8 + cidx] = 1.0 / (hi - lo)
    ppp[:, 4] = inp["pool_scale"][l, gs[0] * 64:(gs[1] + 1) * 64]
    m["pool_pp"] = ppp
    rc = np.arange(hh * 128, (hh + 1) * 128)
    cols = np.concatenate([rc, 256 + rc, 512 + rc, np.arange(768, 896), np.arange(896, 1024)])
    m["w_rwkv"] = np.ascontiguousarray(w_in[:, cols])
    m["mu"] = np.ascontiguousarray(inp["tshift"][l][:, cols])
    m["gate_up"] = np.ascontiguousarray(inp["gate_up"][l][:, rc])
    rp = np.zeros((128, 16), f32)
    rp[:, 0] = inp["k_k"][l, rc]
    rp[:, 1] = inp["k_a"][l, rc]
    rp[:, 2] = inp["r_k"][l].reshape(256)[rc]
    rp[:, 3] = inp["lnx_w"][l, rc]
    rp[:, 4] = inp["lnx_b"][l, rc]
    rp[:, 5] = inp["decay_bias"][l, 0, rc]
    rp[:, 6] = inp["decay_bias"][l, 1, rc]
    rp[:, 7] = inp["iclr_bias"][l, 0, rc]
    rp[:, 8] = inp["iclr_bias"][l, 1, rc]
    m["rw_pp"] = rp
    lora = np.zeros((4, 128, 128), f32)
    for dd in range(2):
        lora[dd, 0:64, :] = inp["decay_up"][l, dd][:, rc]
        lora[2 + dd, 64:128, :] = inp["iclr_up"][l, dd][:, rc]
    m["lora"] = lora
    if l > 0:
        m["w_vdown"] = np.ascontiguousarray(w_in[:, 1024:1056])
        m["vres_up"] = np.ascontiguousarray(inp["vres_up"][l - 1][:, rc])
        m["vres_bias"] = np.ascontiguousarray(inp["vres_bias"][l - 1][rc].reshape(128, 1))
        m["vfirst_in"] = vfirst
    return m


def _wout_rows(gathered=False):
    per = []
    for hh in range(2):
        rows = list(range(hh * 128, (hh + 1) * 128))
        rows += list(range(256 + hh * 256, 256 + (hh + 1) * 256))
        rows += list(range(768 + hh * 128, 768 + (hh + 1) * 128))
        per.append(rows)
    if not gathered:
        return np.array(per[0] + per[1])
    out = []
    for k in range(2):
        for r in range(2):
            out += per[r][k * 256:(k + 1) * 256]
    return np.array(out)


def _ffn_inputs(layer, inp, x_tok, yT_tok, consts, gathered=False):
    l = layer
    m = {"x": x_tok, "yT": yT_tok, "ident": consts["ident"]}
    m["w_out"] = np.ascontiguousarray(inp["w_out"][l][_wout_rows(gathered)])
    m["gffn"] = _colmajor(inp["norm_ffn"][l])
    def gu(wg, wu):
        E = wg.shape[0]
        st = np.stack([wg.reshape(E, KC, 128, NFC, 128), wu.reshape(E, KC, 128, NFC, 128)], axis=0)
        return np.ascontiguousarray(st.transpose(1, 4, 3, 2, 0, 5))
    if l % 2 == 0:
        i = l // 2
        m["w_gu"] = gu(inp["ffn_gate"][i:i + 1], inp["ffn_up"][i:i + 1])
        m["w_down"] = inp["ffn_down"][i:i + 1]
    else:
        i = l // 2
        m["w_gu"] = gu(inp["exp_gate"][i], inp["exp_up"][i])
        m["w_down"] = inp["exp_down"][i]
        m["gffn_row"] = np.ascontiguousarray(inp["norm_ffn"][l].reshape(1, D))
        m["routerT"] = np.ascontiguousarray(inp["router"][i].T)
    if l == 1:
        m["gout_row"] = np.ascontiguousarray(inp["norm_out"].reshape(1, D))
    return m


GROUPS = [[0, 1], [2, 3], [4, 5], [6, 7]]


def _ffn_spec(layer):
    moe = (layer % 2 == 1)
    n_exp = NE if moe else 1
    spec = {"w_out": ([D, D], F32), "ident": ([128, 128], F32), "gffn": ([128, KC], F32),
            "w_gu": ([n_exp, NFC, 128, KC, 2, 128], F32), "w_down": ([n_exp, DFF, D], F32)}
    if moe:
        spec.update({"gffn_row": ([1, D], F32), "routerT": ([NE, D], F32)})
    if layer == 1:
        spec["gout_row"] = ([1, D], F32)
    return spec


def build_fused():
    nc = bass.Bass("TRN2", target_bir_lowering=False)

    def inp(name, shp, dt):
        return nc.dram_tensor(name, shp, dt, kind="ExternalInput").ap()
    x_full = inp("x_full", [S, D], F32)
    x_tok = inp("x_tok", [TPC, D], F32)
    sel = inp("sel", [128, 2], F32)
    out = nc.dram_tensor("out", [TPC, D], F32, kind="ExternalOutput").ap()
    vfirst = nc.dram_tensor("vfirst_s", [128, S], F32, kind="Internal").ap()
    cur_full, cur_tok = x_full, x_tok
    cur_map = None
    cur_hT = None
    for l in range(2):
        spec = dict(MIXER_INPUTS)
        del spec["x"]
        if l > 0:
            spec.update({k: v for k, v in MIXER_INPUTS_L1.items() if k != "vfirst_in"})
        d = {k: inp(f"m{l}_{k}", shp, dt) for k, (shp, dt) in spec.items()}
        yT_t = nc.dram_tensor(f"yT_{l}", [512, S], BF16)
        yTall_t = nc.dram_tensor(f"yTall_{l}", [1024, S], BF16)
        _mixer_body(nc, l, d, cur_full, yT_t.ap(), vfirst, rowmap=cur_map, hT_src=cur_hT)
        ph = Ph(nc)
        ph.allgather(yTall_t, yT_t, GROUPS, 256)
        ph.close()
        moe = (l % 2 == 1)
        fd = {k: inp(f"f{l}_{k}", shp, dt) for k, (shp, dt) in _ffn_spec(l).items()}
        sc = {"x1": nc.dram_tensor(f"s{l}_x1", [TPC, D], F32, kind="Internal").ap(),
              "hT": nc.dram_tensor(f"s{l}_hT", [128, KC, TPC], BF16, kind="Internal").ap(),
              "aT": nc.dram_tensor(f"s{l}_aT", [128, NFC, TPC], BF16, kind="Internal").ap()}
        if moe:
            sc["comb"] = nc.dram_tensor(f"s{l}_comb", [128, NT2, NE], F32, kind="Internal").ap()
        _outproj_norm(nc, cur_tok, yTall_t.ap(), fd, sc, moe, sel=sel)
        if l == 1:
            _ffn_experts(nc, fd, sc, NE if moe else 1, moe, fd["gout_row"], out)
        else:
            x2h_t = nc.dram_tensor(f"x2h_{l}", [TPC, D], F32)
            _ffn_experts(nc, fd, sc, NE if moe else 1, moe, None, x2h_t.ap())
            hTh_t = nc.dram_tensor(f"hTh_{l}", [KC * 128, TPC], BF16)
            hTg_t = nc.dram_tensor(f"hTg_{l}", [2 * KC * 128, TPC], BF16)
            _norm_to_hT(nc, x2h_t.ap(), None, hTh_t.ap().rearrange("(kc p) t -> p kc t", p=128), ntt=NT2)
            ph = Ph(nc)
            ph.allgather(hTg_t, hTh_t, GROUPS, 512)
            ph.close()
            cur_full, cur_tok = None, x2h_t.ap()
            g_ap = hTg_t.ap()
            cur_hT = lambda kc, g_ap=g_ap: [(r * TPC, g_ap[(kc // 4) * 1024 + r * 512 + (kc % 4) * 128:(kc // 4) * 1024 + r * 512 + (kc % 4) * 128 + 128, :]) for r in range(2)]
    return nc


def kernel(**inp):
    inp = {k: np.asarray(v) for k, v in inp.items()}
    consts = _consts()
    x = np.ascontiguousarray(inp["x"], dtype=np.float32)
    nc = build_fused()
    in_maps = []
    for c in range(8):
        b, hh = c // 2, c % 2
        tsl = slice(hh * TPC, (hh + 1) * TPC)
        m = {"x_full": np.ascontiguousarray(x[b]), "x_tok": np.ascontiguousarray(x[b, tsl])}
        selv = np.zeros((128, 2), np.float32)
        selv[:, hh] = 1.0
        m["sel"] = selv
        for l in range(2):
            mi = _mixer_inputs(l, b, hh, inp, None, consts, None)
            for k, v in mi.items():
                if k in ("x", "vfirst_in"):
                    continue
                m[f"m{l}_{k}"] = v
            fi = _ffn_inputs(l, inp, None, None, consts, gathered=True)
            for k, v in fi.items():
                if k in ("x", "yT"):
                    continue
                m[f"f{l}_{k}"] = v
        in_maps.append(m)
    res = run_bass_kernel_spmd(nc, in_maps, core_ids=list(range(8))).results
    outp = np.empty_like(x)
    for c in range(8):
        b, hh = c // 2, c % 2
        outp[b, hh * TPC:(hh + 1) * TPC] = np.asarray(res[c]["out"])
    return outp
```

```python
import numpy as np
import concourse.bass as bass
import concourse.mybir as mybir
from concourse.bass_utils import run_bass_kernel_spmd

F32 = mybir.dt.float32
BF16 = mybir.dt.bfloat16
I32 = mybir.dt.int32
AF = mybir.ActivationFunctionType
ALU = mybir.AluOpType
AX = mybir.AxisListType

SAME_ENG_SYNC = True
ENGS = ["pe", "act", "dve", "pool", "sp"]


class Buf:
    __slots__ = ("name", "w", "rs", "uid", "dram")
    _n = [0]

    def __init__(self, name="", dram=False):
        self.name = name
        self.w = None
        self.rs = []
        Buf._n[0] += 1
        self.uid = Buf._n[0]
        self.dram = dram


class Op:
    __slots__ = ("eng", "fn", "deps", "semkey", "val", "signal", "dma", "force", "cc")


class Prog:
    _uid = [0]

    def __init__(self):
        self.ops = {e: [] for e in ENGS}

    def add(self, eng, fn, reads=(), writes=(), dma=False, semkey=None):
        op = Op()
        op.eng = eng
        op.fn = fn
        op.deps = []
        op.signal = False
        op.val = None
        op.dma = dma
        op.force = []
        op.cc = False
        op.semkey = semkey or (eng + ("_dma" if dma else ""))
        for b in reads:
            if b.w is not None:
                op.deps.append(b.w)
            b.rs.append(op)
        for b in writes:
            if b.w is not None and b.w is not op:
                op.deps.append(b.w)
            op.deps.extend(r for r in b.rs if r is not op)
            b.w = op
            b.rs = []
        self.ops[eng].append(op)
        return op

    @staticmethod
    def _needs_sync(op, d):
        if d.eng != op.eng:
            return True
        if d in op.force:
            return True
        if op.dma or d.dma:
            return True
        if op.eng == "pe":
            return False
        return SAME_ENG_SYNC

    def emit(self, nc, final_waits=()):
        from contextlib import ExitStack
        for e in ENGS:
            for op in self.ops[e]:
                for d in op.deps:
                    if self._needs_sync(op, d):
                        d.signal = True
        for op in final_waits:
            op.signal = True
        last = {}
        for e in ENGS:
            for op in self.ops[e]:
                last[op.semkey] = op
        for op in last.values():
            op.signal = True
        cnt = {}
        for e in ENGS:
            for op in self.ops[e]:
                if op.signal:
                    cnt[op.semkey] = cnt.get(op.semkey, 0) + (16 if (op.dma and not op.cc) else 1)
                    op.val = cnt[op.semkey]
        with ExitStack() as st:
            Prog._uid[0] += 1
            sems = {k: nc.alloc_semaphore(name=f"s{Prog._uid[0]}_" + k) for k in sorted(cnt)}
            block = st.enter_context(nc.Block())

            def run_engine(e, engobj):
                seen = {}
                for op in self.ops[e]:
                    need = {}
                    for d in op.deps:
                        if self._needs_sync(op, d):
                            if need.get(d.semkey, 0) < d.val:
                                need[d.semkey] = d.val
                    for k, v in need.items():
                        if seen.get(k, 0) < v:
                            engobj.wait_ge(sems[k], v)
                            seen[k] = v
                    ins = op.fn(engobj)
                    if op.signal:
                        if op.cc:
                            ins.then_inc(sems[op.semkey])
                        else:
                            ins.then_inc(sems[op.semkey], 16 if op.dma else 1)
                for k in sorted(cnt):
                    if seen.get(k, 0) < cnt[k]:
                        engobj.wait_ge(sems[k], cnt[k])

            block.tensor(lambda eng: run_engine("pe", eng))
            block.scalar(lambda eng: run_engine("act", eng))
            block.vector(lambda eng: run_engine("dve", eng))
            block.gpsimd(lambda eng: run_engine("pool", eng))
            block.sync(lambda eng: run_engine("sp", eng))


class Ph:
    def __init__(self, nc):
        from contextlib import ExitStack
        self.nc = nc
        self.cm = nc.cleanup_on_exit()
        self.cm.__enter__()
        self.st = ExitStack()
        self.P = Prog()
        self.fin = []
        self.n = 0

    _uid = [0]

    def sb(self, shape, dt, name=None):
        Ph._uid[0] += 1
        return self.st.enter_context(self.nc.sbuf_tensor(f"{name or 't'}_{Ph._uid[0]}", list(shape), dt))

    def ps(self, shape, dt, name=None):
        Ph._uid[0] += 1
        return self.st.enter_context(self.nc.psum_tensor(f"{name or 'p'}_{Ph._uid[0]}", list(shape), dt))

    def close(self):
        self.P.emit(self.nc, final_waits=self.fin)
        self.st.close()
        self.cm.__exit__(None, None, None)

    def mm(self, out, lhsT, rhs, start, stop, r, w):
        op = self.P.add("pe", lambda e: e.matmul(out, lhsT=lhsT, rhs=rhs, start=start, stop=stop), r, w)
        rng = (lhsT.base_partition(), lhsT.base_partition() + lhsT.shape[0])
        prev = getattr(self, "_pe_prev", None)
        if prev is not None and (prev[1][1] <= rng[0] or rng[1] <= prev[1][0]):
            op.deps.append(prev[0])
            op.force.append(prev[0])
        self._pe_prev = (op, rng)
        return op

    def tr(self, out, in_, ident, r, w):
        op = self.P.add("pe", lambda e: e.transpose(out, in_, ident), r, w)
        self._pe_prev = (op, (in_.base_partition(), in_.base_partition() + in_.shape[0]))
        return op

    def act(self, out, in_, func, r, w, bias=None, scale=1.0, accum=None):
        def f(e):
            kw = {}
            if bias is not None:
                kw["bias"] = bias
            if accum is not None:
                kw["accum_out"] = accum
            return e.activation(out=out, in_=in_, func=func, scale=scale, **kw)
        return self.P.add("act", f, r, w)

    def tt(self, eng, out, in0, in1, op, r, w):
        return self.P.add(eng, lambda e: e.tensor_tensor(out=out, in0=in0, in1=in1, op=op), r, w)

    def ts(self, eng, out, in0, s1, s2, op0, op1, r, w):
        if s2 is None:
            return self.P.add(eng, lambda e: e.tensor_scalar(out=out, in0=in0, scalar1=s1, scalar2=None, op0=op0), r, w)
        return self.P.add(eng, lambda e: e.tensor_scalar(out=out, in0=in0, scalar1=s1, scalar2=s2, op0=op0, op1=op1), r, w)

    def stt(self, eng, out, in0, scalar, in1, op0, op1, r, w):
        return self.P.add(eng, lambda e: e.scalar_tensor_tensor(out=out, in0=in0, scalar=scalar, in1=in1, op0=op0, op1=op1), r, w)

    def cp(self, eng, out, in_, r, w):
        if eng == "act":
            return self.P.add("act", lambda e: e.copy(out=out, in_=in_), r, w)
        return self.P.add(eng, lambda e: e.tensor_copy(out=out, in_=in_), r, w)

    def memset(self, eng, ap, val, w):
        return self.P.add(eng, lambda e: e.memset(ap, val), (), w)

    def recip(self, out, in_, r, w):
        return self.P.add("dve", lambda e: e.reciprocal(out=out, in_=in_), r, w)

    def scan(self, out, d0, d1, r, w):
        return self.P.add("dve", lambda e: e.tensor_tensor_scan(out=out, data0=d0, data1=d1, initial=0.0, op0=ALU.mult, op1=ALU.add), r, w)

    def dma(self, eng, out, in_, r, w, final=False, key=None):
        slot = None
        for b in list(w) + list(r):
            if not b.dram:
                slot = b
                break
        key = f"{eng}_q{slot.uid}" if slot is not None else f"{eng}_{key or 'x'}"
        op = self.P.add(eng, lambda e: e.dma_start(out=out, in_=in_), r, w, dma=True, semkey=key)
        if final:
            self.fin.append(op)
        return op

    def allgather(self, out_t, in_t, groups, rows_per_chunk):
        nrows = in_t.shape[0]
        RC = rows_per_chunk
        for k in range(nrows // RC):
            Ph._uid[0] += 1
            i_ap = in_t.ap()[k * RC:(k + 1) * RC, :].opt()
            o_ap = out_t.ap()[2 * k * RC:2 * (k + 1) * RC, :].opt()
            op = self.P.add("pool", lambda e, i_ap=i_ap, o_ap=o_ap: e.collective_compute(
                "AllGather", ALU.bypass, replica_groups=groups, ins=[i_ap], outs=[o_ap]),
                (), (), dma=True, semkey=f"pool_cc{Ph._uid[0]}")
            op.cc = True
            self.fin.append(op)

    def rsqrt(self, out, in_, r, w, scale=1.0, bias=None):
        self.act(out, in_, AF.Ln, r, w, bias=bias, scale=scale)
        return self.act(out, out, AF.Exp, w, w, scale=-0.5)


S = 4096
D = 1024
KC = 8
NTB = 8
NTT = 32
DFF = 3584
NE = 8
WDS = 0.6065306597126334
GN_EPS = 64e-5
HP = S + 2


def _norm_to_hT(nc, x, gdummy, hT_dram, rowmap=None):
    ph = Ph(nc)
    xt = [ph.sb([128, D], F32) for _ in range(3)]
    bx = [Buf() for _ in range(3)]
    junk = ph.sb([128, D], BF16)
    bj = Buf()
    ss = ph.sb([128, NTT], F32)
    bss = Buf()
    eps = ph.sb([128, 1], F32)
    beps = Buf()
    ident = ph.sb([128, 128], BF16)
    identf = ph.sb([128, 128], F32)
    bid = Buf()
    xn = [ph.sb([128, D], BF16) for _ in range(2)]
    bxn = [Buf() for _ in range(2)]
    pT = [ph.ps([128, KC, 128], BF16) for _ in range(2)]
    bpT = [Buf() for _ in range(2)]
    hs = [ph.sb([128, KC, 512], BF16) for _ in range(2)]
    bhs = [Buf() for _ in range(2)]
    zc = ph.sb([128, KC, 1], BF16)
    bz = Buf()
    ph.memset("pool", eps[:], 1e-6, [beps])
    ph.memset("pool", zc[:], 0.0, [bz])
    ph.memset("pool", identf[:], 1.0, [bid])
    ph.P.add("pool", lambda e: e.affine_select(out=identf[:], in_=identf[:], pattern=[[-1, 128]], compare_op=ALU.is_equal, fill=0.0, base=0, channel_multiplier=1), [bid], [bid])
    ph.cp("dve", ident[:], identf[:], [bid], [bid])
    class _XV:
        def __getitem__(self, t):
            r0 = t * 128 if rowmap is None else rowmap(t)
            return x[r0:r0 + 128, :]
    xv = _XV()
    for t in range(NTT):
        i = t % 3
        ph.dma("sp", xt[i][:], xv[t], [], [bx[i]], key="ld")
        ph.act(junk[:], xt[i][:], AF.Square, [bx[i]], [bj, bss], accum=ss[:, t:t + 1])
    ph.rsqrt(ss[:], ss[:], [bss, beps], [bss], scale=1.0 / D, bias=eps[:])
    for t in range(NTT):
        i = t % 3
        j = t % 2
        ph.dma("sp", xt[i][:], xv[t], [], [bx[i]], key="ld")
        ph.ts("dve", xn[j][:], xt[i][:], ss[:, t:t + 1], None, ALU.mult, None, [bx[i], bss], [bxn[j]])
        for kc in range(KC):
            ph.tr(pT[j][:, kc, :], xn[j][:, kc * 128:(kc + 1) * 128], ident[:], [bxn[j], bid], [bpT[j]])
        g = (t // 4) % 2
        ph.cp("act", hs[g][:, :, (t % 4) * 128:(t % 4 + 1) * 128], pT[j][:], [bpT[j]], [bhs[g]])
        if t % 4 == 3:
            tb = t // 4
            ph.dma("pool", hT_dram[:, :, tb * 512:(tb + 1) * 512], hs[g][:], [bhs[g]], [], final=True, key="st")
    ph.close()


def _load_hT(ph, hT_dram):
    hT = ph.sb([128, KC, HP], BF16, name="hT")
    bh = [Buf() for _ in range(KC)]
    for kc in range(KC):
        ph.memset("pool", hT[:, kc, 0:1], 0.0, [bh[kc]])
        ph.memset("pool", hT[:, kc, HP - 1:HP], 0.0, [bh[kc]])
        ph.dma("sp" if kc % 2 == 0 else "act", hT[:, kc, 1:1 + S], hT_dram[:, kc, :], [], [bh[kc]])
    return hT, bh


def _prep_w(ph, w_dram, ncols, gm, bgm, name, colvec=None, bcol=None, dst=None):
    wb = dst if dst is not None else ph.sb([128, KC, ncols], BF16, name=name)
    bw = [Buf() for _ in range(KC)]
    stg = [ph.sb([128, ncols], F32, name=f"{name}_s{i}") for i in range(2)]
    bs = [Buf() for _ in range(2)]
    wv = w_dram.rearrange("(kc p) n -> kc p n", p=128)
    for kc in range(KC):
        i = kc % 2
        ph.dma("sp", stg[i][:], wv[kc], [], [bs[i]], key="ldw")
        if colvec is None:
            ph.ts("pool" if kc % 2 else "dve", wb[:, kc, :], stg[i][:], gm[:, kc:kc + 1], None, ALU.mult, None, [bs[i], bgm], [bw[kc]])
        else:
            ph.stt("dve", wb[:, kc, :], stg[i][:], gm[:, kc:kc + 1], colvec, ALU.mult, ALU.mult, [bs[i], bgm, bcol], [bw[kc]])
    return wb, bw


def _attention(nc, hT_dram, d, lam_init, yT):
    ph = Ph(nc)
    hT, bh = _load_hT(ph, hT_dram)
    gm = ph.sb([128, KC], F32)
    bgm = Buf()
    ph.dma("sp", gm[:], d["gmix"], [], [bgm])
    pp = ph.sb([128, 8], F32)
    bpp = Buf()
    ph.dma("sp", pp[:], d["att_pp"], [], [bpp])
    lqk = ph.sb([128, 2, 128], F32)
    blq = Buf()
    ph.dma("sp", lqk[:, 0, :], d["lam_q"].partition_broadcast(128), [], [blq])
    ph.dma("sp", lqk[:, 1, :], d["lam_k"].partition_broadcast(128), [], [blq])
    lprod = ph.sb([128, 128], F32)
    lsum = ph.sb([128, 2], F32)
    neglam = ph.sb([128, 1], F32)
    sw = ph.sb([128, 1], F32)
    bl = Buf()
    ph.tt("dve", lprod[:], lqk[:, 0, :], lqk[:, 1, :], ALU.mult, [blq], [bl])
    ph.P.add("dve", lambda e: e.reduce_sum(out=lsum[:], in_=lprod[:].rearrange("p (a b) -> p a b", a=2), axis=AX.X), [bl], [bl])
    ph.act(lsum[:], lsum[:], AF.Exp, [bl], [bl])
    ph.tt("dve", neglam[:], lsum[:, 1:2], lsum[:, 0:1], ALU.subtract, [bl], [bl])
    ph.ts("dve", neglam[:], neglam[:], -lam_init, None, ALU.add, None, [bl], [bl])
    ph.ts("dve", sw[:], pp[:, 2:3], 1.0 - lam_init, None, ALU.mult, None, [bpp], [bl])
    cosT = ph.sb([128, S], BF16)
    sinT = ph.sb([128, S], BF16)
    halfpi = ph.sb([128, 1], F32)
    bcs = Buf()
    bhp = Buf()
    ph.memset("pool", halfpi[:], float(np.pi / 2), [bhp])
    CW = 1024
    posi = [ph.sb([128, CW], I32)] * 2
    ang = [ph.sb([128, CW], F32)] * 2
    angf = [ph.sb([128, CW], F32)] * 2
    btt = [Buf()] * 2
    for c4 in range(S // CW):
        j = c4 % 2
        cs_ = slice(c4 * CW, (c4 + 1) * CW)
        bt = btt[j]
        ph.dma("sp", posi[j][:], d["pos"][:, cs_].partition_broadcast(128), [], [bt])
        ph.cp("dve", ang[j][:], posi[j][:], [bt], [bt])
        ph.ts("dve", ang[j][:], ang[j][:], pp[:, 0:1], float(1.0 / (2 * np.pi)), ALU.mult, ALU.mult, [bt, bpp], [bt])
        ph.cp("dve", posi[j][:], ang[j][:], [bt], [bt])
        ph.cp("pool", angf[j][:], posi[j][:], [bt], [bt])
        ph.tt("dve", ang[j][:], ang[j][:], angf[j][:], ALU.subtract, [bt], [bt])
        ph.act(angf[j][:], ang[j][:], AF.Sin, [bt], [bt], scale=float(2 * np.pi))
        ph.ts("dve", sinT[:, cs_], angf[j][:], pp[:, 1:2], None, ALU.mult, None, [bt, bpp], [bcs])
        ph.act(ang[j][:], ang[j][:], AF.Abs, [bt], [bt], scale=float(2 * np.pi))
        ph.act(cosT[:, cs_], ang[j][:], AF.Sin, [bt, bhp], [bcs], scale=-1.0, bias=halfpi[:])
    wat, bwat = _prep_w(ph, d["w_att"], 768, gm, bgm, "wat")
    wsw, bwsw = _prep_w(ph, d["w_att_sw"], 512, gm, bgm, "wsw")
    qk = [ph.sb([128, S], BF16, name=f"qk{c}") for c in range(4)]
    bqk = [Buf() for _ in range(4)]
    pa = [ph.ps([128, 512], F32) for _ in range(2)]
    pb = [ph.ps([128, 512], F32) for _ in range(2)]
    bpa = [Buf() for _ in range(2)]
    bpb = [Buf() for _ in range(2)]
    t1 = [ph.sb([128, 512], F32) for _ in range(2)]
    t2 = [ph.sb([128, 512], F32) for _ in range(2)]
    bt1 = [Buf() for _ in range(2)]
    bt2 = [Buf() for _ in range(2)]
    it = 0
    for cc in range(4):
        for tb in range(NTB):
            j = it % 2
            it += 1
            tsl = slice(tb * 512, (tb + 1) * 512)
            for kc in range(KC):
                ph.mm(pa[j][:], wat[:, kc, cc * 128:(cc + 1) * 128], hT[:, kc, 1 + tb * 512:1 + (tb + 1) * 512], kc == 0, kc == KC - 1, [bwat[kc], bh[kc]], [bpa[j]])
            for kc in range(KC):
                ph.mm(pb[j][:], wsw[:, kc, cc * 128:(cc + 1) * 128], hT[:, kc, 1 + tb * 512:1 + (tb + 1) * 512], kc == 0, kc == KC - 1, [bwsw[kc], bh[kc]], [bpb[j]])
            ph.tt("dve", t1[j][:], pa[j][:], cosT[:, tsl], ALU.mult, [bpa[j], bcs], [bt1[j]])
            ph.tt("dve", t2[j][:], pb[j][:], sinT[:, tsl], ALU.mult, [bpb[j], bcs], [bt2[j]])
            ph.tt("pool", qk[cc][:, tsl], t1[j][:], t2[j][:], ALU.add, [bt1[j], bt2[j]], [bqk[cc]])
    vtm = ph.sb([128, NTT, 256], BF16)
    bv = Buf()
    for t in range(NTT):
        j = t % 2
        for kc in range(KC):
            ph.mm(pa[j][:, 0:256], hT[:, kc, 1 + t * 128:1 + (t + 1) * 128], wat[:, kc, 512:768], kc == 0, kc == KC - 1, [bwat[kc], bh[kc]], [bpa[j]])
        ph.cp("act", vtm[:, t, :], pa[j][:, 0:256], [bpa[j]], [bv])
    ones = ph.sb([128, 128], BF16)
    bones = Buf()
    ph.memset("pool", ones[:], 1.0, [bones])
    eps = ph.sb([128, 1], F32)
    ph.memset("pool", eps[:], 1e-5, [bones])
    NPS, NET, LAG = 2, 3, 1
    psS = [ph.ps([128, 2, 512], F32) for _ in range(NPS)]
    bpsS = [Buf() for _ in range(NPS)]
    eT = [ph.sb([128, 2, 512], BF16) for _ in range(NET)]
    beT = [Buf() for _ in range(NET)]
    accO = pa
    baO = bpa
    accZ = pb
    baZ = bpb
    onesf = ph.sb([128, 128], F32)
    ph.memset("pool", onesf[:], 1.0, [bones])
    _zt = [ang[0][:], angf[0][:]]
    zt2 = ph.sb([128, 1024], F32)
    _zt.append(zt2[:])
    _zt.append(posi[0][:].bitcast(F32))
    _zold = [btt[0], btt[0], None, btt[0]]
    zaccs = [[_zt[bk * 2 + m_] for m_ in range(2)] for bk in range(2)]
    bzaccs = [[Buf() for _ in range(2)] for _ in range(2)]
    zold = [[[_zold[bk * 2 + m_]] if _zold[bk * 2 + m_] is not None else [] for m_ in range(2)] for bk in range(2)]
    rz = [t1[0], t1[1]]
    om = [t2[0], t2[1]]
    brz = [bt1[0], bt1[1]]
    bom = [bt2[0], bt2[1]]
    o = ph.sb([128, 512], F32)
    sq = ph.sb([128, 512], BF16)
    yb = [ph.sb([128, 512], BF16) for _ in range(2)]
    byb = [Buf() for _ in range(2)]
    bo = Buf()
    bsq = Buf()
    scale = 0.125
    NKP = NTT // 2
    its = [(h, qb, m, kp) for h in range(2) for qb in range(NTB) for m in range(2) for kp in range(NKP)]
    n = len(its)

    def finalize(h, qb):
        qs = slice(qb * 512, (qb + 1) * 512)
        zacc, bzacc = zaccs[(h * NTB + qb) % 2], bzaccs[(h * NTB + qb) % 2]
        for m in range(2):
            ph.mm(accZ[m][:], onesf[:], zacc[m][:, 0:512], True, False, [bones, bzacc[m]], [baZ[m]])
            ph.mm(accZ[m][:], onesf[:], zacc[m][:, 512:1024], False, True, [bones, bzacc[m]], [baZ[m]])
            ph.recip(rz[m][:], accZ[m][:], [baZ[m]], [brz[m]])
            ph.tt("dve", om[m][:], accO[m][:], rz[m][:], ALU.mult, [baO[m], brz[m]], [bom[m]])
        ph.stt("dve", o[:], om[1][:], neglam[:, 0:1], om[0][:], ALU.mult, ALU.add, [bom[0], bom[1], bl], [bo])
        ph.tt("pool", sq[:], o[:], o[:], ALU.mult, [bo], [bsq])
        ph.mm(accZ[0][:], ones[:], sq[:], True, True, [bones, bsq], [baZ[0]])
        ph.rsqrt(rz[0][:], accZ[0][:], [baZ[0], bones], [brz[0]], scale=1.0 / 128, bias=eps[:])
        ph.tt("dve", o[:], o[:], rz[0][:], ALU.mult, [bo, brz[0]], [bo])
        y = yb[qb % 2]
        ph.ts("dve", y[:], o[:], sw[:, 0:1], None, ALU.mult, None, [bo, bl], [byb[qb % 2]])
        ph.dma("pool", yT[128 + h * 128:256 + h * 128, qs], y[:], [byb[qb % 2]], [], final=True, key="st")

    for i in range(n + LAG):
        if i < n:
            h, qb, m, kp = its[i]
            ms = slice(m * 64, (m + 1) * 64)
            qs = slice(qb * 512, (qb + 1) * 512)
            a, b = i % NPS, i % NET
            for u in range(2):
                kt = kp * 2 + u
                ph.mm(psS[a][:, u, :], qk[2 + h][ms, kt * 128:(kt + 1) * 128], qk[h][ms, qs], True, True, [bqk[2 + h], bqk[h]], [bpsS[a]])
            ph.act(eT[b][:], psS[a][:], AF.Exp, [bpsS[a]], [beT[b]], scale=scale)
            zacc, bzacc = zaccs[(h * NTB + qb) % 2], bzaccs[(h * NTB + qb) % 2]
            ef = eT[b][:].rearrange("p a n -> p (a n)")
            if kp == 0:
                zo = zold[(h * NTB + qb) % 2][m]
                ph.cp("dve", zacc[m], ef, [beT[b]], [bzacc[m]] + zo)
                del zo[:]
            else:
                ph.tt("dve", zacc[m], zacc[m], ef, ALU.add, [beT[b], bzacc[m]], [bzacc[m]])
        if i >= LAG:
            j = i - LAG
            h, qb, m, kp = its[j]
            b = j % NET
            for u in range(2):
                kt = kp * 2 + u
                ph.mm(accO[m][:], vtm[:, kt, h * 128:(h + 1) * 128], eT[b][:, u, :], kt == 0, kt == NTT - 1, [bv, beT[b]], [baO[m]])
            if m == 1 and kp == NKP - 1:
                finalize(h, qb)
    ph.close()


def _pool_mixer(nc, hT_dram, d, yT):
    ph = Ph(nc)
    hT, bh = _load_hT(ph, hT_dram)
    gm = ph.sb([128, KC], F32)
    bgm = Buf()
    ph.dma("sp", gm[:], d["gmix"], [], [bgm])
    pc = ph.sb([128, 8 + 64], F32)
    bpc = Buf()
    ph.dma("sp", pc[:], d["pool_pp"], [], [bpc])
    wp, bwp = _prep_w(ph, d["w_pool"], 128, gm, bgm, "wp")
    pmx_f = ph.sb([128, 128], F32)
    pmx = ph.sb([128, 128], BF16)
    bpm = Buf()
    ph.dma("sp", pmx_f[:], d["pool_mix_bd"], [], [bpm])
    ph.cp("dve", pmx[:], pmx_f[:], [bpm], [bpm])
    PADW = 8
    W = S + 2 * PADW
    u = ph.sb([128, W], F32)
    s2 = ph.sb([128, W], F32)
    s4 = ph.sb([128, W], F32)
    s8 = ph.sb([128, W], F32)
    s16 = ph.sb([128, W], F32)
    bu, b2, b4, b8, b16 = [Buf() for _ in range(5)]
    ph.memset("pool", u[:, 0:PADW], 0.0, [bu])
    ph.memset("pool", u[:, W - PADW:W], 0.0, [bu])
    pa = [ph.ps([128, 512], F32) for _ in range(2)]
    bpa = [Buf() for _ in range(2)]
    for tb in range(NTB):
        j = tb % 2
        for kc in range(KC):
            ph.mm(pa[j][:], wp[:, kc, :], hT[:, kc, 1 + tb * 512:1 + (tb + 1) * 512], kc == 0, kc == KC - 1, [bwp[kc], bh[kc]], [bpa[j]])
        ph.cp("act", u[:, PADW + tb * 512:PADW + (tb + 1) * 512], pa[j][:], [bpa[j]], [bu])
    c = slice(PADW, PADW + S)

    ph.tt("dve", s2[:, 1:W], u[:, 0:W - 1], u[:, 1:W], ALU.add, [bu], [b2])
    ph.tt("pool", s4[:, 2:W - 1], s2[:, 1:W - 2], s2[:, 3:W], ALU.add, [b2], [b4])
    ph.tt("dve", s8[:, 4:W - 3], s4[:, 2:W - 5], s4[:, 6:W - 1], ALU.add, [b4], [b8])
    ph.tt("pool", s16[:, 8:W - 7], s8[:, 4:W - 11], s8[:, 12:W - 3], ALU.add, [b8], [b16])
    acc = ph.sb([128, S], F32)
    bacc = Buf()
    ph.ts("dve", acc[:], s2[:, c], pc[:, 0:1], None, ALU.mult, None, [b2, bpc], [bacc])
    ph.stt("dve", acc[:], s4[:, c], pc[:, 1:2], acc[:], ALU.mult, ALU.add, [b4, bpc, bacc], [bacc])
    ph.stt("dve", acc[:], s8[:, c], pc[:, 2:3], acc[:], ALU.mult, ALU.add, [b8, bpc, bacc], [bacc])
    ph.stt("dve", acc[:], s16[:, c], pc[:, 3:4], acc[:], ALU.mult, ALU.add, [b16, bpc, bacc], [bacc])
    bt_ = ph.sb([128, 16], F32)
    tmpb = ph.sb([128, 16], F32)
    bbt = Buf()
    for wi, (sw_, bs_) in enumerate(((s2, b2), (s4, b4), (s8, b8), (s16, b16))):
        for half in range(2):
            src = sw_[:, PADW:PADW + 8] if half == 0 else sw_[:, PADW + S - 8:PADW + S]
            dst = bt_[:, half * 8:(half + 1) * 8]
            tb_ = pc[:, 8 + wi * 16 + half * 8:8 + wi * 16 + half * 8 + 8]
            if wi == 0:
                ph.tt("dve", dst, src, tb_, ALU.mult, [bs_, bpc], [bbt])
            else:
                ph.tt("dve", tmpb[:, half * 8:(half + 1) * 8], src, tb_, ALU.mult, [bs_, bpc], [bbt])
                ph.tt("dve", dst, dst, tmpb[:, half * 8:(half + 1) * 8], ALU.add, [bbt], [bbt])
    ph.cp("dve", acc[:, 0:8], bt_[:, 0:8], [bbt, bacc], [bacc])
    ph.cp("dve", acc[:, S - 8:S], bt_[:, 8:16], [bbt, bacc], [bacc])
    pooled = ph.sb([128, S], BF16)
    bpl = Buf()
    ph.tt("dve", pooled[:], acc[:], u[:, c], ALU.subtract, [bacc, bu], [bpl])
    yb = [ph.sb([128, 512], BF16) for _ in range(2)]
    byb = [Buf() for _ in range(2)]
    for tb in range(NTB):
        j = tb % 2
        ph.mm(pa[j][:], pmx[:], pooled[:, tb * 512:(tb + 1) * 512], True, True, [bpm, bpl], [bpa[j]])
        ph.ts("dve", yb[j][:], pa[j][:], pc[:, 4:5], None, ALU.mult, None, [bpa[j], bpc], [byb[j]])
        ph.dma("pool", yT[384:512, tb * 512:(tb + 1) * 512], yb[j][:], [byb[j]], [], final=True, key="st")
    ph.close()


def _rwkv_proj(nc, hT_dram, d, layer, rw):
    ph = Ph(nc)
    hT, bh = _load_hT(ph, hT_dram)
    gm = ph.sb([128, KC], F32)
    bgm = Buf()
    ph.dma("sp", gm[:], d["gmix"], [], [bgm])
    mu0 = ph.sb([128, 640], F32)
    mu1 = ph.sb([128, 640], F32)
    c0 = ph.sb([128, 640], F32)
    bmu = Buf()
    ph.dma("sp", mu0[:], d["mu"][0:1, :].partition_broadcast(128), [], [bmu])
    ph.dma("sp", mu1[:], d["mu"][1:2, :].partition_broadcast(128), [], [bmu])
    ph.tt("dve", c0[:], mu0[:], mu1[:], ALU.add, [bmu], [bmu])
    ph.ts("dve", c0[:], c0[:], -1.0, 1.0, ALU.mult, ALU.add, [bmu], [bmu])
    ws = []
    for i, cv in enumerate((c0, mu0, mu1)):
        ws.append(_prep_w(ph, d["w_rwkv"], 640, gm, bgm, f"wr{i}", colvec=cv[:], bcol=bmu))
    offs = (1, 0, 2)
    gup_f = ph.sb([128, 128], F32)
    gup = ph.sb([128, 128], BF16)
    bgu = Buf()
    ph.dma("sp", gup_f[:], d["gate_up"], [], [bgu])
    ph.cp("dve", gup[:], gup_f[:], [bgu], [bgu])
    if layer > 0:
        wvd, bwvd = _prep_w(ph, d["w_vdown"], 32, gm, bgm, "wvd")
        vup_f = ph.sb([32, 128], F32)
        vup = ph.sb([32, 128], BF16)
        vb_ = ph.sb([128, 1], F32)
        bvu = Buf()
        ph.dma("sp", vup_f[:], d["vres_up"], [], [bvu])
        ph.dma("sp", vb_[:], d["vres_bias"], [], [bvu])
        ph.cp("dve", vup[:], vup_f[:], [bvu], [bvu])
        pvd = ph.ps([32, 512], F32)
        pvg = ph.ps([128, 512], F32)
        bpvd, bpvg = Buf(), Buf()
    pr = [ph.ps([128, 512], F32) for _ in range(5)]
    bpr = [Buf() for _ in range(5)]
    pg = ph.ps([128, 512], F32)
    bpg = Buf()
    ob = [ph.sb([128, 5, 512], BF16) for _ in range(2)]
    bob = [Buf() for _ in range(2)]
    vf = [ph.sb([128, 512], F32) for _ in range(2)]
    bvf = [Buf() for _ in range(2)]
    sgx = ph.sb([128, 512], BF16)
    bsgx = Buf()
    if layer > 0:
        vdb = ph.sb([32, 512], BF16)
        sgv = ph.sb([128, 512], F32)
        dif = ph.sb([128, 512], F32)
        bvdb, bsgv, bdif = Buf(), Buf(), Buf()
    for tb in range(NTB):
        j = tb % 2
        tsl = slice(tb * 512, (tb + 1) * 512)
        for cc in range(5):
            n = 0
            for si in range(3):
                wb, bw = ws[si]
                for kc in range(KC):
                    ph.mm(pr[cc][:], wb[:, kc, cc * 128:(cc + 1) * 128], hT[:, kc, offs[si] + tb * 512:offs[si] + (tb + 1) * 512],
                          n == 0, n == 3 * KC - 1, [bw[kc], bh[kc]], [bpr[cc]])
                    n += 1
        ph.cp("act", ob[j][:, 0, :], pr[0][:], [bpr[0]], [bob[j]])
        ph.cp("dve", ob[j][:, 1, :], pr[1][:], [bpr[1]], [bob[j]])
        ph.cp("act", ob[j][:, 3, :], pr[3][:], [bpr[3]], [bob[j]])
        if layer == 0:
            ph.cp("dve", vf[j][:], pr[2][:], [bpr[2]], [bvf[j]])
            ph.cp("pool", ob[j][:, 2, :], vf[j][:], [bvf[j]], [bob[j]])
            ph.dma("pool", rw["vfirst"][:, tsl], vf[j][:], [bvf[j]], [], final=True, key="st")
        else:
            ph.dma("sp", vf[j][:], rw["vfirst"][:, tsl], [], [bvf[j]], key="ldv")
            for kc in range(KC):
                ph.mm(pvd[:], wvd[:, kc, :], hT[:, kc, 1 + tb * 512:1 + (tb + 1) * 512], kc == 0, kc == KC - 1, [bwvd[kc], bh[kc]], [bpvd])
            ph.cp("act", vdb[:], pvd[:], [bpvd], [bvdb])
            ph.mm(pvg[:], vup[:], vdb[:], True, True, [bvu, bvdb], [bpvg])
            ph.act(sgv[:], pvg[:], AF.Sigmoid, [bpvg, bvu], [bsgv], bias=vb_[:])
            ph.tt("dve", dif[:], vf[j][:], pr[2][:], ALU.subtract, [bvf[j], bpr[2]], [bdif])
            ph.tt("pool", dif[:], dif[:], sgv[:], ALU.mult, [bdif, bsgv], [bdif])
            ph.tt("dve", ob[j][:, 2, :], dif[:], pr[2][:], ALU.add, [bdif, bpr[2]], [bob[j]])
        ph.act(sgx[:], pr[4][:], AF.Sigmoid, [bpr[4]], [bsgx])
        ph.mm(pg[:], gup[:], sgx[:], True, True, [bgu, bsgx], [bpg])
        ph.cp("act", ob[j][:, 4, :], pg[:], [bpg], [bob[j]])
        ph.dma("pool", rw["tok"][:, :, tsl], ob[j][:], [bob[j]], [], final=True, key="st")
    ph.close()


QN = 7


def _rwkv_derive(nc, d, rw):
    ph = Ph(nc)
    pp = ph.sb([128, 16], F32)
    bpp = Buf()
    ph.dma("sp", pp[:], d["rw_pp"], [], [bpp])
    bones = ph.sb([128, 128], F32)
    bbo = Buf()
    ph.dma("sp", bones[:], d["blockones"], [], [bbo])
    lo_f = ph.sb([128, 4, 128], F32)
    lo = ph.sb([128, 4, 128], BF16)
    blo = Buf()
    for i in range(4):
        ph.dma("sp", lo_f[:, i, :], d["lora"][i], [], [blo])
    ph.cp("dve", lo[:], lo_f[:], [blo], [blo])
    cmask = ph.sb([128, 512], F32)
    bcm = Buf()
    ph.memset("pool", cmask[:], 1.0, [bcm])
    ph.memset("pool", cmask[:].rearrange("p (c i) -> p c i", i=128)[:, :, 0:1], 0.0, [bcm])
    tiny = ph.sb([128, 1], F32)
    ph.memset("pool", tiny[:], 0.0, [bcm])
    tin = [ph.sb([128, 4, 512], BF16) for _ in range(2)]
    btin = [Buf() for _ in range(2)]
    th = ph.sb([128, 512], BF16)
    bth = Buf()
    pL = [ph.ps([128, 512], F32) for _ in range(4)]
    bpL = [Buf() for _ in range(4)]
    pN = ph.ps([128, 512], F32)
    pB = ph.ps([128, 512], F32)
    bpN, bpB = Buf(), Buf()
    sig = [ph.sb([128, 512], F32) for _ in range(2)]
    aa = [ph.sb([128, 512], F32) for _ in range(2)]
    bsig = [Buf() for _ in range(2)]
    baa = [Buf() for _ in range(2)]
    kk = ph.sb([128, 512], F32)
    sq = ph.sb([128, 512], F32)
    rs = ph.sb([128, 512], F32)
    kkn = ph.sb([128, 512], F32)
    rk = ph.sb([128, 512], F32)
    kd = [ph.sb([128, 512], F32) for _ in range(2)]
    tmp = ph.sb([128, 512], F32)
    ksum = ph.sb([128, 512], F32)
    bon = [ph.sb([128, 512], BF16) for _ in range(2)]
    bkk, bsq, brs, bkkn, brk, btmp, bks = [Buf() for _ in range(7)]
    bkd = [Buf() for _ in range(2)]
    bbon = [Buf() for _ in range(2)]
    cs = ph.sb([128, 512], F32)
    csx = ph.sb([128, 512], F32)
    dln = ph.sb([128, 512], F32)
    eL = ph.sb([128, 512], F32)
    eLm = ph.sb([128, 512], F32)
    eLex = ph.sb([128, 512], F32)
    eLC = ph.sb([128, 512], F32)
    ka = ph.sb([128, 512], F32)
    bcs, bcsx, bdln, beL, beLm, beLex, beLC, bka = [Buf() for _ in range(8)]
    outq = [ph.sb([128, QN, 512], BF16) for _ in range(2)]
    bout = [Buf() for _ in range(2)]
    wc = [ph.sb([128, 4], F32) for _ in range(2)]
    bwc = [Buf() for _ in range(2)]
    oi = 0
    for tb in range(NTB):
        j = tb % 2
        tsl = slice(tb * 512, (tb + 1) * 512)
        ti = tin[j]
        ph.dma("sp", ti[:], rw["tok"][:, 0:4, tsl], [], [btin[j]], key="ld")
        rb, kb, vb, xb = ti[:, 0, :], ti[:, 1, :], ti[:, 2, :], ti[:, 3, :]
        ph.act(th[:], xb, AF.Tanh, [btin[j]], [bth])
        for dd in range(2):
            ph.mm(pL[dd][:], lo[:, dd, :], th[:], True, True, [blo, bth], [bpL[dd]])
            ph.mm(pL[2 + dd][:], lo[:, 2 + dd, :], xb, True, True, [blo, btin[j]], [bpL[2 + dd]])
        for dd in range(2):
            ph.act(sig[dd][:], pL[dd][:], AF.Sigmoid, [bpL[dd], bpp], [bsig[dd]], bias=pp[:, 5 + dd:6 + dd])
            ph.act(aa[dd][:], pL[2 + dd][:], AF.Sigmoid, [bpL[2 + dd], bpp], [baa[dd]], bias=pp[:, 7 + dd:8 + dd])
        ph.ts("dve", kk[:], kb, pp[:, 0:1], None, ALU.mult, None, [btin[j], bpp], [bkk])
        ph.tt("pool", sq[:], kk[:], kk[:], ALU.mult, [bkk], [bsq])
        ph.mm(pN[:], bones[:], sq[:], True, True, [bbo, bsq], [bpN])
        ph.ts("dve", rs[:], pN[:], 1e-24, None, ALU.max, None, [bpN], [brs])
        ph.rsqrt(rs[:], rs[:], [brs, bcm], [brs], bias=tiny[:])
        ph.tt("dve", kkn[:], kk[:], rs[:], ALU.mult, [bkk, brs], [bkkn])
        ph.ts("pool", rk[:], rb, pp[:, 2:3], None, ALU.mult, None, [btin[j], bpp], [brk])
        for dd in range(2):
            ph.ts("dve", tmp[:], aa[dd][:], -1.0, pp[:, 1:2], ALU.add, ALU.mult, [baa[dd], bpp], [btmp])
            ph.stt("dve", kd[dd][:], tmp[:], 1.0, kb, ALU.add, ALU.mult, [btmp, btin[j]], [bkd[dd]])
        ph.tt("pool", ksum[:], kd[0][:], kd[1][:], ALU.add, [bkd[0], bkd[1]], [bks])
        ph.tt("pool", ksum[:], ksum[:], rk[:], ALU.mult, [bks, brk], [bks])
        ph.mm(pB[:], bones[:], ksum[:], True, True, [bbo, bks], [bpB])
        ph.tt("dve", bon[j][:], pB[:], vb, ALU.mult, [bpB, btin[j]], [bbon[j]])
        ph.dma("pool", rw["bonus"][:, tsl], bon[j][:], [bbon[j]], [], final=True, key="st")
        for dd in range(2):
            def rv(ap, dd=dd):
                return ap if dd == 0 else ap[:, ::-1]
            obk = tb if dd == 0 else NTB - 1 - tb
            osl = slice(obk * 512, (obk + 1) * 512)
            oq = outq[oi % 2]
            boq = bout[oi % 2]
            wct = wc[oi % 2]
            bwct = bwc[oi % 2]
            oi += 1
            ph.scan(cs[:], cmask[:], rv(sig[dd][:]), [bcm, bsig[dd]], [bcs])
            ph.tt("pool", csx[:], cs[:], rv(sig[dd][:]), ALU.subtract, [bcs, bsig[dd]], [bcsx])
            csv = cs[:].rearrange("p (c i) -> p c i", i=128)
            ph.tt("pool", dln[:].rearrange("p (c i) -> p c i", i=128), csv, csv[:, :, 127:128].to_broadcast([128, 4, 128]), ALU.subtract, [bcs], [bdln])
            ph.act(eL[:], cs[:], AF.Exp, [bcs], [beL], scale=-WDS)
            ph.act(eLm[:], cs[:], AF.Exp, [bcs], [beLm], scale=WDS)
            ph.act(eLex[:], csx[:], AF.Exp, [bcsx], [beLex], scale=-WDS)
            ph.act(eLC[:], dln[:], AF.Exp, [bdln], [beLC], scale=WDS)
            ph.act(wct[:], csv[:, :, 127], AF.Exp, [bcs], [bwct], scale=-WDS)
            ph.tt("pool", ka[:], kkn[:], aa[dd][:], ALU.mult, [bkkn, baa[dd]], [bka])
            ph.stt("dve", oq[:, 0, :], rv(kkn[:]), -1.0, eLex[:], ALU.mult, ALU.mult, [bkkn, beLex], [boq])
            ph.tt("dve", oq[:, 1, :], rv(ka[:]), eLm[:], ALU.mult, [bka, beLm], [boq])
            ph.tt("pool", oq[:, 2, :], rv(kd[dd][:]), eLm[:], ALU.mult, [bkd[dd], beLm], [boq])
            ph.tt("dve", oq[:, 3, :], rv(rb), eL[:], ALU.mult, [btin[j], beL], [boq])
            ph.tt("pool", oq[:, 4, :], rv(ka[:]), eLC[:], ALU.mult, [bka, beLC], [boq])
            ph.tt("dve", oq[:, 5, :], rv(kd[dd][:]), eLC[:], ALU.mult, [bkd[dd], beLC], [boq])
            ph.cp("pool", oq[:, 6, :], rv(vb), [btin[j]], [boq])
            ph.dma("pool", rw["sc"][dd][:, :, osl], oq[:], [boq], [], final=True, key="st")
            ph.dma("pool", rw["wc"][dd][:, obk * 4:(obk + 1) * 4], wct[:], [bwct], [], final=True, key="st")
    ph.close()


def _rwkv_scan(nc, d, rw):
    ph = Ph(nc)
    NCH = S // 128
    X = []
    bX = []
    for dd in range(2):
        x_ = ph.sb([128, QN, S], BF16, name=f"X{dd}")
        b_ = [Buf() for _ in range(QN)]
        for q in range(QN):
            ph.dma("sp" if q % 2 == 0 else "act", x_[:, q, :], rw["sc"][dd][:, q, :], [], [b_[q]])
        X.append(x_)
        bX.append(b_)
    WC = ph.sb([128, 2, NCH], F32)
    bWC = Buf()
    for dd in range(2):
        ph.dma("sp", WC[:, dd, :], rw["wc"][dd], [], [bWC])
    mk = ph.sb([128, 3, 4, 128], F32)
    bmk = Buf()
    for i in range(3):
        for r_ in range(4):
            ph.dma("sp", mk[:, i, r_, :], d["masks"][i], [], [bmk])
    idf = ph.sb([128, 128], F32)
    ident = ph.sb([128, 128], BF16)
    id64 = ph.sb([128, 64], F32)
    bid = Buf()
    ph.dma("sp", idf[:], d["ident"], [], [bid])
    ph.dma("sp", id64[:], d["ident64x2"], [], [bid])
    ph.cp("dve", ident[:], idf[:], [bid], [bid])
    B = [ph.ps([128, 4, 128], F32, name=f"B{i}") for i in range(7)]
    bB = [Buf() for _ in range(7)]
    BT = ph.ps([128, 4, 2, 128], BF16, name="BT")
    bBT = Buf()
    Nn = [ph.sb([128, 4, 128], BF16) for _ in range(2)]
    NT = [ph.sb([128, 4, 128], BF16) for _ in range(2)]
    bNn = [Buf() for _ in range(2)]
    bNT = [Buf() for _ in range(2)]
    Mak = ph.sb([128, 4, 128], BF16)
    Mbr = ph.sb([128, 4, 128], BF16)
    Mkr = ph.sb([128, 4, 128], BF16)
    bMak, bMbr, bMkr = Buf(), Buf(), Buf()
    TTs = ph.sb([128, 4, 2, 128], BF16)
    bTT = Buf()
    Z = [ph.sb([128, 4, 128], BF16) for _ in range(2)]
    bZ = [Buf() for _ in range(2)]
    G = ph.sb([128, 2, 128], BF16)
    Phi = ph.sb([128, 2, 64], BF16)
    bG, bPhi = Buf(), Buf()
    ST = [ph.sb([128, 2, 64], BF16) for _ in range(2)]
    bST = [Buf() for _ in range(2)]
    Yo = [ph.sb([128, 2, 128], F32) for _ in range(2)]
    bYo = [Buf() for _ in range(2)]
    qsel = (0, 4, 5, 6)
    for c in range(NCH):
        cs_ = slice(c * 128, (c + 1) * 128)
        for h in range(2):
            for dd in range(2):
                i = dd * 2 + h
                hs = slice(h * 64, (h + 1) * 64)
                At, Bt, Kt, Rt = (X[dd][hs, q, cs_] for q in range(4))
                ph.mm(B[0][:, i, :], Bt, At, True, True, bX[dd][0:4], [bB[0]])
                ph.mm(B[1][:, i, :], At, Bt, True, True, bX[dd][0:4], [bB[1]])
                ph.mm(B[2][:, i, :], Kt, At, True, True, bX[dd][0:4], [bB[2]])
                ph.mm(B[3][:, i, :], Bt, Rt, True, True, bX[dd][0:4], [bB[3]])
                ph.mm(B[4][:, i, :], Kt, Rt, True, True, bX[dd][0:4], [bB[4]])
        ph.tt("dve", Nn[0][:], B[0][:], mk[:, 0], ALU.mult, [bB[0], bmk], [bNn[0]])
        ph.tt("dve", NT[0][:], B[1][:], mk[:, 1], ALU.mult, [bB[1], bmk], [bNT[0]])
        ph.tt("dve", Mak[:], B[2][:], mk[:, 0], ALU.mult, [bB[2], bmk], [bMak])
        ph.tt("dve", Mbr[:], B[3][:], mk[:, 2], ALU.mult, [bB[3], bmk], [bMbr])
        ph.tt("dve", Mkr[:], B[4][:], mk[:, 2], ALU.mult, [bB[4], bmk], [bMkr])
        for qi, q in enumerate(qsel):
            for dd in range(2):
                ph.tr(BT[:, qi, dd, :], X[dd][:, q, cs_], ident[:], [bX[dd][q], bid], [bBT])
        ph.cp("act", TTs[:], BT[:], [bBT], [bTT])
        for dd in range(2):
            for h in range(2):
                i = dd * 2 + h
                ph.mm(B[5][:, i, 0:64], Mak[:, i, :], TTs[:, 3, dd, h * 64:(h + 1) * 64], True, True, [bMak, bTT], [bB[5]])
        ph.cp("pool", Z[0][:, :, 0:64], TTs[:, 0].rearrange("p d (h k) -> p (d h) k", h=2), [bTT], [bZ[0]])
        ph.cp("act", Z[0][:, :, 64:128], B[5][:, :, 0:64], [bB[5]], [bZ[0]])
        zi = 0
        ni = 0
        for lvl in range(7):
            for i in range(4):
                ph.mm(B[0][:, i, :], Nn[ni][:, i, :], Z[zi][:, i, :], True, True, [bNn[ni], bZ[zi]], [bB[0]])
            if lvl < 6:
                for i in range(4):
                    ph.mm(B[1][:, i, :], NT[ni][:, i, :], Nn[ni][:, i, :], True, True, [bNn[ni], bNT[ni]], [bB[1]])
                    ph.mm(B[2][:, i, :], Nn[ni][:, i, :], NT[ni][:, i, :], True, True, [bNn[ni], bNT[ni]], [bB[2]])
            ph.tt("dve", Z[1 - zi][:], B[0][:], Z[zi][:], ALU.add, [bB[0], bZ[zi]], [bZ[1 - zi]])
            zi = 1 - zi
            if lvl < 6:
                ph.cp("act", Nn[1 - ni][:], B[1][:], [bB[1]], [bNn[1 - ni]])
                ph.cp("act", NT[1 - ni][:], B[2][:], [bB[2]], [bNT[1 - ni]])
                ni = 1 - ni
        Zf = Z[zi]
        bZf = bZ[zi]
        so = c % 2
        for dd in range(2):
            for h in range(2):
                i = dd * 2 + h
                hc = slice(h * 64, (h + 1) * 64)
                ph.mm(B[3][0:64, i, :], Zf[:, i, 0:64], Mbr[:, i, :], True, True, [bZf, bMbr], [bB[3]])
                ph.mm(B[4][0:64, i, 0:64], Zf[:, i, 0:64], TTs[:, 1, dd, hc], True, True, [bZf, bTT], [bB[4]])
        for dd in range(2):
            for h in range(2):
                i = dd * 2 + h
                hs = slice(h * 64, (h + 1) * 64)
                ph.tt("dve", G[hs, dd, :], B[3][0:64, i, :], X[dd][hs, 3, cs_], ALU.add, [bB[3], bX[dd][3]], [bG])
                ph.stt("dve", Phi[hs, dd, :], id64[hs, :], WC[hs, dd, c:c + 1], B[4][0:64, i, 0:64], ALU.mult, ALU.add, [bid, bWC, bB[4]], [bPhi])
        for dd in range(2):
            for h in range(2):
                i = dd * 2 + h
                hs = slice(h * 64, (h + 1) * 64)
                hc = hs
                last = (c == 0)
                ph.mm(B[5][0:64, i, 0:64], TTs[:, 1, dd, hc], Zf[:, i, 64:128], True, False, [bTT, bZf], [bB[5]])
                ph.mm(B[5][0:64, i, 0:64], TTs[:, 2, dd, hc], TTs[:, 3, dd, hc], False, last, [bTT], [bB[5]])
                if not last:
                    ph.mm(B[5][0:64, i, 0:64], Phi[hs, dd, :], ST[so][hs, dd, :], False, True, [bPhi, bST[so]], [bB[5]])
                ph.mm(B[6][0:64, i, :], Zf[:, i, 64:128], Mbr[:, i, :], True, False, [bZf, bMbr], [bB[6]])
                ph.mm(B[6][0:64, i, :], TTs[:, 3, dd, hc], Mkr[:, i, :], False, last, [bTT, bMkr], [bB[6]])
                if not last:
                    ph.mm(B[6][0:64, i, :], ST[so][hs, dd, :], G[hs, dd, :], False, True, [bST[so], bG], [bB[6]])
        yo = Yo[c % 2]
        for dd in range(2):
            for h in range(2):
                i = dd * 2 + h
                hs = slice(h * 64, (h + 1) * 64)
                ph.cp("act", ST[1 - so][hs, dd, :], B[5][0:64, i, 0:64], [bB[5]], [bST[1 - so]])
                ph.cp("dve" if h else "act", yo[hs, dd, :], B[6][0:64, i, :], [bB[6]], [bYo[c % 2]])
        for dd in range(2):
            ph.dma("pool", rw["y"][dd][:, cs_], yo[:, dd, :], [bYo[c % 2]], [], final=True, key="st")
    ph.close()


def _rwkv_final(nc, d, rw, yT):
    ph = Ph(nc)
    pp = ph.sb([128, 16], F32)
    bpp = Buf()
    ph.dma("sp", pp[:], d["rw_pp"], [], [bpp])
    bones = ph.sb([128, 128], F32)
    bbo = Buf()
    ph.dma("sp", bones[:], d["blockones"], [], [bbo])
    geps = ph.sb([128, 1], F32)
    ph.memset("pool", geps[:], GN_EPS, [bbo])
    y0 = [ph.sb([128, 512], F32) for _ in range(2)]
    y1 = [ph.sb([128, 512], F32) for _ in range(2)]
    bg = [ph.sb([128, 2, 512], BF16) for _ in range(2)]
    by = [Buf() for _ in range(2)]
    bbg = [Buf() for _ in range(2)]
    ysum = ph.sb([128, 512], F32)
    yc = ph.sb([128, 512], F32)
    sq = ph.sb([128, 512], F32)
    rstd = ph.sb([128, 512], F32)
    bys, byc, bsq, brs = Buf(), Buf(), Buf(), Buf()
    pM = ph.ps([128, 512], F32)
    pV = ph.ps([128, 512], F32)
    bpM, bpV = Buf(), Buf()
    ob = [ph.sb([128, 512], BF16) for _ in range(2)]
    bob = [Buf() for _ in range(2)]
    for tb in range(NTB):
        j = tb % 2
        tsl = slice(tb * 512, (tb + 1) * 512)
        rsl = slice((NTB - 1 - tb) * 512, (NTB - tb) * 512)
        ph.dma("sp", y0[j][:], rw["y"][0][:, tsl], [], [by[j]], key="ld")
        ph.dma("sp", y1[j][:], rw["y"][1][:, rsl], [], [by[j]], key="ld")
        ph.dma("sp", bg[j][:, 0, :], rw["bonus"][:, tsl], [], [bbg[j]], key="ld2")
        ph.dma("sp", bg[j][:, 1, :], rw["tok"][:, 4, tsl], [], [bbg[j]], key="ld2")
        ph.tt("dve", ysum[:], y0[j][:], y1[j][:, ::-1], ALU.add, [by[j]], [bys])
        ph.mm(pM[:], bones[:], ysum[:], True, True, [bbo, bys], [bpM])
        ph.stt("dve", yc[:], pM[:], -1.0 / 64, ysum[:], ALU.mult, ALU.add, [bpM, bys], [byc])
        ph.tt("pool", sq[:], yc[:], yc[:], ALU.mult, [byc], [bsq])
        ph.mm(pV[:], bones[:], sq[:], True, True, [bbo, bsq], [bpV])
        ph.rsqrt(rstd[:], pV[:], [bpV, bbo], [brs], scale=1.0 / 64, bias=geps[:])
        ph.tt("dve", yc[:], yc[:], rstd[:], ALU.mult, [byc, brs], [byc])
        ph.ts("dve", yc[:], yc[:], pp[:, 3:4], pp[:, 4:5], ALU.mult, ALU.add, [byc, bpp], [byc])
        ph.tt("pool", yc[:], yc[:], bg[j][:, 0, :], ALU.add, [byc, bbg[j]], [byc])
        ph.tt("dve", ob[j][:], yc[:], bg[j][:, 1, :], ALU.mult, [byc, bbg[j]], [bob[j]])
        ph.dma("pool", yT[0:128, tsl], ob[j][:], [bob[j]], [], final=True, key="st")
    ph.close()


TPC = 2048
NT2 = TPC // 128
NB2 = TPC // 512
NFC = DFF // 128


def _outproj_norm(nc, x, yT, d, sc, moe, sel=None):
    ph = Ph(nc)
    wo_f = [ph.sb([128, D], F32) for _ in range(2)]
    bwf = [Buf() for _ in range(2)]
    wo = ph.sb([128, KC, D], BF16)
    bwo = Buf()
    wv = d["w_out"].rearrange("(kc p) n -> kc p n", p=128)
    for kc in range(KC):
        ph.dma("sp", wo_f[kc % 2][:], wv[kc], [], [bwf[kc % 2]], key="ldw")
        ph.cp("pool" if kc % 2 else "dve", wo[:, kc, :], wo_f[kc % 2][:], [bwf[kc % 2]], [bwo])
    ident = ph.sb([128, 128], BF16)
    identf = ph.sb([128, 128], F32)
    bid = Buf()
    ph.dma("sp", identf[:], d["ident"], [], [bid])
    ph.cp("dve", ident[:], identf[:], [bid], [bid])
    eps = ph.sb([128, 1], F32)
    ph.memset("pool", eps[:], 1e-6, [bid])
    if moe:
        rg = ph.sb([128, NE, D], F32)
        gb = ph.sb([128, D], F32)
        brg = Buf()
        ph.dma("sp", gb[:], d["gffn_row"].partition_broadcast(128), [], [brg])
        for e_ in range(NE):
            ph.dma("sp", rg[:, e_, :], d["routerT"][e_:e_ + 1, :].partition_broadcast(128), [], [brg])
        ph.tt("dve", rg[:], rg[:], gb[:].unsqueeze(1).to_broadcast([128, NE, D]), ALU.mult, [brg], [brg])
        lg = ph.sb([128, NE], F32)
        l2 = ph.sb([128, NE], F32)
        m1 = ph.sb([128, 1], F32)
        m2 = ph.sb([128, 1], F32)
        k1 = ph.sb([128, NE], F32)
        k2 = ph.sb([128, NE], F32)
        g1 = ph.sb([128, 1], F32)
        g2 = ph.sb([128, 1], F32)
        comb = [ph.sb([128, NE], F32) for _ in range(2)]
        bcomb = [Buf() for _ in range(2)]
        blg = Buf()
        junkf = ph.sb([128, D], F32)
        bjf = Buf()
    yt = [ph.sb([128, KC, 128], BF16) for _ in range(2)]
    byt = [Buf() for _ in range(2)]
    xt = [ph.sb([128, D], F32) for _ in range(2)]
    bxt = [Buf() for _ in range(2)]
    x1 = [ph.sb([128, D], F32) for _ in range(2)]
    bx1 = [Buf() for _ in range(2)]
    pO = [ph.ps([128, 2, 512], F32) for _ in range(2)]
    bpO = [Buf() for _ in range(2)]
    junk = ph.sb([128, D], BF16)
    bj = Buf()
    ss = [ph.sb([128, 1], F32) for _ in range(2)]
    bss = [Buf() for _ in range(2)]
    xn = [ph.sb([128, D], BF16) for _ in range(2)]
    bxn = [Buf() for _ in range(2)]
    xnf = ph.sb([128, D], F32)
    bxnf = Buf()
    pT = [ph.ps([128, KC, 128], BF16) for _ in range(2)]
    bpT = [Buf() for _ in range(2)]
    hs = [ph.sb([128, KC, 128], BF16) for _ in range(2)]
    bhs = [Buf() for _ in range(2)]
    yv = yT.rearrange("(kc p) t -> p kc t", p=128)
    xv = x.rearrange("(t p) d -> t p d", p=128)
    if sel is not None:
        msk = ph.sb([128, 2], F32)
        bmsk = Buf()
        ph.dma("sp", msk[:], sel, [], [bmsk])
        yt2 = [ph.sb([128, KC, 128], BF16) for _ in range(2)]
        byt2 = [Buf() for _ in range(2)]
        ysel = [ph.sb([128, KC, 128], BF16) for _ in range(2)]
        bysel = [Buf() for _ in range(2)]
    x1v = sc["x1"].rearrange("(t p) d -> t p d", p=128)
    for t in range(NT2):
        j = t % 2
        ph.dma("sp", yt[j][:], yv[:, :, t * 128:(t + 1) * 128], [], [byt[j]], key="ld")
        ph.dma("sp", xt[j][:], xv[t], [], [bxt[j]], key="ld2")
        ysrc, bysrc = yt[j], byt[j]
        if sel is not None:
            ph.dma("sp", yt2[j][:], yv[:, :, TPC + t * 128:TPC + (t + 1) * 128], [], [byt2[j]], key="ld")
            ph.ts("pool", ysel[j][:], yt[j][:], msk[:, 0:1], None, ALU.mult, None, [byt[j], bmsk], [bysel[j]])
            ph.stt("dve", ysel[j][:], yt2[j][:], msk[:, 1:2], ysel[j][:], ALU.mult, ALU.add, [byt2[j], bmsk, bysel[j]], [bysel[j]])
            ysrc, bysrc = ysel[j], bysel[j]
        for hf in range(2):
            for kc in range(KC):
                ph.mm(pO[j][:, hf, :], ysrc[:, kc, :], wo[:, kc, hf * 512:(hf + 1) * 512], kc == 0, kc == KC - 1, [bysrc, bwo], [bpO[j]])
        ph.tt("dve", x1[j][:], pO[j][:].rearrange("p a b -> p (a b)"), xt[j][:], ALU.add, [bpO[j], bxt[j]], [bx1[j]])
        ph.dma("pool", x1v[t], x1[j][:], [bx1[j]], [], final=True, key="st")
        ph.act(junk[:], x1[j][:], AF.Square, [bx1[j]], [bj, bss[j]], accum=ss[j][:])
        ph.rsqrt(ss[j][:], ss[j][:], [bss[j], bid], [bss[j]], scale=1.0 / D, bias=eps[:])
        ph.ts("dve", xn[j][:], x1[j][:], ss[j][:, 0:1], None, ALU.mult, None, [bx1[j], bss[j]], [bxn[j]])
        for kc in range(KC):
            ph.tr(pT[j][:, kc, :], xn[j][:, kc * 128:(kc + 1) * 128], ident[:], [bxn[j], bid], [bpT[j]])
        ph.cp("act", hs[j][:], pT[j][:], [bpT[j]], [bhs[j]])
        ph.dma("pool", sc["hT"][:, :, t * 128:(t + 1) * 128], hs[j][:], [bhs[j]], [], final=True, key="st")
        if moe:
            ph.ts("pool", xnf[:], x1[j][:], ss[j][:, 0:1], None, ALU.mult, None, [bx1[j], bss[j]], [bxnf])
            for e_ in range(NE):
                ph.P.add("dve", lambda e, e_=e_: e.scalar_tensor_tensor(out=junkf[:], in0=xnf[:], scalar=1.0, in1=rg[:, e_, :], op0=ALU.mult, op1=ALU.mult, accum_out=lg[:, e_:e_ + 1]), [bxnf, brg], [bjf, blg])
            ph.P.add("dve", lambda e: e.reduce_max(out=m1[:], in_=lg[:], axis=AX.X), [blg], [blg])
            ph.ts("dve", k1[:], lg[:], m1[:, 0:1], None, ALU.is_equal, None, [blg], [blg])
            ph.stt("dve", l2[:], k1[:], -1e30, lg[:], ALU.mult, ALU.add, [blg], [blg])
            ph.P.add("dve", lambda e: e.reduce_max(out=m2[:], in_=l2[:], axis=AX.X), [blg], [blg])
            ph.ts("dve", k2[:], l2[:], m2[:, 0:1], None, ALU.is_equal, None, [blg], [blg])
            ph.tt("dve", g2[:], m2[:], m1[:], ALU.subtract, [blg], [blg])
            ph.act(g2[:], g2[:], AF.Sigmoid, [blg], [blg])
            ph.ts("dve", g1[:], g2[:], -1.0, 1.0, ALU.mult, ALU.add, [blg], [blg])
            ph.ts("dve", k1[:], k1[:], g1[:, 0:1], None, ALU.mult, None, [blg], [blg])
            ph.stt("dve", comb[j][:], k2[:], g2[:, 0:1], k1[:], ALU.mult, ALU.add, [blg], [bcomb[j]])
            ph.dma("pool", sc["comb"][:, t, :], comb[j][:], [bcomb[j]], [], final=True, key="st")
    ph.close()


def _ffn_experts(nc, d, sc, n_exp, moe, final_gain, out):
    ph = Ph(nc)
    FG = 4
    NG = NFC // FG
    hT = ph.sb([128, KC, TPC], BF16)
    bh = [Buf() for _ in range(KC)]
    for kc in range(KC):
        ph.dma("sp", hT[:, kc, :], sc["hT"][:, kc, :], [], [bh[kc]])
    gm = ph.sb([128, KC], F32)
    bgm = Buf()
    ph.dma("sp", gm[:], d["gffn"], [], [bgm])
    acc = ph.sb([128, NT2, D], F32)
    bacc = [Buf() for _ in range(NT2)]
    x1v = sc["x1"].rearrange("(t p) d -> p t d", p=128)
    for t in range(NT2):
        ph.dma("sp", acc[:, t, :], x1v[:, t, :], [], [bacc[t]])
    if moe:
        comb = ph.sb([128, NT2, NE], F32)
        bcomb = Buf()
        ph.dma("sp", comb[:], sc["comb"], [], [bcomb])
    wgs = [ph.sb([128, KC, 2, 128], F32) for _ in range(2)]
    bwgs = [Buf() for _ in range(2)]
    wgb = [ph.sb([128, KC, 2, 128], BF16) for _ in range(2)]
    bwgb = [Buf() for _ in range(2)]
    wds = ph.sb([128, FG, D], F32)
    bwds = Buf()
    wdg = [ph.sb([128, FG, D], BF16) for _ in range(2)]
    bwdg = [Buf() for _ in range(2)]
    aTg = [ph.sb([128, FG, TPC], BF16) for _ in range(2)]
    baTg = [[Buf() for _ in range(FG)] for _ in range(2)]
    pG = [ph.ps([128, 512], F32) for _ in range(2)]
    pU = [ph.ps([128, 512], F32) for _ in range(2)]
    bpG = [Buf() for _ in range(2)]
    bpU = [Buf() for _ in range(2)]
    pD = [ph.ps([128, 2, 512], F32) for _ in range(2)]
    bpD = [Buf() for _ in range(2)]
    sg = [ph.sb([128, 512], F32) for _ in range(2)]
    bsg = [Buf() for _ in range(2)]
    it = 0
    gi = 0
    di = 0
    wi = 0
    pending = []
    for e_ in range(n_exp):
        wd_v = d["w_down"][e_].rearrange("(g f p) n -> g p f n", p=128, f=FG)
        for g in range(NG):
            gb_ = gi % 2
            gi += 1
            ph.dma("sp", wds[:], wd_v[g], [], [bwds])
            ph.cp("pool", wdg[gb_][:], wds[:], [bwds], [bwdg[gb_]])
            for f in range(FG):
                fc = g * FG + f
                j = wi % 2
                wi += 1
                ph.dma("sp", wgs[j][:], d["w_gu"][e_, fc], [], [bwgs[j]])
                ph.tt("pool", wgb[j][:].rearrange("p k a n -> p k (a n)"), wgs[j][:].rearrange("p k a n -> p k (a n)"),
                      gm[:].unsqueeze(2).to_broadcast([128, KC, 256]), ALU.mult, [bwgs[j], bgm], [bwgb[j]])
                for tb in range(NB2):
                    a = it % 2
                    it += 1
                    tsl = slice(tb * 512, (tb + 1) * 512)
                    for kc in range(KC):
                        ph.mm(pG[a][:], wgb[j][:, kc, 0, :], hT[:, kc, tsl], kc == 0, kc == KC - 1, [bwgb[j], bh[kc]], [bpG[a]])
                    for kc in range(KC):
                        ph.mm(pU[a][:], wgb[j][:, kc, 1, :], hT[:, kc, tsl], kc == 0, kc == KC - 1, [bwgb[j], bh[kc]], [bpU[a]])
                    ph.act(sg[a][:], pG[a][:], AF.Silu, [bpG[a]], [bsg[a]])
                    ph.tt("dve", aTg[gb_][:, f, tsl], pU[a][:], sg[a][:], ALU.mult, [bpU[a], bsg[a]], [baTg[gb_][f]])
                if f == 0:
                    for fn in pending:
                        fn()
                    del pending[:]
            def down(gb_=gb_, e_=e_):
                nonlocal di
                for t in range(NT2):
                    k = di % 2
                    di += 1
                    for hf in range(2):
                        for f in range(FG):
                            ph.mm(pD[k][:, hf, :], aTg[gb_][:, f, t * 128:(t + 1) * 128], wdg[gb_][:, f, hf * 512:(hf + 1) * 512],
                                  f == 0, f == FG - 1, [baTg[gb_][f], bwdg[gb_]], [bpD[k]])
                    pflat = pD[k][:].rearrange("p a b -> p (a b)")
                    if moe:
                        ph.stt("dve", acc[:, t, :], pflat, comb[:, t, e_:e_ + 1], acc[:, t, :], ALU.mult, ALU.add, [bpD[k], bcomb, bacc[t]], [bacc[t]])
                    else:
                        ph.tt("dve", acc[:, t, :], pflat, acc[:, t, :], ALU.add, [bpD[k], bacc[t]], [bacc[t]])
            pending.append(down)
    for fn in pending:
        fn()
    del pending[:]
    ov = out.rearrange("(t p) d -> t p d", p=128)
    if final_gain is None:
        for t in range(NT2):
            ph.dma("pool", ov[t], acc[:, t, :], [bacc[t]], [], final=True)
    else:
        gb = wds[:, 0, :]
        bgb = bwds
        ph.dma("sp", gb, final_gain.partition_broadcast(128), [], [bgb])
        eps = ph.sb([128, 1], F32)
        beps_ = Buf()
        ph.memset("pool", eps[:], 1e-6, [beps_])
        ss = ph.sb([128, NT2], F32)
        bss = Buf()
        junk = aTg[0][:, 0, 0:D]
        bj = baTg[0][0]
        for t in range(NT2):
            ph.act(junk, acc[:, t, :], AF.Square, [bacc[t]], [bj, bss], accum=ss[:, t:t + 1])
        ph.rsqrt(ss[:], ss[:], [bss, beps_], [bss], scale=1.0 / D, bias=eps[:])
        for t in range(NT2):
            ph.stt("dve", acc[:, t, :], acc[:, t, :], ss[:, t:t + 1], gb, ALU.mult, ALU.mult, [bacc[t], bss, bgb], [bacc[t]])
            ph.dma("pool", ov[t], acc[:, t, :], [bacc[t]], [], final=True)
    ph.close()


def _lam_init(layer):
    import math
    return 0.8 - 0.6 * math.exp(-0.3 * layer)


MIXER_INPUTS = {
    "x": ([S, D], F32), "gmix": ([128, KC], F32), "att_pp": ([128, 8], F32), "lam_q": ([1, 128], F32),
    "lam_k": ([1, 128], F32), "pos": ([1, S], I32), "w_att": ([D, 768], F32), "w_att_sw": ([D, 512], F32),
    "pool_pp": ([128, 72], F32), "w_pool": ([D, 128], F32), "pool_mix_bd": ([128, 128], F32),
    "mu": ([2, 640], F32), "w_rwkv": ([D, 640], F32), "gate_up": ([128, 128], F32),
    "rw_pp": ([128, 16], F32), "blockones": ([128, 128], F32), "lora": ([4, 128, 128], F32),
    "masks": ([3, 128, 128], F32), "ident": ([128, 128], F32), "ident64x2": ([128, 64], F32),
}
MIXER_INPUTS_L1 = {"w_vdown": ([D, 32], F32), "vres_up": ([32, 128], F32), "vres_bias": ([128, 1], F32),
                   "vfirst_in": ([128, S], F32)}


def _mixer_body(nc, layer, d, x, yT, vfirst, stages=None, rowmap=None):
    def scr(name, shape, dt):
        return nc.dram_tensor(f"scr{layer}_{name}", shape, dt, kind="Internal").ap()
    hT_dram = scr("hT", [128, KC, S], BF16)
    rw = {"tok": scr("tok", [128, 5, S], BF16), "bonus": scr("bonus", [128, S], BF16),
          "sc": [scr(f"sc{i}", [128, QN, S], BF16) for i in range(2)],
          "wc": [scr(f"wc{i}", [128, S // 128], F32) for i in range(2)],
          "y": [scr(f"y{i}", [128, S], F32) for i in range(2)], "vfirst": vfirst}
    _norm_to_hT(nc, x, None, hT_dram, rowmap)
    if stages is None or "att" in stages:
        _attention(nc, hT_dram, d, _lam_init(layer), yT)
    if stages is None or "pool" in stages:
        _pool_mixer(nc, hT_dram, d, yT)
    if stages is None or "rwkv" in stages or "rwkv1" in stages:
        _rwkv_proj(nc, hT_dram, d, layer, rw)
    if stages is None or "rwkv" in stages or "rwkv2" in stages:
        _rwkv_derive(nc, d, rw)
    if stages is None or "rwkv" in stages or "rwkv3" in stages:
        _rwkv_scan(nc, d, rw)
    if stages is None or "rwkv" in stages or "rwkv4" in stages:
        _rwkv_final(nc, d, rw, yT)
    return rw


def build_mixer(layer, stages=None):
    nc = bass.Bass("TRN2", target_bir_lowering=False)
    d = {}
    spec = dict(MIXER_INPUTS)
    if layer > 0:
        spec.update(MIXER_INPUTS_L1)
    for k, (shp, dt) in spec.items():
        d[k] = nc.dram_tensor(k, shp, dt, kind="ExternalInput").ap()
    yT = nc.dram_tensor("yT", [512, S], BF16, kind="ExternalOutput").ap()
    if layer == 0:
        vfirst = nc.dram_tensor("vfirst", [128, S], F32, kind="ExternalOutput").ap()
    else:
        vfirst = d["vfirst_in"]
    _mixer_body(nc, layer, d, d["x"], yT, vfirst, stages)
    return nc


def build_ffn(layer):
    moe = (layer % 2 == 1)
    last = (layer == 1)
    n_exp = NE if moe else 1
    nc = bass.Bass("TRN2", target_bir_lowering=False)
    d = {}
    spec = {"x": ([TPC, D], F32), "yT": ([D, TPC], BF16), "w_out": ([D, D], F32), "ident": ([128, 128], F32),
            "gffn": ([128, KC], F32), "w_gu": ([n_exp, NFC, 128, KC, 2, 128], F32),
            "w_down": ([n_exp, DFF, D], F32)}
    if moe:
        spec.update({"gffn_row": ([1, D], F32), "routerT": ([NE, D], F32)})
    if last:
        spec["gout_row"] = ([1, D], F32)
    for k, (shp, dt) in spec.items():
        d[k] = nc.dram_tensor(k, shp, dt, kind="ExternalInput").ap()
    out = nc.dram_tensor("out", [TPC, D], F32, kind="ExternalOutput").ap()
    sc = {"x1": nc.dram_tensor("s_x1", [TPC, D], F32, kind="Internal").ap(),
          "hT": nc.dram_tensor("s_hT", [128, KC, TPC], BF16, kind="Internal").ap(),
          "aT": nc.dram_tensor("s_aT", [128, NFC, TPC], BF16, kind="Internal").ap()}
    if moe:
        sc["comb"] = nc.dram_tensor("s_comb", [128, NT2, NE], F32, kind="Internal").ap()
    _outproj_norm(nc, d["x"], d["yT"], d, sc, moe)
    _ffn_experts(nc, d, sc, n_exp, moe, d["gout_row"] if last else None, out)
    return nc


def _consts():
    c = {}
    c["blockones"] = np.kron(np.eye(2, dtype=np.float32), np.ones((64, 64), np.float32))
    c["masks"] = np.stack([np.triu(np.ones((128, 128), np.float32), 1), np.tril(np.ones((128, 128), np.float32), -1),
                           np.triu(np.ones((128, 128), np.float32), 0)])
    c["ident"] = np.eye(128, dtype=np.float32)
    c["ident64x2"] = np.concatenate([np.eye(64, dtype=np.float32)] * 2, axis=0)
    return c


def _colmajor(v):
    return np.ascontiguousarray(v.reshape(KC, 128).T)


def _mixer_inputs(layer, b, hh, inp, x_b, consts, vfirst=None):
    f32 = np.float32
    l = layer
    w_in = inp["w_in_first"] if l == 0 else inp["w_in_rest"][l - 1]
    o_q = 1024 if l == 0 else 1056
    o_k, o_v, o_p = o_q + 512, o_q + 1024, o_q + 1536
    hs = [2 * hh, 2 * hh + 1]
    m = dict(consts)
    m["x"] = x_b
    m["gmix"] = _colmajor(inp["norm_mix"][l])
    m["pos"] = np.ascontiguousarray(inp["positions"][b:b + 1].astype(np.int32))
    qc = np.concatenate([np.arange(o_q + h * 128, o_q + (h + 1) * 128) for h in hs])
    kc = np.concatenate([np.arange(o_k + h * 128, o_k + (h + 1) * 128) for h in hs])
    vc = np.concatenate([np.arange(o_v + h * 128, o_v + (h + 1) * 128) for h in hs])
    m["w_att"] = np.ascontiguousarray(w_in[:, np.concatenate([qc, kc, vc])])
    perm = np.arange(64)
    perm[0:8] = np.arange(8, 16)
    perm[8:16] = np.arange(0, 8)
    perm512 = np.concatenate([blk * 64 + perm for blk in range(8)])
    m["w_att_sw"] = np.ascontiguousarray(w_in[:, np.concatenate([qc, kc])][:, perm512])
    half = 8
    inv_freq = np.power(f32(500000.0), -(np.arange(half, dtype=f32) * f32(2.0) / f32(16)))
    pp = np.zeros((128, 8), f32)
    for p in range(128):
        dloc = p % 64
        if dloc < 16:
            pp[p, 0] = inv_freq[dloc % 8]
            pp[p, 1] = -1.0 if dloc < 8 else 1.0
    pp[:, 2] = inp["subln_w"][l]
    m["att_pp"] = pp
    m["lam_q"] = np.ascontiguousarray(inp["lambda_q"][l].reshape(1, 128))
    m["lam_k"] = np.ascontiguousarray(inp["lambda_k"][l].reshape(1, 128))
    gs = hs
    m["w_pool"] = np.ascontiguousarray(w_in[:, o_p + gs[0] * 64:o_p + (gs[1] + 1) * 64])
    pmb = np.zeros((128, 128), f32)
    for i, g in enumerate(gs):
        pmb[i * 64:(i + 1) * 64, i * 64:(i + 1) * 64] = inp["pool_mix"][l, g]
    m["pool_mix_bd"] = pmb
    ppp = np.zeros((128, 72), f32)
    wins = (2, 4, 8, 16)
    for i, g in enumerate(gs):
        rows = slice(i * 64, (i + 1) * 64)
        w = wins[g]
        ppp[rows, g] = 1.0 / w
        for half_ in range(2):
            for cidx in range(8):
                t = cidx if half_ == 0 else S - 8 + cidx
                lo = min(max(t - w // 2, 0), S)
                hi = min(max(t + (w - w // 2), 0), S)
                ppp[rows, 8 + g * 16 + half_ * 8 + cidx] = 1.0 / (hi - lo)
    ppp[:, 4] = inp["pool_scale"][l, gs[0] * 64:(gs[1] + 1) * 64]
    m["pool_pp"] = ppp
    rc = np.arange(hh * 128, (hh + 1) * 128)
    cols = np.concatenate([rc, 256 + rc, 512 + rc, np.arange(768, 896), np.arange(896, 1024)])
    m["w_rwkv"] = np.ascontiguousarray(w_in[:, cols])
    m["mu"] = np.ascontiguousarray(inp["tshift"][l][:, cols])
    m["gate_up"] = np.ascontiguousarray(inp["gate_up"][l][:, rc])
    rp = np.zeros((128, 16), f32)
    rp[:, 0] = inp["k_k"][l, rc]
    rp[:, 1] = inp["k_a"][l, rc]
    rp[:, 2] = inp["r_k"][l].reshape(256)[rc]
    rp[:, 3] = inp["lnx_w"][l, rc]
    rp[:, 4] = inp["lnx_b"][l, rc]
    rp[:, 5] = inp["decay_bias"][l, 0, rc]
    rp[:, 6] = inp["decay_bias"][l, 1, rc]
    rp[:, 7] = inp["iclr_bias"][l, 0, rc]
    rp[:, 8] = inp["iclr_bias"][l, 1, rc]
    m["rw_pp"] = rp
    lora = np.zeros((4, 128, 128), f32)
    for dd in range(2):
        lora[dd, 0:64, :] = inp["decay_up"][l, dd][:, rc]
        lora[2 + dd, 64:128, :] = inp["iclr_up"][l, dd][:, rc]
    m["lora"] = lora
    if l > 0:
        m["w_vdown"] = np.ascontiguousarray(w_in[:, 1024:1056])
        m["vres_up"] = np.ascontiguousarray(inp["vres_up"][l - 1][:, rc])
        m["vres_bias"] = np.ascontiguousarray(inp["vres_bias"][l - 1][rc].reshape(128, 1))
        m["vfirst_in"] = vfirst
    return m


def _wout_rows(gathered=False):
    per = []
    for hh in range(2):
        rows = list(range(hh * 128, (hh + 1) * 128))
        rows += list(range(256 + hh * 256, 256 + (hh + 1) * 256))
        rows += list(range(768 + hh * 128, 768 + (hh + 1) * 128))
        per.append(rows)
    if not gathered:
        return np.array(per[0] + per[1])
    out = []
    for k in range(2):
        for r in range(2):
            out += per[r][k * 256:(k + 1) * 256]
    return np.array(out)


def _ffn_inputs(layer, inp, x_tok, yT_tok, consts, gathered=False):
    l = layer
    m = {"x": x_tok, "yT": yT_tok, "ident": consts["ident"]}
    m["w_out"] = np.ascontiguousarray(inp["w_out"][l][_wout_rows(gathered)])
    m["gffn"] = _colmajor(inp["norm_ffn"][l])
    def gu(wg, wu):
        E = wg.shape[0]
        st = np.stack([wg.reshape(E, KC, 128, NFC, 128), wu.reshape(E, KC, 128, NFC, 128)], axis=0)
        return np.ascontiguousarray(st.transpose(1, 4, 3, 2, 0, 5))
    if l % 2 == 0:
        i = l // 2
        m["w_gu"] = gu(inp["ffn_gate"][i:i + 1], inp["ffn_up"][i:i + 1])
        m["w_down"] = inp["ffn_down"][i:i + 1]
    else:
        i = l // 2
        m["w_gu"] = gu(inp["exp_gate"][i], inp["exp_up"][i])
        m["w_down"] = inp["exp_down"][i]
        m["gffn_row"] = np.ascontiguousarray(inp["norm_ffn"][l].reshape(1, D))
        m["routerT"] = np.ascontiguousarray(inp["router"][i].T)
    if l == 1:
        m["gout_row"] = np.ascontiguousarray(inp["norm_out"].reshape(1, D))
    return m


GROUPS = [[0, 1], [2, 3], [4, 5], [6, 7]]


def _ffn_spec(layer):
    moe = (layer % 2 == 1)
    n_exp = NE if moe else 1
    spec = {"w_out": ([D, D], F32), "ident": ([128, 128], F32), "gffn": ([128, KC], F32),
            "w_gu": ([n_exp, NFC, 128, KC, 2, 128], F32), "w_down": ([n_exp, DFF, D], F32)}
    if moe:
        spec.update({"gffn_row": ([1, D], F32), "routerT": ([NE, D], F32)})
    if layer == 1:
        spec["gout_row"] = ([1, D], F32)
    return spec


def build_fused():
    nc = bass.Bass("TRN2", target_bir_lowering=False)

    def inp(name, shp, dt):
        return nc.dram_tensor(name, shp, dt, kind="ExternalInput").ap()
    x_full = inp("x_full", [S, D], F32)
    x_tok = inp("x_tok", [TPC, D], F32)
    sel = inp("sel", [128, 2], F32)
    out = nc.dram_tensor("out", [TPC, D], F32, kind="ExternalOutput").ap()
    vfirst = nc.dram_tensor("vfirst_s", [128, S], F32, kind="Internal").ap()
    cur_full, cur_tok = x_full, x_tok
    cur_map = None
    for l in range(2):
        spec = dict(MIXER_INPUTS)
        del spec["x"]
        if l > 0:
            spec.update({k: v for k, v in MIXER_INPUTS_L1.items() if k != "vfirst_in"})
        d = {k: inp(f"m{l}_{k}", shp, dt) for k, (shp, dt) in spec.items()}
        yT_t = nc.dram_tensor(f"yT_{l}", [512, S], BF16)
        yTall_t = nc.dram_tensor(f"yTall_{l}", [1024, S], BF16)
        _mixer_body(nc, l, d, cur_full, yT_t.ap(), vfirst, rowmap=cur_map)
        ph = Ph(nc)
        ph.allgather(yTall_t, yT_t, GROUPS, 256)
        ph.close()
        moe = (l % 2 == 1)
        fd = {k: inp(f"f{l}_{k}", shp, dt) for k, (shp, dt) in _ffn_spec(l).items()}
        sc = {"x1": nc.dram_tensor(f"s{l}_x1", [TPC, D], F32, kind="Internal").ap(),
              "hT": nc.dram_tensor(f"s{l}_hT", [128, KC, TPC], BF16, kind="Internal").ap(),
              "aT": nc.dram_tensor(f"s{l}_aT", [128, NFC, TPC], BF16, kind="Internal").ap()}
        if moe:
            sc["comb"] = nc.dram_tensor(f"s{l}_comb", [128, NT2, NE], F32, kind="Internal").ap()
        _outproj_norm(nc, cur_tok, yTall_t.ap(), fd, sc, moe, sel=sel)
        if l == 1:
            _ffn_experts(nc, fd, sc, NE if moe else 1, moe, fd["gout_row"], out)
        else:
            x2h_t = nc.dram_tensor(f"x2h_{l}", [TPC, D], F32)
            x2f_t = nc.dram_tensor(f"x2f_{l}", [S, D], F32)
            _ffn_experts(nc, fd, sc, NE if moe else 1, moe, None, x2h_t.ap())
            ph = Ph(nc)
            ph.allgather(x2f_t, x2h_t, GROUPS, 512)
            ph.close()
            cur_full, cur_tok = x2f_t.ap(), x2h_t.ap()
            cur_map = lambda t: ((t % 16) // 4) * 1024 + (t // 16) * 512 + (t % 4) * 128
    return nc


def kernel(**inp):
    inp = {k: np.asarray(v) for k, v in inp.items()}
    consts = _consts()
    x = np.ascontiguousarray(inp["x"], dtype=np.float32)
    nc = build_fused()
    in_maps = []
    for c in range(8):
        b, hh = c // 2, c % 2
        tsl = slice(hh * TPC, (hh + 1) * TPC)
        m = {"x_full": np.ascontiguousarray(x[b]), "x_tok": np.ascontiguousarray(x[b, tsl])}
        selv = np.zeros((128, 2), np.float32)
        selv[:, hh] = 1.0
        m["sel"] = selv
        for l in range(2):
            mi = _mixer_inputs(l, b, hh, inp, None, consts, None)
            for k, v in mi.items():
                if k in ("x", "vfirst_in"):
                    continue
                m[f"m{l}_{k}"] = v
            fi = _ffn_inputs(l, inp, None, None, consts, gathered=True)
            for k, v in fi.items():
                if k in ("x", "yT"):
                    continue
                m[f"f{l}_{k}"] = v
        in_maps.append(m)
    res = run_bass_kernel_spmd(nc, in_maps, core_ids=list(range(8))).results
    outp = np.empty_like(x)
    for c in range(8):
        b, hh = c // 2, c % 2
        outp[b, hh * TPC:(hh + 1) * TPC] = np.asarray(res[c]["out"])
    return outp
```

```python
import numpy as np
import concourse.bass as bass
import concourse.mybir as mybir
from concourse.bass_utils import run_bass_kernel_spmd

F32 = mybir.dt.float32
BF16 = mybir.dt.bfloat16
I32 = mybir.dt.int32
AF = mybir.ActivationFunctionType
ALU = mybir.AluOpType
AX = mybir.AxisListType

SAME_ENG_SYNC = True
ENGS = ["pe", "act", "dve", "pool", "sp"]


class Buf:
    __slots__ = ("name", "w", "rs", "uid", "dram")
    _n = [0]

    def __init__(self, name="", dram=False):
        self.name = name
        self.w = None
        self.rs = []
        Buf._n[0] += 1
        self.uid = Buf._n[0]
        self.dram = dram


class Op:
    __slots__ = ("eng", "fn", "deps", "semkey", "val", "signal", "dma", "force", "cc")


class Prog:
    _uid = [0]

    def __init__(self):
        self.ops = {e: [] for e in ENGS}

    def add(self, eng, fn, reads=(), writes=(), dma=False, semkey=None):
        op = Op()
        op.eng = eng
        op.fn = fn
        op.deps = []
        op.signal = False
        op.val = None
        op.dma = dma
        op.force = []
        op.cc = False
        op.semkey = semkey or (eng + ("_dma" if dma else ""))
        for b in reads:
            if b.w is not None:
                op.deps.append(b.w)
            b.rs.append(op)
        for b in writes:
            if b.w is not None and b.w is not op:
                op.deps.append(b.w)
            op.deps.extend(r for r in b.rs if r is not op)
            b.w = op
            b.rs = []
        self.ops[eng].append(op)
        return op

    @staticmethod
    def _needs_sync(op, d):
        if d.eng != op.eng:
            return True
        if d in op.force:
            return True
        if op.dma or d.dma:
            return True
        if op.eng == "pe":
            return False
        return SAME_ENG_SYNC

    def emit(self, nc, final_waits=()):
        from contextlib import ExitStack
        for e in ENGS:
            for op in self.ops[e]:
                for d in op.deps:
                    if self._needs_sync(op, d):
                        d.signal = True
        for op in final_waits:
            op.signal = True
        last = {}
        for e in ENGS:
            for op in self.ops[e]:
                last[op.semkey] = op
        for op in last.values():
            op.signal = True
        cnt = {}
        for e in ENGS:
            for op in self.ops[e]:
                if op.signal:
                    cnt[op.semkey] = cnt.get(op.semkey, 0) + (16 if (op.dma and not op.cc) else 1)
                    op.val = cnt[op.semkey]
        with ExitStack() as st:
            Prog._uid[0] += 1
            sems = {k: nc.alloc_semaphore(name=f"s{Prog._uid[0]}_" + k) for k in sorted(cnt)}
            block = st.enter_context(nc.Block())

            def run_engine(e, engobj):
                seen = {}
                for op in self.ops[e]:
                    need = {}
                    for d in op.deps:
                        if self._needs_sync(op, d):
                            if need.get(d.semkey, 0) < d.val:
                                need[d.semkey] = d.val
                    for k, v in need.items():
                        if seen.get(k, 0) < v:
                            engobj.wait_ge(sems[k], v)
                            seen[k] = v
                    ins = op.fn(engobj)
                    if op.signal:
                        if op.cc:
                            ins.then_inc(sems[op.semkey])
                        else:
                            ins.then_inc(sems[op.semkey], 16 if op.dma else 1)
                for k in sorted(cnt):
                    if seen.get(k, 0) < cnt[k]:
                        engobj.wait_ge(sems[k], cnt[k])

            block.tensor(lambda eng: run_engine("pe", eng))
            block.scalar(lambda eng: run_engine("act", eng))
            block.vector(lambda eng: run_engine("dve", eng))
            block.gpsimd(lambda eng: run_engine("pool", eng))
            block.sync(lambda eng: run_engine("sp", eng))


class Ph:
    def __init__(self, nc):
        from contextlib import ExitStack
        self.nc = nc
        self.cm = nc.cleanup_on_exit()
        self.cm.__enter__()
        self.st = ExitStack()
        self.P = Prog()
        self.fin = []
        self.n = 0

    _uid = [0]

    def sb(self, shape, dt, name=None):
        Ph._uid[0] += 1
        return self.st.enter_context(self.nc.sbuf_tensor(f"{name or 't'}_{Ph._uid[0]}", list(shape), dt))

    def ps(self, shape, dt, name=None):
        Ph._uid[0] += 1
        return self.st.enter_context(self.nc.psum_tensor(f"{name or 'p'}_{Ph._uid[0]}", list(shape), dt))

    def close(self):
        self.P.emit(self.nc, final_waits=self.fin)
        self.st.close()
        self.cm.__exit__(None, None, None)

    def mm(self, out, lhsT, rhs, start, stop, r, w):
        op = self.P.add("pe", lambda e: e.matmul(out, lhsT=lhsT, rhs=rhs, start=start, stop=stop), r, w)
        rng = (lhsT.base_partition(), lhsT.base_partition() + lhsT.shape[0])
        prev = getattr(self, "_pe_prev", None)
        if prev is not None and (prev[1][1] <= rng[0] or rng[1] <= prev[1][0]):
            op.deps.append(prev[0])
            op.force.append(prev[0])
        self._pe_prev = (op, rng)
        return op

    def tr(self, out, in_, ident, r, w):
        op = self.P.add("pe", lambda e: e.transpose(out, in_, ident), r, w)
        self._pe_prev = (op, (in_.base_partition(), in_.base_partition() + in_.shape[0]))
        return op

    def act(self, out, in_, func, r, w, bias=None, scale=1.0, accum=None):
        def f(e):
            kw = {}
            if bias is not None:
                kw["bias"] = bias
            if accum is not None:
                kw["accum_out"] = accum
            return e.activation(out=out, in_=in_, func=func, scale=scale, **kw)
        return self.P.add("act", f, r, w)

    def tt(self, eng, out, in0, in1, op, r, w):
        return self.P.add(eng, lambda e: e.tensor_tensor(out=out, in0=in0, in1=in1, op=op), r, w)

    def ts(self, eng, out, in0, s1, s2, op0, op1, r, w):
        if s2 is None:
            return self.P.add(eng, lambda e: e.tensor_scalar(out=out, in0=in0, scalar1=s1, scalar2=None, op0=op0), r, w)
        return self.P.add(eng, lambda e: e.tensor_scalar(out=out, in0=in0, scalar1=s1, scalar2=s2, op0=op0, op1=op1), r, w)

    def stt(self, eng, out, in0, scalar, in1, op0, op1, r, w):
        return self.P.add(eng, lambda e: e.scalar_tensor_tensor(out=out, in0=in0, scalar=scalar, in1=in1, op0=op0, op1=op1), r, w)

    def cp(self, eng, out, in_, r, w):
        if eng == "act":
            return self.P.add("act", lambda e: e.copy(out=out, in_=in_), r, w)
        return self.P.add(eng, lambda e: e.tensor_copy(out=out, in_=in_), r, w)

    def memset(self, eng, ap, val, w):
        return self.P.add(eng, lambda e: e.memset(ap, val), (), w)

    def recip(self, out, in_, r, w):
        return self.P.add("dve", lambda e: e.reciprocal(out=out, in_=in_), r, w)

    def scan(self, out, d0, d1, r, w):
        return self.P.add("dve", lambda e: e.tensor_tensor_scan(out=out, data0=d0, data1=d1, initial=0.0, op0=ALU.mult, op1=ALU.add), r, w)

    def dma(self, eng, out, in_, r, w, final=False, key=None):
        slot = None
        for b in list(w) + list(r):
            if not b.dram:
                slot = b
                break
        key = f"{eng}_q{slot.uid}" if slot is not None else f"{eng}_{key or 'x'}"
        op = self.P.add(eng, lambda e: e.dma_start(out=out, in_=in_), r, w, dma=True, semkey=key)
        if final:
            self.fin.append(op)
        return op

    def allgather(self, out_t, in_t, groups, rows_per_chunk):
        nrows = in_t.shape[0]
        RC = rows_per_chunk
        for k in range(nrows // RC):
            Ph._uid[0] += 1
            i_ap = in_t.ap()[k * RC:(k + 1) * RC, :].opt()
            o_ap = out_t.ap()[2 * k * RC:2 * (k + 1) * RC, :].opt()
            op = self.P.add("pool", lambda e, i_ap=i_ap, o_ap=o_ap: e.collective_compute(
                "AllGather", ALU.bypass, replica_groups=groups, ins=[i_ap], outs=[o_ap]),
                (), (), dma=True, semkey=f"pool_cc{Ph._uid[0]}")
            op.cc = True
            self.fin.append(op)

    def rsqrt(self, out, in_, r, w, scale=1.0, bias=None):
        self.act(out, in_, AF.Ln, r, w, bias=bias, scale=scale)
        return self.act(out, out, AF.Exp, w, w, scale=-0.5)


S = 4096
D = 1024
KC = 8
NTB = 8
NTT = 32
DFF = 3584
NE = 8
WDS = 0.6065306597126334
GN_EPS = 64e-5
HP = S + 2


def _norm_to_hT(nc, x, gdummy, hT_dram, rowmap=None):
    ph = Ph(nc)
    xt = [ph.sb([128, D], F32) for _ in range(3)]
    bx = [Buf() for _ in range(3)]
    junk = ph.sb([128, D], BF16)
    bj = Buf()
    ss = ph.sb([128, NTT], F32)
    bss = Buf()
    eps = ph.sb([128, 1], F32)
    beps = Buf()
    ident = ph.sb([128, 128], BF16)
    identf = ph.sb([128, 128], F32)
    bid = Buf()
    xn = [ph.sb([128, D], BF16) for _ in range(2)]
    bxn = [Buf() for _ in range(2)]
    pT = [ph.ps([128, KC, 128], BF16) for _ in range(2)]
    bpT = [Buf() for _ in range(2)]
    hs = [ph.sb([128, KC, 512], BF16) for _ in range(2)]
    bhs = [Buf() for _ in range(2)]
    zc = ph.sb([128, KC, 1], BF16)
    bz = Buf()
    ph.memset("pool", eps[:], 1e-6, [beps])
    ph.memset("pool", zc[:], 0.0, [bz])
    ph.memset("pool", identf[:], 1.0, [bid])
    ph.P.add("pool", lambda e: e.affine_select(out=identf[:], in_=identf[:], pattern=[[-1, 128]], compare_op=ALU.is_equal, fill=0.0, base=0, channel_multiplier=1), [bid], [bid])
    ph.cp("dve", ident[:], identf[:], [bid], [bid])
    class _XV:
        def __getitem__(self, t):
            r0 = t * 128 if rowmap is None else rowmap(t)
            return x[r0:r0 + 128, :]
    xv = _XV()
    for t in range(NTT):
        i = t % 3
        ph.dma("sp", xt[i][:], xv[t], [], [bx[i]], key="ld")
        ph.act(junk[:], xt[i][:], AF.Square, [bx[i]], [bj, bss], accum=ss[:, t:t + 1])
    ph.rsqrt(ss[:], ss[:], [bss, beps], [bss], scale=1.0 / D, bias=eps[:])
    for t in range(NTT):
        i = t % 3
        j = t % 2
        ph.dma("sp", xt[i][:], xv[t], [], [bx[i]], key="ld")
        ph.ts("dve", xn[j][:], xt[i][:], ss[:, t:t + 1], None, ALU.mult, None, [bx[i], bss], [bxn[j]])
        for kc in range(KC):
            ph.tr(pT[j][:, kc, :], xn[j][:, kc * 128:(kc + 1) * 128], ident[:], [bxn[j], bid], [bpT[j]])
        g = (t // 4) % 2
        ph.cp("act", hs[g][:, :, (t % 4) * 128:(t % 4 + 1) * 128], pT[j][:], [bpT[j]], [bhs[g]])
        if t % 4 == 3:
            tb = t // 4
            ph.dma("pool", hT_dram[:, :, tb * 512:(tb + 1) * 512], hs[g][:], [bhs[g]], [], final=True, key="st")
    ph.close()


def _load_hT(ph, hT_dram):
    hT = ph.sb([128, KC, HP], BF16, name="hT")
    bh = [Buf() for _ in range(KC)]
    for kc in range(KC):
        ph.memset("pool", hT[:, kc, 0:1], 0.0, [bh[kc]])
        ph.memset("pool", hT[:, kc, HP - 1:HP], 0.0, [bh[kc]])
        ph.dma("sp" if kc % 2 == 0 else "act", hT[:, kc, 1:1 + S], hT_dram[:, kc, :], [], [bh[kc]])
    return hT, bh


def _prep_w(ph, w_dram, ncols, gm, bgm, name, colvec=None, bcol=None, dst=None):
    wb = dst if dst is not None else ph.sb([128, KC, ncols], BF16, name=name)
    bw = [Buf() for _ in range(KC)]
    stg = [ph.sb([128, ncols], F32, name=f"{name}_s{i}") for i in range(2)]
    bs = [Buf() for _ in range(2)]
    wv = w_dram.rearrange("(kc p) n -> kc p n", p=128)
    for kc in range(KC):
        i = kc % 2
        ph.dma("sp", stg[i][:], wv[kc], [], [bs[i]], key="ldw")
        if colvec is None:
            ph.ts("pool" if kc % 2 else "dve", wb[:, kc, :], stg[i][:], gm[:, kc:kc + 1], None, ALU.mult, None, [bs[i], bgm], [bw[kc]])
        else:
            ph.stt("dve", wb[:, kc, :], stg[i][:], gm[:, kc:kc + 1], colvec, ALU.mult, ALU.mult, [bs[i], bgm, bcol], [bw[kc]])
    return wb, bw


def _attention(nc, hT_dram, d, lam_init, yT):
    ph = Ph(nc)
    hT, bh = _load_hT(ph, hT_dram)
    gm = ph.sb([128, KC], F32)
    bgm = Buf()
    ph.dma("sp", gm[:], d["gmix"], [], [bgm])
    pp = ph.sb([128, 8], F32)
    bpp = Buf()
    ph.dma("sp", pp[:], d["att_pp"], [], [bpp])
    lqk = ph.sb([128, 2, 128], F32)
    blq = Buf()
    ph.dma("sp", lqk[:, 0, :], d["lam_q"].partition_broadcast(128), [], [blq])
    ph.dma("sp", lqk[:, 1, :], d["lam_k"].partition_broadcast(128), [], [blq])
    lprod = ph.sb([128, 128], F32)
    lsum = ph.sb([128, 2], F32)
    neglam = ph.sb([128, 1], F32)
    sw = ph.sb([128, 1], F32)
    bl = Buf()
    ph.tt("dve", lprod[:], lqk[:, 0, :], lqk[:, 1, :], ALU.mult, [blq], [bl])
    ph.P.add("dve", lambda e: e.reduce_sum(out=lsum[:], in_=lprod[:].rearrange("p (a b) -> p a b", a=2), axis=AX.X), [bl], [bl])
    ph.act(lsum[:], lsum[:], AF.Exp, [bl], [bl])
    ph.tt("dve", neglam[:], lsum[:, 1:2], lsum[:, 0:1], ALU.subtract, [bl], [bl])
    ph.ts("dve", neglam[:], neglam[:], -lam_init, None, ALU.add, None, [bl], [bl])
    ph.ts("dve", sw[:], pp[:, 2:3], 1.0 - lam_init, None, ALU.mult, None, [bpp], [bl])
    cosT = ph.sb([128, S], BF16)
    sinT = ph.sb([128, S], BF16)
    halfpi = ph.sb([128, 1], F32)
    bcs = Buf()
    bhp = Buf()
    ph.memset("pool", halfpi[:], float(np.pi / 2), [bhp])
    CW = 1024
    posi = [ph.sb([128, CW], I32)] * 2
    ang = [ph.sb([128, CW], F32)] * 2
    angf = [ph.sb([128, CW], F32)] * 2
    btt = [Buf()] * 2
    for c4 in range(S // CW):
        j = c4 % 2
        cs_ = slice(c4 * CW, (c4 + 1) * CW)
        bt = btt[j]
        ph.dma("sp", posi[j][:], d["pos"][:, cs_].partition_broadcast(128), [], [bt])
        ph.cp("dve", ang[j][:], posi[j][:], [bt], [bt])
        ph.ts("dve", ang[j][:], ang[j][:], pp[:, 0:1], float(1.0 / (2 * np.pi)), ALU.mult, ALU.mult, [bt, bpp], [bt])
        ph.cp("dve", posi[j][:], ang[j][:], [bt], [bt])
        ph.cp("pool", angf[j][:], posi[j][:], [bt], [bt])
        ph.tt("dve", ang[j][:], ang[j][:], angf[j][:], ALU.subtract, [bt], [bt])
        ph.act(angf[j][:], ang[j][:], AF.Sin, [bt], [bt], scale=float(2 * np.pi))
        ph.ts("dve", sinT[:, cs_], angf[j][:], pp[:, 1:2], None, ALU.mult, None, [bt, bpp], [bcs])
        ph.act(ang[j][:], ang[j][:], AF.Abs, [bt], [bt], scale=float(2 * np.pi))
        ph.act(cosT[:, cs_], ang[j][:], AF.Sin, [bt, bhp], [bcs], scale=-1.0, bias=halfpi[:])
    wat, bwat = _prep_w(ph, d["w_att"], 768, gm, bgm, "wat")
    wsw, bwsw = _prep_w(ph, d["w_att_sw"], 512, gm, bgm, "wsw")
    qk = [ph.sb([128, S], BF16, name=f"qk{c}") for c in range(4)]
    bqk = [Buf() for _ in range(4)]
    pa = [ph.ps([128, 512], F32) for _ in range(2)]
    pb = [ph.ps([128, 512], F32) for _ in range(2)]
    bpa = [Buf() for _ in range(2)]
    bpb = [Buf() for _ in range(2)]
    t1 = [ph.sb([128, 512], F32) for _ in range(2)]
    t2 = [ph.sb([128, 512], F32) for _ in range(2)]
    bt1 = [Buf() for _ in range(2)]
    bt2 = [Buf() for _ in range(2)]
    it = 0
    for cc in range(4):
        for tb in range(NTB):
            j = it % 2
            it += 1
            tsl = slice(tb * 512, (tb + 1) * 512)
            for kc in range(KC):
                ph.mm(pa[j][:], wat[:, kc, cc * 128:(cc + 1) * 128], hT[:, kc, 1 + tb * 512:1 + (tb + 1) * 512], kc == 0, kc == KC - 1, [bwat[kc], bh[kc]], [bpa[j]])
            for kc in range(KC):
                ph.mm(pb[j][:], wsw[:, kc, cc * 128:(cc + 1) * 128], hT[:, kc, 1 + tb * 512:1 + (tb + 1) * 512], kc == 0, kc == KC - 1, [bwsw[kc], bh[kc]], [bpb[j]])
            ph.tt("dve", t1[j][:], pa[j][:], cosT[:, tsl], ALU.mult, [bpa[j], bcs], [bt1[j]])
            ph.tt("dve", t2[j][:], pb[j][:], sinT[:, tsl], ALU.mult, [bpb[j], bcs], [bt2[j]])
            ph.tt("pool", qk[cc][:, tsl], t1[j][:], t2[j][:], ALU.add, [bt1[j], bt2[j]], [bqk[cc]])
    vtm = ph.sb([128, NTT, 256], BF16)
    bv = Buf()
    for t in range(NTT):
        j = t % 2
        for kc in range(KC):
            ph.mm(pa[j][:, 0:256], hT[:, kc, 1 + t * 128:1 + (t + 1) * 128], wat[:, kc, 512:768], kc == 0, kc == KC - 1, [bwat[kc], bh[kc]], [bpa[j]])
        ph.cp("act", vtm[:, t, :], pa[j][:, 0:256], [bpa[j]], [bv])
    ones = ph.sb([128, 128], BF16)
    bones = Buf()
    ph.memset("pool", ones[:], 1.0, [bones])
    eps = ph.sb([128, 1], F32)
    ph.memset("pool", eps[:], 1e-5, [bones])
    NPS, NET, LAG = 2, 3, 1
    psS = [ph.ps([128, 2, 512], F32) for _ in range(NPS)]
    bpsS = [Buf() for _ in range(NPS)]
    eT = [ph.sb([128, 2, 512], BF16) for _ in range(NET)]
    beT = [Buf() for _ in range(NET)]
    accO = pa
    baO = bpa
    accZ = pb
    baZ = bpb
    onesf = ph.sb([128, 128], F32)
    ph.memset("pool", onesf[:], 1.0, [bones])
    _zt = [ang[0][:], angf[0][:]]
    zt2 = ph.sb([128, 1024], F32)
    _zt.append(zt2[:])
    _zt.append(posi[0][:].bitcast(F32))
    _zold = [btt[0], btt[0], None, btt[0]]
    zaccs = [[_zt[bk * 2 + m_] for m_ in range(2)] for bk in range(2)]
    bzaccs = [[Buf() for _ in range(2)] for _ in range(2)]
    zold = [[[_zold[bk * 2 + m_]] if _zold[bk * 2 + m_] is not None else [] for m_ in range(2)] for bk in range(2)]
    rz = [t1[0], t1[1]]
    om = [t2[0], t2[1]]
    brz = [bt1[0], bt1[1]]
    bom = [bt2[0], bt2[1]]
    o = ph.sb([128, 512], F32)
    sq = ph.sb([128, 512], BF16)
    yb = [ph.sb([128, 512], BF16) for _ in range(2)]
    byb = [Buf() for _ in range(2)]
    bo = Buf()
    bsq = Buf()
    scale = 0.125
    NKP = NTT // 2
    its = [(h, qb, m, kp) for h in range(2) for qb in range(NTB) for m in range(2) for kp in range(NKP)]
    n = len(its)

    def finalize(h, qb):
        qs = slice(qb * 512, (qb + 1) * 512)
        zacc, bzacc = zaccs[(h * NTB + qb) % 2], bzaccs[(h * NTB + qb) % 2]
        for m in range(2):
            ph.mm(accZ[m][:], onesf[:], zacc[m][:, 0:512], True, False, [bones, bzacc[m]], [baZ[m]])
            ph.mm(accZ[m][:], onesf[:], zacc[m][:, 512:1024], False, True, [bones, bzacc[m]], [baZ[m]])
            ph.recip(rz[m][:], accZ[m][:], [baZ[m]], [brz[m]])
            ph.tt("dve", om[m][:], accO[m][:], rz[m][:], ALU.mult, [baO[m], brz[m]], [bom[m]])
        ph.stt("dve", o[:], om[1][:], neglam[:, 0:1], om[0][:], ALU.mult, ALU.add, [bom[0], bom[1], bl], [bo])
        ph.tt("pool", sq[:], o[:], o[:], ALU.mult, [bo], [bsq])
        ph.mm(accZ[0][:], ones[:], sq[:], True, True, [bones, bsq], [baZ[0]])
        ph.rsqrt(rz[0][:], accZ[0][:], [baZ[0], bones], [brz[0]], scale=1.0 / 128, bias=eps[:])
        ph.tt("dve", o[:], o[:], rz[0][:], ALU.mult, [bo, brz[0]], [bo])
        y = yb[qb % 2]
        ph.ts("dve", y[:], o[:], sw[:, 0:1], None, ALU.mult, None, [bo, bl], [byb[qb % 2]])
        ph.dma("pool", yT[128 + h * 128:256 + h * 128, qs], y[:], [byb[qb % 2]], [], final=True, key="st")

    for i in range(n + LAG):
        if i < n:
            h, qb, m, kp = its[i]
            ms = slice(m * 64, (m + 1) * 64)
            qs = slice(qb * 512, (qb + 1) * 512)
            a, b = i % NPS, i % NET
            for u in range(2):
                kt = kp * 2 + u
                ph.mm(psS[a][:, u, :], qk[2 + h][ms, kt * 128:(kt + 1) * 128], qk[h][ms, qs], True, True, [bqk[2 + h], bqk[h]], [bpsS[a]])
            ph.act(eT[b][:], psS[a][:], AF.Exp, [bpsS[a]], [beT[b]], scale=scale)
            zacc, bzacc = zaccs[(h * NTB + qb) % 2], bzaccs[(h * NTB + qb) % 2]
            ef = eT[b][:].rearrange("p a n -> p (a n)")
            if kp == 0:
                zo = zold[(h * NTB + qb) % 2][m]
                ph.cp("dve", zacc[m], ef, [beT[b]], [bzacc[m]] + zo)
                del zo[:]
            else:
                ph.tt("dve", zacc[m], zacc[m], ef, ALU.add, [beT[b], bzacc[m]], [bzacc[m]])
        if i >= LAG:
            j = i - LAG
            h, qb, m, kp = its[j]
            b = j % NET
            for u in range(2):
                kt = kp * 2 + u
                ph.mm(accO[m][:], vtm[:, kt, h * 128:(h + 1) * 128], eT[b][:, u, :], kt == 0, kt == NTT - 1, [bv, beT[b]], [baO[m]])
            if m == 1 and kp == NKP - 1:
                finalize(h, qb)
    ph.close()


def _pool_mixer(nc, hT_dram, d, yT):
    ph = Ph(nc)
    hT, bh = _load_hT(ph, hT_dram)
    gm = ph.sb([128, KC], F32)
    bgm = Buf()
    ph.dma("sp", gm[:], d["gmix"], [], [bgm])
    pc = ph.sb([128, 8 + 64], F32)
    bpc = Buf()
    ph.dma("sp", pc[:], d["pool_pp"], [], [bpc])
    wp, bwp = _prep_w(ph, d["w_pool"], 128, gm, bgm, "wp")
    pmx_f = ph.sb([128, 128], F32)
    pmx = ph.sb([128, 128], BF16)
    bpm = Buf()
    ph.dma("sp", pmx_f[:], d["pool_mix_bd"], [], [bpm])
    ph.cp("dve", pmx[:], pmx_f[:], [bpm], [bpm])
    PADW = 8
    W = S + 2 * PADW
    u = ph.sb([128, W], F32)
    s2 = ph.sb([128, W], F32)
    s4 = ph.sb([128, W], F32)
    s8 = ph.sb([128, W], F32)
    s16 = ph.sb([128, W], F32)
    bu, b2, b4, b8, b16 = [Buf() for _ in range(5)]
    ph.memset("pool", u[:, 0:PADW], 0.0, [bu])
    ph.memset("pool", u[:, W - PADW:W], 0.0, [bu])
    pa = [ph.ps([128, 512], F32) for _ in range(2)]
    bpa = [Buf() for _ in range(2)]
    for tb in range(NTB):
        j = tb % 2
        for kc in range(KC):
            ph.mm(pa[j][:], wp[:, kc, :], hT[:, kc, 1 + tb * 512:1 + (tb + 1) * 512], kc == 0, kc == KC - 1, [bwp[kc], bh[kc]], [bpa[j]])
        ph.cp("act", u[:, PADW + tb * 512:PADW + (tb + 1) * 512], pa[j][:], [bpa[j]], [bu])
    c = slice(PADW, PADW + S)

    ph.tt("dve", s2[:, 1:W], u[:, 0:W - 1], u[:, 1:W], ALU.add, [bu], [b2])
    ph.tt("pool", s4[:, 2:W - 1], s2[:, 1:W - 2], s2[:, 3:W], ALU.add, [b2], [b4])
    ph.tt("dve", s8[:, 4:W - 3], s4[:, 2:W - 5], s4[:, 6:W - 1], ALU.add, [b4], [b8])
    ph.tt("pool", s16[:, 8:W - 7], s8[:, 4:W - 11], s8[:, 12:W - 3], ALU.add, [b8], [b16])
    acc = ph.sb([128, S], F32)
    bacc = Buf()
    ph.ts("dve", acc[:], s2[:, c], pc[:, 0:1], None, ALU.mult, None, [b2, bpc], [bacc])
    ph.stt("dve", acc[:], s4[:, c], pc[:, 1:2], acc[:], ALU.mult, ALU.add, [b4, bpc, bacc], [bacc])
    ph.stt("dve", acc[:], s8[:, c], pc[:, 2:3], acc[:], ALU.mult, ALU.add, [b8, bpc, bacc], [bacc])
    ph.stt("dve", acc[:], s16[:, c], pc[:, 3:4], acc[:], ALU.mult, ALU.add, [b16, bpc, bacc], [bacc])
    bt_ = ph.sb([128, 16], F32)
    tmpb = ph.sb([128, 16], F32)
    bbt = Buf()
    for wi, (sw_, bs_) in enumerate(((s2, b2), (s4, b4), (s8, b8), (s16, b16))):
        for half in range(2):
            src = sw_[:, PADW:PADW + 8] if half == 0 else sw_[:, PADW + S - 8:PADW + S]
            dst = bt_[:, half * 8:(half + 1) * 8]
            tb_ = pc[:, 8 + wi * 16 + half * 8:8 + wi * 16 + half * 8 + 8]
            if wi == 0:
                ph.tt("dve", dst, src, tb_, ALU.mult, [bs_, bpc], [bbt])
            else:
                ph.tt("dve", tmpb[:, half * 8:(half + 1) * 8], src, tb_, ALU.mult, [bs_, bpc], [bbt])
                ph.tt("dve", dst, dst, tmpb[:, half * 8:(half + 1) * 8], ALU.add, [bbt], [bbt])
    ph.cp("dve", acc[:, 0:8], bt_[:, 0:8], [bbt, bacc], [bacc])
    ph.cp("dve", acc[:, S - 8:S], bt_[:, 8:16], [bbt, bacc], [bacc])
    pooled = ph.sb([128, S], BF16)
    bpl = Buf()
    ph.tt("dve", pooled[:], acc[:], u[:, c], ALU.subtract, [bacc, bu], [bpl])
    yb = [ph.sb([128, 512], BF16) for _ in range(2)]
    byb = [Buf() for _ in range(2)]
    for tb in range(NTB):
        j = tb % 2
        ph.mm(pa[j][:], pmx[:], pooled[:, tb * 512:(tb + 1) * 512], True, True, [bpm, bpl], [bpa[j]])
        ph.ts("dve", yb[j][:], pa[j][:], pc[:, 4:5], None, ALU.mult, None, [bpa[j], bpc], [byb[j]])
        ph.dma("pool", yT[384:512, tb * 512:(tb + 1) * 512], yb[j][:], [byb[j]], [], final=True, key="st")
    ph.close()


def _rwkv_proj(nc, hT_dram, d, layer, rw):
    ph = Ph(nc)
    hT, bh = _load_hT(ph, hT_dram)
    gm = ph.sb([128, KC], F32)
    bgm = Buf()
    ph.dma("sp", gm[:], d["gmix"], [], [bgm])
    mu0 = ph.sb([128, 640], F32)
    mu1 = ph.sb([128, 640], F32)
    c0 = ph.sb([128, 640], F32)
    bmu = Buf()
    ph.dma("sp", mu0[:], d["mu"][0:1, :].partition_broadcast(128), [], [bmu])
    ph.dma("sp", mu1[:], d["mu"][1:2, :].partition_broadcast(128), [], [bmu])
    ph.tt("dve", c0[:], mu0[:], mu1[:], ALU.add, [bmu], [bmu])
    ph.ts("dve", c0[:], c0[:], -1.0, 1.0, ALU.mult, ALU.add, [bmu], [bmu])
    ws = []
    for i, cv in enumerate((c0, mu0, mu1)):
        ws.append(_prep_w(ph, d["w_rwkv"], 640, gm, bgm, f"wr{i}", colvec=cv[:], bcol=bmu))
    offs = (1, 0, 2)
    gup_f = ph.sb([128, 128], F32)
    gup = ph.sb([128, 128], BF16)
    bgu = Buf()
    ph.dma("sp", gup_f[:], d["gate_up"], [], [bgu])
    ph.cp("dve", gup[:], gup_f[:], [bgu], [bgu])
    if layer > 0:
        wvd, bwvd = _prep_w(ph, d["w_vdown"], 32, gm, bgm, "wvd")
        vup_f = ph.sb([32, 128], F32)
        vup = ph.sb([32, 128], BF16)
        vb_ = ph.sb([128, 1], F32)
        bvu = Buf()
        ph.dma("sp", vup_f[:], d["vres_up"], [], [bvu])
        ph.dma("sp", vb_[:], d["vres_bias"], [], [bvu])
        ph.cp("dve", vup[:], vup_f[:], [bvu], [bvu])
        pvd = ph.ps([32, 512], F32)
        pvg = ph.ps([128, 512], F32)
        bpvd, bpvg = Buf(), Buf()
    pr = [ph.ps([128, 512], F32) for _ in range(5)]
    bpr = [Buf() for _ in range(5)]
    pg = ph.ps([128, 512], F32)
    bpg = Buf()
    ob = [ph.sb([128, 5, 512], BF16) for _ in range(2)]
    bob = [Buf() for _ in range(2)]
    vf = [ph.sb([128, 512], F32) for _ in range(2)]
    bvf = [Buf() for _ in range(2)]
    sgx = ph.sb([128, 512], BF16)
    bsgx = Buf()
    if layer > 0:
        vdb = ph.sb([32, 512], BF16)
        sgv = ph.sb([128, 512], F32)
        dif = ph.sb([128, 512], F32)
        bvdb, bsgv, bdif = Buf(), Buf(), Buf()
    for tb in range(NTB):
        j = tb % 2
        tsl = slice(tb * 512, (tb + 1) * 512)
        for cc in range(5):
            n = 0
            for si in range(3):
                wb, bw = ws[si]
                for kc in range(KC):
                    ph.mm(pr[cc][:], wb[:, kc, cc * 128:(cc + 1) * 128], hT[:, kc, offs[si] + tb * 512:offs[si] + (tb + 1) * 512],
                          n == 0, n == 3 * KC - 1, [bw[kc], bh[kc]], [bpr[cc]])
                    n += 1
        ph.cp("act", ob[j][:, 0, :], pr[0][:], [bpr[0]], [bob[j]])
        ph.cp("dve", ob[j][:, 1, :], pr[1][:], [bpr[1]], [bob[j]])
        ph.cp("act", ob[j][:, 3, :], pr[3][:], [bpr[3]], [bob[j]])
        if layer == 0:
            ph.cp("dve", vf[j][:], pr[2][:], [bpr[2]], [bvf[j]])
            ph.cp("pool", ob[j][:, 2, :], vf[j][:], [bvf[j]], [bob[j]])
            ph.dma("pool", rw["vfirst"][:, tsl], vf[j][:], [bvf[j]], [], final=True, key="st")
        else:
            ph.dma("sp", vf[j][:], rw["vfirst"][:, tsl], [], [bvf[j]], key="ldv")
            for kc in range(KC):
                ph.mm(pvd[:], wvd[:, kc, :], hT[:, kc, 1 + tb * 512:1 + (tb + 1) * 512], kc == 0, kc == KC - 1, [bwvd[kc], bh[kc]], [bpvd])
            ph.cp("act", vdb[:], pvd[:], [bpvd], [bvdb])
            ph.mm(pvg[:], vup[:], vdb[:], True, True, [bvu, bvdb], [bpvg])
            ph.act(sgv[:], pvg[:], AF.Sigmoid, [bpvg, bvu], [bsgv], bias=vb_[:])
            ph.tt("dve", dif[:], vf[j][:], pr[2][:], ALU.subtract, [bvf[j], bpr[2]], [bdif])
            ph.tt("pool", dif[:], dif[:], sgv[:], ALU.mult, [bdif, bsgv], [bdif])
            ph.tt("dve", ob[j][:, 2, :], dif[:], pr[2][:], ALU.add, [bdif, bpr[2]], [bob[j]])
        ph.act(sgx[:], pr[4][:], AF.Sigmoid, [bpr[4]], [bsgx])
        ph.mm(pg[:], gup[:], sgx[:], True, True, [bgu, bsgx], [bpg])
        ph.cp("act", ob[j][:, 4, :], pg[:], [bpg], [bob[j]])
        ph.dma("pool", rw["tok"][:, :, tsl], ob[j][:], [bob[j]], [], final=True, key="st")
    ph.close()


QN = 7


def _rwkv_derive(nc, d, rw):
    ph = Ph(nc)
    pp = ph.sb([128, 16], F32)
    bpp = Buf()
    ph.dma("sp", pp[:], d["rw_pp"], [], [bpp])
    bones = ph.sb([128, 128], F32)
    bbo = Buf()
    ph.dma("sp", bones[:], d["blockones"], [], [bbo])
    lo_f = ph.sb([128, 4, 128], F32)
    lo = ph.sb([128, 4, 128], BF16)
    blo = Buf()
    for i in range(4):
        ph.dma("sp", lo_f[:, i, :], d["lora"][i], [], [blo])
    ph.cp("dve", lo[:], lo_f[:], [blo], [blo])
    cmask = ph.sb([128, 512], F32)
    bcm = Buf()
    ph.memset("pool", cmask[:], 1.0, [bcm])
    ph.memset("pool", cmask[:].rearrange("p (c i) -> p c i", i=128)[:, :, 0:1], 0.0, [bcm])
    tiny = ph.sb([128, 1], F32)
    ph.memset("pool", tiny[:], 0.0, [bcm])
    tin = [ph.sb([128, 4, 512], BF16) for _ in range(2)]
    btin = [Buf() for _ in range(2)]
    th = ph.sb([128, 512], BF16)
    bth = Buf()
    pL = [ph.ps([128, 512], F32) for _ in range(4)]
    bpL = [Buf() for _ in range(4)]
    pN = ph.ps([128, 512], F32)
    pB = ph.ps([128, 512], F32)
    bpN, bpB = Buf(), Buf()
    sig = [ph.sb([128, 512], F32) for _ in range(2)]
    aa = [ph.sb([128, 512], F32) for _ in range(2)]
    bsig = [Buf() for _ in range(2)]
    baa = [Buf() for _ in range(2)]
    kk = ph.sb([128, 512], F32)
    sq = ph.sb([128, 512], F32)
    rs = ph.sb([128, 512], F32)
    kkn = ph.sb([128, 512], F32)
    rk = ph.sb([128, 512], F32)
    kd = [ph.sb([128, 512], F32) for _ in range(2)]
    tmp = ph.sb([128, 512], F32)
    ksum = ph.sb([128, 512], F32)
    bon = [ph.sb([128, 512], BF16) for _ in range(2)]
    bkk, bsq, brs, bkkn, brk, btmp, bks = [Buf() for _ in range(7)]
    bkd = [Buf() for _ in range(2)]
    bbon = [Buf() for _ in range(2)]
    cs = ph.sb([128, 512], F32)
    csx = ph.sb([128, 512], F32)
    dln = ph.sb([128, 512], F32)
    eL = ph.sb([128, 512], F32)
    eLm = ph.sb([128, 512], F32)
    eLex = ph.sb([128, 512], F32)
    eLC = ph.sb([128, 512], F32)
    ka = ph.sb([128, 512], F32)
    bcs, bcsx, bdln, beL, beLm, beLex, beLC, bka = [Buf() for _ in range(8)]
    outq = [ph.sb([128, QN, 512], BF16) for _ in range(2)]
    bout = [Buf() for _ in range(2)]
    wc = [ph.sb([128, 4], F32) for _ in range(2)]
    bwc = [Buf() for _ in range(2)]
    oi = 0
    for tb in range(NTB):
        j = tb % 2
        tsl = slice(tb * 512, (tb + 1) * 512)
        ti = tin[j]
        ph.dma("sp", ti[:], rw["tok"][:, 0:4, tsl], [], [btin[j]], key="ld")
        rb, kb, vb, xb = ti[:, 0, :], ti[:, 1, :], ti[:, 2, :], ti[:, 3, :]
        ph.act(th[:], xb, AF.Tanh, [btin[j]], [bth])
        for dd in range(2):
            ph.mm(pL[dd][:], lo[:, dd, :], th[:], True, True, [blo, bth], [bpL[dd]])
            ph.mm(pL[2 + dd][:], lo[:, 2 + dd, :], xb, True, True, [blo, btin[j]], [bpL[2 + dd]])
        for dd in range(2):
            ph.act(sig[dd][:], pL[dd][:], AF.Sigmoid, [bpL[dd], bpp], [bsig[dd]], bias=pp[:, 5 + dd:6 + dd])
            ph.act(aa[dd][:], pL[2 + dd][:], AF.Sigmoid, [bpL[2 + dd], bpp], [baa[dd]], bias=pp[:, 7 + dd:8 + dd])
        ph.ts("dve", kk[:], kb, pp[:, 0:1], None, ALU.mult, None, [btin[j], bpp], [bkk])
        ph.tt("pool", sq[:], kk[:], kk[:], ALU.mult, [bkk], [bsq])
        ph.mm(pN[:], bones[:], sq[:], True, True, [bbo, bsq], [bpN])
        ph.ts("dve", rs[:], pN[:], 1e-24, None, ALU.max, None, [bpN], [brs])
        ph.rsqrt(rs[:], rs[:], [brs, bcm], [brs], bias=tiny[:])
        ph.tt("dve", kkn[:], kk[:], rs[:], ALU.mult, [bkk, brs], [bkkn])
        ph.ts("pool", rk[:], rb, pp[:, 2:3], None, ALU.mult, None, [btin[j], bpp], [brk])
        for dd in range(2):
            ph.ts("dve", tmp[:], aa[dd][:], -1.0, pp[:, 1:2], ALU.add, ALU.mult, [baa[dd], bpp], [btmp])
            ph.stt("dve", kd[dd][:], tmp[:], 1.0, kb, ALU.add, ALU.mult, [btmp, btin[j]], [bkd[dd]])
        ph.tt("pool", ksum[:], kd[0][:], kd[1][:], ALU.add, [bkd[0], bkd[1]], [bks])
        ph.tt("pool", ksum[:], ksum[:], rk[:], ALU.mult, [bks, brk], [bks])
        ph.mm(pB[:], bones[:], ksum[:], True, True, [bbo, bks], [bpB])
        ph.tt("dve", bon[j][:], pB[:], vb, ALU.mult, [bpB, btin[j]], [bbon[j]])
        ph.dma("pool", rw["bonus"][:, tsl], bon[j][:], [bbon[j]], [], final=True, key="st")
        for dd in range(2):
            def rv(ap, dd=dd):
                return ap if dd == 0 else ap[:, ::-1]
            obk = tb if dd == 0 else NTB - 1 - tb
            osl = slice(obk * 512, (obk + 1) * 512)
            oq = outq[oi % 2]
            boq = bout[oi % 2]
            wct = wc[oi % 2]
            bwct = bwc[oi % 2]
            oi += 1
            ph.scan(cs[:], cmask[:], rv(sig[dd][:]), [bcm, bsig[dd]], [bcs])
            ph.tt("pool", csx[:], cs[:], rv(sig[dd][:]), ALU.subtract, [bcs, bsig[dd]], [bcsx])
            csv = cs[:].rearrange("p (c i) -> p c i", i=128)
            ph.tt("pool", dln[:].rearrange("p (c i) -> p c i", i=128), csv, csv[:, :, 127:128].to_broadcast([128, 4, 128]), ALU.subtract, [bcs], [bdln])
            ph.act(eL[:], cs[:], AF.Exp, [bcs], [beL], scale=-WDS)
            ph.act(eLm[:], cs[:], AF.Exp, [bcs], [beLm], scale=WDS)
            ph.act(eLex[:], csx[:], AF.Exp, [bcsx], [beLex], scale=-WDS)
            ph.act(eLC[:], dln[:], AF.Exp, [bdln], [beLC], scale=WDS)
            ph.act(wct[:], csv[:, :, 127], AF.Exp, [bcs], [bwct], scale=-WDS)
            ph.tt("pool", ka[:], kkn[:], aa[dd][:], ALU.mult, [bkkn, baa[dd]], [bka])
            ph.stt("dve", oq[:, 0, :], rv(kkn[:]), -1.0, eLex[:], ALU.mult, ALU.mult, [bkkn, beLex], [boq])
            ph.tt("dve", oq[:, 1, :], rv(ka[:]), eLm[:], ALU.mult, [bka, beLm], [boq])
            ph.tt("pool", oq[:, 2, :], rv(kd[dd][:]), eLm[:], ALU.mult, [bkd[dd], beLm], [boq])
            ph.tt("dve", oq[:, 3, :], rv(rb), eL[:], ALU.mult, [btin[j], beL], [boq])
            ph.tt("pool", oq[:, 4, :], rv(ka[:]), eLC[:], ALU.mult, [bka, beLC], [boq])
            ph.tt("dve", oq[:, 5, :], rv(kd[dd][:]), eLC[:], ALU.mult, [bkd[dd], beLC], [boq])
            ph.cp("pool", oq[:, 6, :], rv(vb), [btin[j]], [boq])
            ph.dma("pool", rw["sc"][dd][:, :, osl], oq[:], [boq], [], final=True, key="st")
            ph.dma("pool", rw["wc"][dd][:, obk * 4:(obk + 1) * 4], wct[:], [bwct], [], final=True, key="st")
    ph.close()


def _rwkv_scan(nc, d, rw):
    ph = Ph(nc)
    NCH = S // 128
    X = []
    bX = []
    for dd in range(2):
        x_ = ph.sb([128, QN, S], BF16, name=f"X{dd}")
        b_ = [Buf() for _ in range(QN)]
        for q in range(QN):
            ph.dma("sp" if q % 2 == 0 else "act", x_[:, q, :], rw["sc"][dd][:, q, :], [], [b_[q]])
        X.append(x_)
        bX.append(b_)
    WC = ph.sb([128, 2, NCH], F32)
    bWC = Buf()
    for dd in range(2):
        ph.dma("sp", WC[:, dd, :], rw["wc"][dd], [], [bWC])
    mk = ph.sb([128, 3, 4, 128], F32)
    bmk = Buf()
    for i in range(3):
        for r_ in range(4):
            ph.dma("sp", mk[:, i, r_, :], d["masks"][i], [], [bmk])
    idf = ph.sb([128, 128], F32)
    ident = ph.sb([128, 128], BF16)
    id64 = ph.sb([128, 64], F32)
    bid = Buf()
    ph.dma("sp", idf[:], d["ident"], [], [bid])
    ph.dma("sp", id64[:], d["ident64x2"], [], [bid])
    ph.cp("dve", ident[:], idf[:], [bid], [bid])
    B = [ph.ps([128, 4, 128], F32, name=f"B{i}") for i in range(7)]
    bB = [Buf() for _ in range(7)]
    BT = ph.ps([128, 4, 2, 128], BF16, name="BT")
    bBT = Buf()
    Nn = [ph.sb([128, 4, 128], BF16) for _ in range(2)]
    NT = [ph.sb([128, 4, 128], BF16) for _ in range(2)]
    bNn = [Buf() for _ in range(2)]
    bNT = [Buf() for _ in range(2)]
    Mak = ph.sb([128, 4, 128], BF16)
    Mbr = ph.sb([128, 4, 128], BF16)
    Mkr = ph.sb([128, 4, 128], BF16)
    bMak, bMbr, bMkr = Buf(), Buf(), Buf()
    TTs = ph.sb([128, 4, 2, 128], BF16)
    bTT = Buf()
    Z = [ph.sb([128, 4, 128], BF16) for _ in range(2)]
    bZ = [Buf() for _ in range(2)]
    G = ph.sb([128, 2, 128], BF16)
    Phi = ph.sb([128, 2, 64], BF16)
    bG, bPhi = Buf(), Buf()
    ST = [ph.sb([128, 2, 64], BF16) for _ in range(2)]
    bST = [Buf() for _ in range(2)]
    Yo = [ph.sb([128, 2, 128], F32) for _ in range(2)]
    bYo = [Buf() for _ in range(2)]
    qsel = (0, 4, 5, 6)
    for c in range(NCH):
        cs_ = slice(c * 128, (c + 1) * 128)
        for h in range(2):
            for dd in range(2):
                i = dd * 2 + h
                hs = slice(h * 64, (h + 1) * 64)
                At, Bt, Kt, Rt = (X[dd][hs, q, cs_] for q in range(4))
                ph.mm(B[0][:, i, :], Bt, At, True, True, bX[dd][0:4], [bB[0]])
                ph.mm(B[1][:, i, :], At, Bt, True, True, bX[dd][0:4], [bB[1]])
                ph.mm(B[2][:, i, :], Kt, At, True, True, bX[dd][0:4], [bB[2]])
                ph.mm(B[3][:, i, :], Bt, Rt, True, True, bX[dd][0:4], [bB[3]])
                ph.mm(B[4][:, i, :], Kt, Rt, True, True, bX[dd][0:4], [bB[4]])
        ph.tt("dve", Nn[0][:], B[0][:], mk[:, 0], ALU.mult, [bB[0], bmk], [bNn[0]])
        ph.tt("dve", NT[0][:], B[1][:], mk[:, 1], ALU.mult, [bB[1], bmk], [bNT[0]])
        ph.tt("dve", Mak[:], B[2][:], mk[:, 0], ALU.mult, [bB[2], bmk], [bMak])
        ph.tt("dve", Mbr[:], B[3][:], mk[:, 2], ALU.mult, [bB[3], bmk], [bMbr])
        ph.tt("dve", Mkr[:], B[4][:], mk[:, 2], ALU.mult, [bB[4], bmk], [bMkr])
        for qi, q in enumerate(qsel):
            for dd in range(2):
                ph.tr(BT[:, qi, dd, :], X[dd][:, q, cs_], ident[:], [bX[dd][q], bid], [bBT])
        ph.cp("act", TTs[:], BT[:], [bBT], [bTT])
        for dd in range(2):
            for h in range(2):
                i = dd * 2 + h
                ph.mm(B[5][:, i, 0:64], Mak[:, i, :], TTs[:, 3, dd, h * 64:(h + 1) * 64], True, True, [bMak, bTT], [bB[5]])
        ph.cp("pool", Z[0][:, :, 0:64], TTs[:, 0].rearrange("p d (h k) -> p (d h) k", h=2), [bTT], [bZ[0]])
        ph.cp("act", Z[0][:, :, 64:128], B[5][:, :, 0:64], [bB[5]], [bZ[0]])
        zi = 0
        ni = 0
        for lvl in range(7):
            if lvl < 6:
                for i in range(4):
                    ph.mm(B[1][:, i, :], NT[ni][:, i, :], Nn[ni][:, i, :], True, True, [bNn[ni], bNT[ni]], [bB[1]])
                if lvl < 5:
                    for i in range(4):
                        ph.mm(B[2][:, i, :], Nn[ni][:, i, :], NT[ni][:, i, :], True, True, [bNn[ni], bNT[ni]], [bB[2]])
            for i in range(4):
                ph.mm(B[0][:, i, :], Nn[ni][:, i, :], Z[zi][:, i, :], True, True, [bNn[ni], bZ[zi]], [bB[0]])
            if lvl < 6:
                ph.cp("act", Nn[1 - ni][:], B[1][:], [bB[1]], [bNn[1 - ni]])
                if lvl < 5:
                    ph.cp("dve", NT[1 - ni][:], B[2][:], [bB[2]], [bNT[1 - ni]])
            ph.tt("dve", Z[1 - zi][:], B[0][:], Z[zi][:], ALU.add, [bB[0], bZ[zi]], [bZ[1 - zi]])
            zi = 1 - zi
            if lvl < 6:
                ni = 1 - ni
        Zf = Z[zi]
        bZf = bZ[zi]
        so = c % 2
        for dd in range(2):
            for h in range(2):
                i = dd * 2 + h
                hc = slice(h * 64, (h + 1) * 64)
                ph.mm(B[3][0:64, i, :], Zf[:, i, 0:64], Mbr[:, i, :], True, True, [bZf, bMbr], [bB[3]])
                ph.mm(B[4][0:64, i, 0:64], Zf[:, i, 0:64], TTs[:, 1, dd, hc], True, True, [bZf, bTT], [bB[4]])
        for dd in range(2):
            for h in range(2):
                i = dd * 2 + h
                hs = slice(h * 64, (h + 1) * 64)
                ph.tt("dve", G[hs, dd, :], B[3][0:64, i, :], X[dd][hs, 3, cs_], ALU.add, [bB[3], bX[dd][3]], [bG])
                ph.stt("dve", Phi[hs, dd, :], id64[hs, :], WC[hs, dd, c:c + 1], B[4][0:64, i, 0:64], ALU.mult, ALU.add, [bid, bWC, bB[4]], [bPhi])
        for dd in range(2):
            for h in range(2):
                i = dd * 2 + h
                hs = slice(h * 64, (h + 1) * 64)
                hc = hs
                last = (c == 0)
                ph.mm(B[5][0:64, i, 0:64], TTs[:, 1, dd, hc], Zf[:, i, 64:128], True, False, [bTT, bZf], [bB[5]])
                ph.mm(B[5][0:64, i, 0:64], TTs[:, 2, dd, hc], TTs[:, 3, dd, hc], False, last, [bTT], [bB[5]])
                if not last:
                    ph.mm(B[5][0:64, i, 0:64], Phi[hs, dd, :], ST[so][hs, dd, :], False, True, [bPhi, bST[so]], [bB[5]])
                ph.mm(B[6][0:64, i, :], Zf[:, i, 64:128], Mbr[:, i, :], True, False, [bZf, bMbr], [bB[6]])
                ph.mm(B[6][0:64, i, :], TTs[:, 3, dd, hc], Mkr[:, i, :], False, last, [bTT, bMkr], [bB[6]])
                if not last:
                    ph.mm(B[6][0:64, i, :], ST[so][hs, dd, :], G[hs, dd, :], False, True, [bST[so], bG], [bB[6]])
        yo = Yo[c % 2]
        for dd in range(2):
            for h in range(2):
                i = dd * 2 + h
                hs = slice(h * 64, (h + 1) * 64)
                ph.cp("act", ST[1 - so][hs, dd, :], B[5][0:64, i, 0:64], [bB[5]], [bST[1 - so]])
                ph.cp("dve" if h else "act", yo[hs, dd, :], B[6][0:64, i, :], [bB[6]], [bYo[c % 2]])
        for dd in range(2):
            ph.dma("pool", rw["y"][dd][:, cs_], yo[:, dd, :], [bYo[c % 2]], [], final=True, key="st")
    ph.close()


def _rwkv_final(nc, d, rw, yT):
    ph = Ph(nc)
    pp = ph.sb([128, 16], F32)
    bpp = Buf()
    ph.dma("sp", pp[:], d["rw_pp"], [], [bpp])
    bones = ph.sb([128, 128], F32)
    bbo = Buf()
    ph.dma("sp", bones[:], d["blockones"], [], [bbo])
    geps = ph.sb([128, 1], F32)
    ph.memset("pool", geps[:], GN_EPS, [bbo])
    y0 = [ph.sb([128, 512], F32) for _ in range(2)]
    y1 = [ph.sb([128, 512], F32) for _ in range(2)]
    bg = [ph.sb([128, 2, 512], BF16) for _ in range(2)]
    by = [Buf() for _ in range(2)]
    bbg = [Buf() for _ in range(2)]
    ysum = ph.sb([128, 512], F32)
    yc = ph.sb([128, 512], F32)
    sq = ph.sb([128, 512], F32)
    rstd = ph.sb([128, 512], F32)
    bys, byc, bsq, brs = Buf(), Buf(), Buf(), Buf()
    pM = ph.ps([128, 512], F32)
    pV = ph.ps([128, 512], F32)
    bpM, bpV = Buf(), Buf()
    ob = [ph.sb([128, 512], BF16) for _ in range(2)]
    bob = [Buf() for _ in range(2)]
    for tb in range(NTB):
        j = tb % 2
        tsl = slice(tb * 512, (tb + 1) * 512)
        rsl = slice((NTB - 1 - tb) * 512, (NTB - tb) * 512)
        ph.dma("sp", y0[j][:], rw["y"][0][:, tsl], [], [by[j]], key="ld")
        ph.dma("sp", y1[j][:], rw["y"][1][:, rsl], [], [by[j]], key="ld")
        ph.dma("sp", bg[j][:, 0, :], rw["bonus"][:, tsl], [], [bbg[j]], key="ld2")
        ph.dma("sp", bg[j][:, 1, :], rw["tok"][:, 4, tsl], [], [bbg[j]], key="ld2")
        ph.tt("dve", ysum[:], y0[j][:], y1[j][:, ::-1], ALU.add, [by[j]], [bys])
        ph.mm(pM[:], bones[:], ysum[:], True, True, [bbo, bys], [bpM])
        ph.stt("dve", yc[:], pM[:], -1.0 / 64, ysum[:], ALU.mult, ALU.add, [bpM, bys], [byc])
        ph.tt("pool", sq[:], yc[:], yc[:], ALU.mult, [byc], [bsq])
        ph.mm(pV[:], bones[:], sq[:], True, True, [bbo, bsq], [bpV])
        ph.rsqrt(rstd[:], pV[:], [bpV, bbo], [brs], scale=1.0 / 64, bias=geps[:])
        ph.tt("dve", yc[:], yc[:], rstd[:], ALU.mult, [byc, brs], [byc])
        ph.ts("dve", yc[:], yc[:], pp[:, 3:4], pp[:, 4:5], ALU.mult, ALU.add, [byc, bpp], [byc])
        ph.tt("pool", yc[:], yc[:], bg[j][:, 0, :], ALU.add, [byc, bbg[j]], [byc])
        ph.tt("dve", ob[j][:], yc[:], bg[j][:, 1, :], ALU.mult, [byc, bbg[j]], [bob[j]])
        ph.dma("pool", yT[0:128, tsl], ob[j][:], [bob[j]], [], final=True, key="st")
    ph.close()


TPC = 2048
NT2 = TPC // 128
NB2 = TPC // 512
NFC = DFF // 128


def _outproj_norm(nc, x, yT, d, sc, moe, sel=None):
    ph = Ph(nc)
    wo_f = [ph.sb([128, D], F32) for _ in range(2)]
    bwf = [Buf() for _ in range(2)]
    wo = ph.sb([128, KC, D], BF16)
    bwo = Buf()
    wv = d["w_out"].rearrange("(kc p) n -> kc p n", p=128)
    for kc in range(KC):
        ph.dma("sp", wo_f[kc % 2][:], wv[kc], [], [bwf[kc % 2]], key="ldw")
        ph.cp("pool" if kc % 2 else "dve", wo[:, kc, :], wo_f[kc % 2][:], [bwf[kc % 2]], [bwo])
    ident = ph.sb([128, 128], BF16)
    identf = ph.sb([128, 128], F32)
    bid = Buf()
    ph.dma("sp", identf[:], d["ident"], [], [bid])
    ph.cp("dve", ident[:], identf[:], [bid], [bid])
    eps = ph.sb([128, 1], F32)
    ph.memset("pool", eps[:], 1e-6, [bid])
    if moe:
        rg = ph.sb([128, NE, D], F32)
        gb = ph.sb([128, D], F32)
        brg = Buf()
        ph.dma("sp", gb[:], d["gffn_row"].partition_broadcast(128), [], [brg])
        for e_ in range(NE):
            ph.dma("sp", rg[:, e_, :], d["routerT"][e_:e_ + 1, :].partition_broadcast(128), [], [brg])
        ph.tt("dve", rg[:], rg[:], gb[:].unsqueeze(1).to_broadcast([128, NE, D]), ALU.mult, [brg], [brg])
        lg = ph.sb([128, NE], F32)
        l2 = ph.sb([128, NE], F32)
        m1 = ph.sb([128, 1], F32)
        m2 = ph.sb([128, 1], F32)
        k1 = ph.sb([128, NE], F32)
        k2 = ph.sb([128, NE], F32)
        g1 = ph.sb([128, 1], F32)
        g2 = ph.sb([128, 1], F32)
        comb = [ph.sb([128, NE], F32) for _ in range(2)]
        bcomb = [Buf() for _ in range(2)]
        blg = Buf()
        junkf = ph.sb([128, D], F32)
        bjf = Buf()
    yt = [ph.sb([128, KC, 128], BF16) for _ in range(2)]
    byt = [Buf() for _ in range(2)]
    xt = [ph.sb([128, D], F32) for _ in range(2)]
    bxt = [Buf() for _ in range(2)]
    x1 = [ph.sb([128, D], F32) for _ in range(2)]
    bx1 = [Buf() for _ in range(2)]
    pO = [ph.ps([128, 2, 512], F32) for _ in range(2)]
    bpO = [Buf() for _ in range(2)]
    junk = ph.sb([128, D], BF16)
    bj = Buf()
    ss = [ph.sb([128, 1], F32) for _ in range(2)]
    bss = [Buf() for _ in range(2)]
    xn = [ph.sb([128, D], BF16) for _ in range(2)]
    bxn = [Buf() for _ in range(2)]
    xnf = ph.sb([128, D], F32)
    bxnf = Buf()
    pT = [ph.ps([128, KC, 128], BF16) for _ in range(2)]
    bpT = [Buf() for _ in range(2)]
    hs = [ph.sb([128, KC, 128], BF16) for _ in range(2)]
    bhs = [Buf() for _ in range(2)]
    yv = yT.rearrange("(kc p) t -> p kc t", p=128)
    xv = x.rearrange("(t p) d -> t p d", p=128)
    if sel is not None:
        msk = ph.sb([128, 2], F32)
        bmsk = Buf()
        ph.dma("sp", msk[:], sel, [], [bmsk])
        yt2 = [ph.sb([128, KC, 128], BF16) for _ in range(2)]
        byt2 = [Buf() for _ in range(2)]
        ysel = [ph.sb([128, KC, 128], BF16) for _ in range(2)]
        bysel = [Buf() for _ in range(2)]
    x1v = sc["x1"].rearrange("(t p) d -> t p d", p=128)
    for t in range(NT2):
        j = t % 2
        ph.dma("sp", yt[j][:], yv[:, :, t * 128:(t + 1) * 128], [], [byt[j]], key="ld")
        ph.dma("sp", xt[j][:], xv[t], [], [bxt[j]], key="ld2")
        ysrc, bysrc = yt[j], byt[j]
        if sel is not None:
            ph.dma("sp", yt2[j][:], yv[:, :, TPC + t * 128:TPC + (t + 1) * 128], [], [byt2[j]], key="ld")
            ph.ts("pool", ysel[j][:], yt[j][:], msk[:, 0:1], None, ALU.mult, None, [byt[j], bmsk], [bysel[j]])
            ph.stt("dve", ysel[j][:], yt2[j][:], msk[:, 1:2], ysel[j][:], ALU.mult, ALU.add, [byt2[j], bmsk, bysel[j]], [bysel[j]])
            ysrc, bysrc = ysel[j], bysel[j]
        for hf in range(2):
            for kc in range(KC):
                ph.mm(pO[j][:, hf, :], ysrc[:, kc, :], wo[:, kc, hf * 512:(hf + 1) * 512], kc == 0, kc == KC - 1, [bysrc, bwo], [bpO[j]])
        ph.tt("dve", x1[j][:], pO[j][:].rearrange("p a b -> p (a b)"), xt[j][:], ALU.add, [bpO[j], bxt[j]], [bx1[j]])
        ph.dma("pool", x1v[t], x1[j][:], [bx1[j]], [], final=True, key="st")
        ph.act(junk[:], x1[j][:], AF.Square, [bx1[j]], [bj, bss[j]], accum=ss[j][:])
        ph.rsqrt(ss[j][:], ss[j][:], [bss[j], bid], [bss[j]], scale=1.0 / D, bias=eps[:])
        ph.ts("dve", xn[j][:], x1[j][:], ss[j][:, 0:1], None, ALU.mult, None, [bx1[j], bss[j]], [bxn[j]])
        for kc in range(KC):
            ph.tr(pT[j][:, kc, :], xn[j][:, kc * 128:(kc + 1) * 128], ident[:], [bxn[j], bid], [bpT[j]])
        ph.cp("act", hs[j][:], pT[j][:], [bpT[j]], [bhs[j]])
        ph.dma("pool", sc["hT"][:, :, t * 128:(t + 1) * 128], hs[j][:], [bhs[j]], [], final=True, key="st")
        if moe:
            ph.ts("pool", xnf[:], x1[j][:], ss[j][:, 0:1], None, ALU.mult, None, [bx1[j], bss[j]], [bxnf])
            for e_ in range(NE):
                ph.P.add("dve", lambda e, e_=e_: e.scalar_tensor_tensor(out=junkf[:], in0=xnf[:], scalar=1.0, in1=rg[:, e_, :], op0=ALU.mult, op1=ALU.mult, accum_out=lg[:, e_:e_ + 1]), [bxnf, brg], [bjf, blg])
            ph.P.add("dve", lambda e: e.reduce_max(out=m1[:], in_=lg[:], axis=AX.X), [blg], [blg])
            ph.ts("dve", k1[:], lg[:], m1[:, 0:1], None, ALU.is_equal, None, [blg], [blg])
            ph.stt("dve", l2[:], k1[:], -1e30, lg[:], ALU.mult, ALU.add, [blg], [blg])
            ph.P.add("dve", lambda e: e.reduce_max(out=m2[:], in_=l2[:], axis=AX.X), [blg], [blg])
            ph.ts("dve", k2[:], l2[:], m2[:, 0:1], None, ALU.is_equal, None, [blg], [blg])
            ph.tt("dve", g2[:], m2[:], m1[:], ALU.subtract, [blg], [blg])
            ph.act(g2[:], g2[:], AF.Sigmoid, [blg], [blg])
            ph.ts("dve", g1[:], g2[:], -1.0, 1.0, ALU.mult, ALU.add, [blg], [blg])
            ph.ts("dve", k1[:], k1[:], g1[:, 0:1], None, ALU.mult, None, [blg], [blg])
            ph.stt("dve", comb[j][:], k2[:], g2[:, 0:1], k1[:], ALU.mult, ALU.add, [blg], [bcomb[j]])
            ph.dma("pool", sc["comb"][:, t, :], comb[j][:], [bcomb[j]], [], final=True, key="st")
    ph.close()


def _ffn_experts(nc, d, sc, n_exp, moe, final_gain, out):
    ph = Ph(nc)
    FG = 4
    NG = NFC // FG
    hT = ph.sb([128, KC, TPC], BF16)
    bh = [Buf() for _ in range(KC)]
    for kc in range(KC):
        ph.dma("sp", hT[:, kc, :], sc["hT"][:, kc, :], [], [bh[kc]])
    gm = ph.sb([128, KC], F32)
    bgm = Buf()
    ph.dma("sp", gm[:], d["gffn"], [], [bgm])
    acc = ph.sb([128, NT2, D], F32)
    bacc = [Buf() for _ in range(NT2)]
    x1v = sc["x1"].rearrange("(t p) d -> p t d", p=128)
    for t in range(NT2):
        ph.dma("sp", acc[:, t, :], x1v[:, t, :], [], [bacc[t]])
    if moe:
        comb = ph.sb([128, NT2, NE], F32)
        bcomb = Buf()
        ph.dma("sp", comb[:], sc["comb"], [], [bcomb])
    wgs = [ph.sb([128, KC, 2, 128], F32) for _ in range(2)]
    bwgs = [Buf() for _ in range(2)]
    wgb = [ph.sb([128, KC, 2, 128], BF16) for _ in range(2)]
    bwgb = [Buf() for _ in range(2)]
    wds = ph.sb([128, FG, D], F32)
    bwds = Buf()
    wdg = [ph.sb([128, FG, D], BF16) for _ in range(2)]
    bwdg = [Buf() for _ in range(2)]
    aTg = [ph.sb([128, FG, TPC], BF16) for _ in range(2)]
    baTg = [[Buf() for _ in range(FG)] for _ in range(2)]
    pG = [ph.ps([128, 512], F32) for _ in range(2)]
    pU = [ph.ps([128, 512], F32) for _ in range(2)]
    bpG = [Buf() for _ in range(2)]
    bpU = [Buf() for _ in range(2)]
    pD = [ph.ps([128, 2, 512], F32) for _ in range(2)]
    bpD = [Buf() for _ in range(2)]
    sg = [ph.sb([128, 512], F32) for _ in range(2)]
    bsg = [Buf() for _ in range(2)]
    it = 0
    gi = 0
    di = 0
    wi = 0
    pending = []
    for e_ in range(n_exp):
        wd_v = d["w_down"][e_].rearrange("(g f p) n -> g p f n", p=128, f=FG)
        for g in range(NG):
            gb_ = gi % 2
            gi += 1
            ph.dma("sp", wds[:], wd_v[g], [], [bwds])
            ph.cp("pool", wdg[gb_][:], wds[:], [bwds], [bwdg[gb_]])
            for f in range(FG):
                fc = g * FG + f
                j = wi % 2
                wi += 1
                ph.dma("sp", wgs[j][:], d["w_gu"][e_, fc], [], [bwgs[j]])
                ph.tt("pool", wgb[j][:].rearrange("p k a n -> p k (a n)"), wgs[j][:].rearrange("p k a n -> p k (a n)"),
                      gm[:].unsqueeze(2).to_broadcast([128, KC, 256]), ALU.mult, [bwgs[j], bgm], [bwgb[j]])
                for tb in range(NB2):
                    a = it % 2
                    it += 1
                    tsl = slice(tb * 512, (tb + 1) * 512)
                    for kc in range(KC):
                        ph.mm(pG[a][:], wgb[j][:, kc, 0, :], hT[:, kc, tsl], kc == 0, kc == KC - 1, [bwgb[j], bh[kc]], [bpG[a]])
                    for kc in range(KC):
                        ph.mm(pU[a][:], wgb[j][:, kc, 1, :], hT[:, kc, tsl], kc == 0, kc == KC - 1, [bwgb[j], bh[kc]], [bpU[a]])
                    ph.act(sg[a][:], pG[a][:], AF.Silu, [bpG[a]], [bsg[a]])
                    ph.tt("dve", aTg[gb_][:, f, tsl], pU[a][:], sg[a][:], ALU.mult, [bpU[a], bsg[a]], [baTg[gb_][f]])
                if f == 0:
                    for fn in pending:
                        fn()
                    del pending[:]
            def down(gb_=gb_, e_=e_):
                nonlocal di
                for t in range(NT2):
                    k = di % 2
                    di += 1
                    for hf in range(2):
                        for f in range(FG):
                            ph.mm(pD[k][:, hf, :], aTg[gb_][:, f, t * 128:(t + 1) * 128], wdg[gb_][:, f, hf * 512:(hf + 1) * 512],
                                  f == 0, f == FG - 1, [baTg[gb_][f], bwdg[gb_]], [bpD[k]])
                    pflat = pD[k][:].rearrange("p a b -> p (a b)")
                    if moe:
                        ph.stt("dve", acc[:, t, :], pflat, comb[:, t, e_:e_ + 1], acc[:, t, :], ALU.mult, ALU.add, [bpD[k], bcomb, bacc[t]], [bacc[t]])
                    else:
                        ph.tt("dve", acc[:, t, :], pflat, acc[:, t, :], ALU.add, [bpD[k], bacc[t]], [bacc[t]])
            pending.append(down)
    for fn in pending:
        fn()
    del pending[:]
    ov = out.rearrange("(t p) d -> t p d", p=128)
    if final_gain is None:
        for t in range(NT2):
            ph.dma("pool", ov[t], acc[:, t, :], [bacc[t]], [], final=True)
    else:
        gb = wds[:, 0, :]
        bgb = bwds
        ph.dma("sp", gb, final_gain.partition_broadcast(128), [], [bgb])
        eps = ph.sb([128, 1], F32)
        beps_ = Buf()
        ph.memset("pool", eps[:], 1e-6, [beps_])
        ss = ph.sb([128, NT2], F32)
        bss = Buf()
        junk = aTg[0][:, 0, 0:D]
        bj = baTg[0][0]
        for t in range(NT2):
            ph.act(junk, acc[:, t, :], AF.Square, [bacc[t]], [bj, bss], accum=ss[:, t:t + 1])
        ph.rsqrt(ss[:], ss[:], [bss, beps_], [bss], scale=1.0 / D, bias=eps[:])
        for t in range(NT2):
            ph.stt("dve", acc[:, t, :], acc[:, t, :], ss[:, t:t + 1], gb, ALU.mult, ALU.mult, [bacc[t], bss, bgb], [bacc[t]])
            ph.dma("pool", ov[t], acc[:, t, :], [bacc[t]], [], final=True)
    ph.close()


def _lam_init(layer):
    import math
    return 0.8 - 0.6 * math.exp(-0.3 * layer)


MIXER_INPUTS = {
    "x": ([S, D], F32), "gmix": ([128, KC], F32), "att_pp": ([128, 8], F32), "lam_q": ([1, 128], F32),
    "lam_k": ([1, 128], F32), "pos": ([1, S], I32), "w_att": ([D, 768], F32), "w_att_sw": ([D, 512], F32),
    "pool_pp": ([128, 72], F32), "w_pool": ([D, 128], F32), "pool_mix_bd": ([128, 128], F32),
    "mu": ([2, 640], F32), "w_rwkv": ([D, 640], F32), "gate_up": ([128, 128], F32),
    "rw_pp": ([128, 16], F32), "blockones": ([128, 128], F32), "lora": ([4, 128, 128], F32),
    "masks": ([3, 128, 128], F32), "ident": ([128, 128], F32), "ident64x2": ([128, 64], F32),
}
MIXER_INPUTS_L1 = {"w_vdown": ([D, 32], F32), "vres_up": ([32, 128], F32), "vres_bias": ([128, 1], F32),
                   "vfirst_in": ([128, S], F32)}


def _mixer_body(nc, layer, d, x, yT, vfirst, stages=None, rowmap=None):
    def scr(name, shape, dt):
        return nc.dram_tensor(f"scr{layer}_{name}", shape, dt, kind="Internal").ap()
    hT_dram = scr("hT", [128, KC, S], BF16)
    rw = {"tok": scr("tok", [128, 5, S], BF16), "bonus": scr("bonus", [128, S], BF16),
          "sc": [scr(f"sc{i}", [128, QN, S], BF16) for i in range(2)],
          "wc": [scr(f"wc{i}", [128, S // 128], F32) for i in range(2)],
          "y": [scr(f"y{i}", [128, S], F32) for i in range(2)], "vfirst": vfirst}
    _norm_to_hT(nc, x, None, hT_dram, rowmap)
    if stages is None or "att" in stages:
        _attention(nc, hT_dram, d, _lam_init(layer), yT)
    if stages is None or "pool" in stages:
        _pool_mixer(nc, hT_dram, d, yT)
    if stages is None or "rwkv" in stages or "rwkv1" in stages:
        _rwkv_proj(nc, hT_dram, d, layer, rw)
    if stages is None or "rwkv" in stages or "rwkv2" in stages:
        _rwkv_derive(nc, d, rw)
    if stages is None or "rwkv" in stages or "rwkv3" in stages:
        _rwkv_scan(nc, d, rw)
    if stages is None or "rwkv" in stages or "rwkv4" in stages:
        _rwkv_final(nc, d, rw, yT)
    return rw


def build_mixer(layer, stages=None):
    nc = bass.Bass("TRN2", target_bir_lowering=False)
    d = {}
    spec = dict(MIXER_INPUTS)
    if layer > 0:
        spec.update(MIXER_INPUTS_L1)
    for k, (shp, dt) in spec.items():
        d[k] = nc.dram_tensor(k, shp, dt, kind="ExternalInput").ap()
    yT = nc.dram_tensor("yT", [512, S], BF16, kind="ExternalOutput").ap()
    if layer == 0:
        vfirst = nc.dram_tensor("vfirst", [128, S], F32, kind="ExternalOutput").ap()
    else:
        vfirst = d["vfirst_in"]
    _mixer_body(nc, layer, d, d["x"], yT, vfirst, stages)
    return nc


def build_ffn(layer):
    moe = (layer % 2 == 1)
    last = (layer == 1)
    n_exp = NE if moe else 1
    nc = bass.Bass("TRN2", target_bir_lowering=False)
    d = {}
    spec = {"x": ([TPC, D], F32), "yT": ([D, TPC], BF16), "w_out": ([D, D], F32), "ident": ([128, 128], F32),
            "gffn": ([128, KC], F32), "w_gu": ([n_exp, NFC, 128, KC, 2, 128], F32),
            "w_down": ([n_exp, DFF, D], F32)}
    if moe:
        spec.update({"gffn_row": ([1, D], F32), "routerT": ([NE, D], F32)})
    if last:
        spec["gout_row"] = ([1, D], F32)
    for k, (shp, dt) in spec.items():
        d[k] = nc.dram_tensor(k, shp, dt, kind="ExternalInput").ap()
    out = nc.dram_tensor("out", [TPC, D], F32, kind="ExternalOutput").ap()
    sc = {"x1": nc.dram_tensor("s_x1", [TPC, D], F32, kind="Internal").ap(),
          "hT": nc.dram_tensor("s_hT", [128, KC, TPC], BF16, kind="Internal").ap(),
          "aT": nc.dram_tensor("s_aT", [128, NFC, TPC], BF16, kind="Internal").ap()}
    if moe:
        sc["comb"] = nc.dram_tensor("s_comb", [128, NT2, NE], F32, kind="Internal").ap()
    _outproj_norm(nc, d["x"], d["yT"], d, sc, moe)
    _ffn_experts(nc, d, sc, n_exp, moe, d["gout_row"] if last else None, out)
    return nc


def _consts():
    c = {}
    c["blockones"] = np.kron(np.eye(2, dtype=np.float32), np.ones((64, 64), np.float32))
    c["masks"] = np.stack([np.triu(np.ones((128, 128), np.float32), 1), np.tril(np.ones((128, 128), np.float32), -1),
                           np.triu(np.ones((128, 128), np.float32), 0)])
    c["ident"] = np.eye(128, dtype=np.float32)
    c["ident64x2"] = np.concatenate([np.eye(64, dtype=np.float32)] * 2, axis=0)
    return c


def _colmajor(v):
    return np.ascontiguousarray(v.reshape(KC, 128).T)


def _mixer_inputs(layer, b, hh, inp, x_b, consts, vfirst=None):
    f32 = np.float32
    l = layer
    w_in = inp["w_in_first"] if l == 0 else inp["w_in_rest"][l - 1]
    o_q = 1024 if l == 0 else 1056
    o_k, o_v, o_p = o_q + 512, o_q + 1024, o_q + 1536
    hs = [2 * hh, 2 * hh + 1]
    m = dict(consts)
    m["x"] = x_b
    m["gmix"] = _colmajor(inp["norm_mix"][l])
    m["pos"] = np.ascontiguousarray(inp["positions"][b:b + 1].astype(np.int32))
    qc = np.concatenate([np.arange(o_q + h * 128, o_q + (h + 1) * 128) for h in hs])
    kc = np.concatenate([np.arange(o_k + h * 128, o_k + (h + 1) * 128) for h in hs])
    vc = np.concatenate([np.arange(o_v + h * 128, o_v + (h + 1) * 128) for h in hs])
    m["w_att"] = np.ascontiguousarray(w_in[:, np.concatenate([qc, kc, vc])])
    perm = np.arange(64)
    perm[0:8] = np.arange(8, 16)
    perm[8:16] = np.arange(0, 8)
    perm512 = np.concatenate([blk * 64 + perm for blk in range(8)])
    m["w_att_sw"] = np.ascontiguousarray(w_in[:, np.concatenate([qc, kc])][:, perm512])
    half = 8
    inv_freq = np.power(f32(500000.0), -(np.arange(half, dtype=f32) * f32(2.0) / f32(16)))
    pp = np.zeros((128, 8), f32)
    for p in range(128):
        dloc = p % 64
        if dloc < 16:
            pp[p, 0] = inv_freq[dloc % 8]
            pp[p, 1] = -1.0 if dloc < 8 else 1.0
    pp[:, 2] = inp["subln_w"][l]
    m["att_pp"] = pp
    m["lam_q"] = np.ascontiguousarray(inp["lambda_q"][l].reshape(1, 128))
    m["lam_k"] = np.ascontiguousarray(inp["lambda_k"][l].reshape(1, 128))
    gs = hs
    m["w_pool"] = np.ascontiguousarray(w_in[:, o_p + gs[0] * 64:o_p + (gs[1] + 1) * 64])
    pmb = np.zeros((128, 128), f32)
    for i, g in enumerate(gs):
        pmb[i * 64:(i + 1) * 64, i * 64:(i + 1) * 64] = inp["pool_mix"][l, g]
    m["pool_mix_bd"] = pmb
    ppp = np.zeros((128, 72), f32)
    wins = (2, 4, 8, 16)
    for i, g in enumerate(gs):
        rows = slice(i * 64, (i + 1) * 64)
        w = wins[g]
        ppp[rows, g] = 1.0 / w
        for half_ in range(2):
            for cidx in range(8):
                t = cidx if half_ == 0 else S - 8 + cidx
                lo = min(max(t - w // 2, 0), S)
                hi = min(max(t + (w - w // 2), 0), S)
                ppp[rows, 8 + g * 16 + half_ * 8 + cidx] = 1.0 / (hi - lo)
    ppp[:, 4] = inp["pool_scale"][l, gs[0] * 64:(gs[1] + 1) * 64]
    m["pool_pp"] = ppp
    rc = np.arange(hh * 128, (hh + 1) * 128)
    cols = np.concatenate([rc, 256 + rc, 512 + rc, np.arange(768, 896), np.arange(896, 1024)])
    m["w_rwkv"] = np.ascontiguousarray(w_in[:, cols])
    m["mu"] = np.ascontiguousarray(inp["tshift"][l][:, cols])
    m["gate_up"] = np.ascontiguousarray(inp["gate_up"][l][:, rc])
    rp = np.zeros((128, 16), f32)
    rp[:, 0] = inp["k_k"][l, rc]
    rp[:, 1] = inp["k_a"][l, rc]
    rp[:, 2] = inp["r_k"][l].reshape(256)[rc]
    rp[:, 3] = inp["lnx_w"][l, rc]
    rp[:, 4] = inp["lnx_b"][l, rc]
    rp[:, 5] = inp["decay_bias"][l, 0, rc]
    rp[:, 6] = inp["decay_bias"][l, 1, rc]
    rp[:, 7] = inp["iclr_bias"][l, 0, rc]
    rp[:, 8] = inp["iclr_bias"][l, 1, rc]
    m["rw_pp"] = rp
    lora = np.zeros((4, 128, 128), f32)
    for dd in range(2):
        lora[dd, 0:64, :] = inp["decay_up"][l, dd][:, rc]
        lora[2 + dd, 64:128, :] = inp["iclr_up"][l, dd][:, rc]
    m["lora"] = lora
    if l > 0:
        m["w_vdown"] = np.ascontiguousarray(w_in[:, 1024:1056])
        m["vres_up"] = np.ascontiguousarray(inp["vres_up"][l - 1][:, rc])
        m["vres_bias"] = np.ascontiguousarray(inp["vres_bias"][l - 1][rc].reshape(128, 1))
        m["vfirst_in"] = vfirst
    return m


def _wout_rows(gathered=False):
    per = []
    for hh in range(2):
        rows = list(range(hh * 128, (hh + 1) * 128))
        rows += list(range(256 + hh * 256, 256 + (hh + 1) * 256))
        rows += list(range(768 + hh * 128, 768 + (hh + 1) * 128))
        per.append(rows)
    if not gathered:
        return np.array(per[0] + per[1])
    out = []
    for k in range(2):
        for r in range(2):
            out += per[r][k * 256:(k + 1) * 256]
    return np.array(out)


def _ffn_inputs(layer, inp, x_tok, yT_tok, consts, gathered=False):
    l = layer
    m = {"x": x_tok, "yT": yT_tok, "ident": consts["ident"]}
    m["w_out"] = np.ascontiguousarray(inp["w_out"][l][_wout_rows(gathered)])
    m["gffn"] = _colmajor(inp["norm_ffn"][l])
    def gu(wg, wu):
        E = wg.shape[0]
        st = np.stack([wg.reshape(E, KC, 128, NFC, 128), wu.reshape(E, KC, 128, NFC, 128)], axis=0)
        return np.ascontiguousarray(st.transpose(1, 4, 3, 2, 0, 5))
    if l % 2 == 0:
        i = l // 2
        m["w_gu"] = gu(inp["ffn_gate"][i:i + 1], inp["ffn_up"][i:i + 1])
        m["w_down"] = inp["ffn_down"][i:i + 1]
    else:
        i = l // 2
        m["w_gu"] = gu(inp["exp_gate"][i], inp["exp_up"][i])
        m["w_down"] = inp["exp_down"][i]
        m["gffn_row"] = np.ascontiguousarray(inp["norm_ffn"][l].reshape(1, D))
        m["routerT"] = np.ascontiguousarray(inp["router"][i].T)
    if l == 1:
        m["gout_row"] = np.ascontiguousarray(inp["norm_out"].reshape(1, D))
    return m


GROUPS = [[0, 1], [2, 3], [4, 5], [6, 7]]


def _ffn_spec(layer):
    moe = (layer % 2 == 1)
    n_exp = NE if moe else 1
    spec = {"w_out": ([D, D], F32), "ident": ([128, 128], F32), "gffn": ([128, KC], F32),
            "w_gu": ([n_exp, NFC, 128, KC, 2, 128], F32), "w_down": ([n_exp, DFF, D], F32)}
    if moe:
        spec.update({"gffn_row": ([1, D], F32), "routerT": ([NE, D], F32)})
    if layer == 1:
        spec["gout_row"] = ([1, D], F32)
    return spec


def build_fused():
    nc = bass.Bass("TRN2", target_bir_lowering=False)

    def inp(name, shp, dt):
        return nc.dram_tensor(name, shp, dt, kind="ExternalInput").ap()
    x_full = inp("x_full", [S, D], F32)
    x_tok = inp("x_tok", [TPC, D], F32)
    sel = inp("sel", [128, 2], F32)
    out = nc.dram_tensor("out", [TPC, D], F32, kind="ExternalOutput").ap()
    vfirst = nc.dram_tensor("vfirst_s", [128, S], F32, kind="Internal").ap()
    cur_full, cur_tok = x_full, x_tok
    cur_map = None
    for l in range(2):
        spec = dict(MIXER_INPUTS)
        del spec["x"]
        if l > 0:
            spec.update({k: v for k, v in MIXER_INPUTS_L1.items() if k != "vfirst_in"})
        d = {k: inp(f"m{l}_{k}", shp, dt) for k, (shp, dt) in spec.items()}
        yT_t = nc.dram_tensor(f"yT_{l}", [512, S], BF16)
        yTall_t = nc.dram_tensor(f"yTall_{l}", [1024, S], BF16)
        _mixer_body(nc, l, d, cur_full, yT_t.ap(), vfirst, rowmap=cur_map)
        ph = Ph(nc)
        ph.allgather(yTall_t, yT_t, GROUPS, 256)
        ph.close()
        moe = (l % 2 == 1)
        fd = {k: inp(f"f{l}_{k}", shp, dt) for k, (shp, dt) in _ffn_spec(l).items()}
        sc = {"x1": nc.dram_tensor(f"s{l}_x1", [TPC, D], F32, kind="Internal").ap(),
              "hT": nc.dram_tensor(f"s{l}_hT", [128, KC, TPC], BF16, kind="Internal").ap(),
              "aT": nc.dram_tensor(f"s{l}_aT", [128, NFC, TPC], BF16, kind="Internal").ap()}
        if moe:
            sc["comb"] = nc.dram_tensor(f"s{l}_comb", [128, NT2, NE], F32, kind="Internal").ap()
        _outproj_norm(nc, cur_tok, yTall_t.ap(), fd, sc, moe, sel=sel)
        if l == 1:
            _ffn_experts(nc, fd, sc, NE if moe else 1, moe, fd["gout_row"], out)
        else:
            x2h_t = nc.dram_tensor(f"x2h_{l}", [TPC, D], F32)
            x2f_t = nc.dram_tensor(f"x2f_{l}", [S, D], F32)
            _ffn_experts(nc, fd, sc, NE if moe else 1, moe, None, x2h_t.ap())
            ph = Ph(nc)
            ph.allgather(x2f_t, x2h_t, GROUPS, 512)
            ph.close()
            cur_full, cur_tok = x2f_t.ap(), x2h_t.ap()
            cur_map = lambda t: ((t % 16) // 4) * 1024 + (t // 16) * 512 + (t % 4) * 128
    return nc


def kernel(**inp):
    inp = {k: np.asarray(v) for k, v in inp.items()}
    consts = _consts()
    x = np.ascontiguousarray(inp["x"], dtype=np.float32)
    nc = build_fused()
    in_maps = []
    for c in range(8):
        b, hh = c // 2, c % 2
        tsl = slice(hh * TPC, (hh + 1) * TPC)
        m = {"x_full": np.ascontiguousarray(x[b]), "x_tok": np.ascontiguousarray(x[b, tsl])}
        selv = np.zeros((128, 2), np.float32)
        selv[:, hh] = 1.0
        m["sel"] = selv
        for l in range(2):
            mi = _mixer_inputs(l, b, hh, inp, None, consts, None)
            for k, v in mi.items():
                if k in ("x", "vfirst_in"):
                    continue
                m[f"m{l}_{k}"] = v
            fi = _ffn_inputs(l, inp, None, None, consts, gathered=True)
            for k, v in fi.items():
                if k in ("x", "yT"):
                    continue
                m[f"f{l}_{k}"] = v
        in_maps.append(m)
    res = run_bass_kernel_spmd(nc, in_maps, core_ids=list(range(8))).results
    outp = np.empty_like(x)
    for c in range(8):
        b, hh = c // 2, c % 2
        outp[b, hh * TPC:(hh + 1) * TPC] = np.asarray(res[c]["out"])
    return outp
```
